# Optimizing a Trainium2 kernel written in Bass

```python
import math, functools
import jax
import jax.numpy as jnp
from jax import lax
import numpy as np

D_MODEL = 2048
BATCH = 8
SEQ = 4096
DEPTH = 4

N_MIXERS = 4
RMS_EPS = 1e-6
Q_BLOCK = 128

REL_BUCKETS = 32
REL_MAX_DIST = 128
ATTN_HEAD_DIM = 128
ATTN_HEADS = D_MODEL // ATTN_HEAD_DIM

GLA_HEADS = 4
GLA_DK = D_MODEL // (2 * GLA_HEADS)
GLA_DV = D_MODEL // GLA_HEADS
GLA_GATE_RANK = 16
GLA_TAU = 16.0
GLA_CHUNK = 64
GLA_QK_DIM = GLA_HEADS * GLA_DK
GLA_V_DIM = GLA_HEADS * GLA_DV
GLA_IN = 2 * GLA_QK_DIM + 2 * GLA_V_DIM + 2 * GLA_GATE_RANK

GDN_HEAD_DIM = 128
GDN_QK_HEADS = D_MODEL // GDN_HEAD_DIM
GDN_V_HEADS = 2 * GDN_QK_HEADS
GDN_CONV = 5
GDN_CHUNK = 64
GDN_QK_DIM = GDN_QK_HEADS * GDN_HEAD_DIM
GDN_V_DIM = GDN_V_HEADS * GDN_HEAD_DIM
GDN_CONV_DIM = 2 * GDN_QK_DIM + GDN_V_DIM
GDN_IN = GDN_CONV_DIM + GDN_V_DIM + 4 * GDN_V_HEADS

DIFF_HEADS = ATTN_HEADS
DIFF_DQK = D_MODEL // (2 * DIFF_HEADS)
DIFF_DV = 2 * DIFF_DQK
DIFF_IN = 3 * D_MODEL

SWA_HEADS = ATTN_HEADS
SWA_KV_HEADS = 4
SWA_GROUP = SWA_HEADS // SWA_KV_HEADS
SWA_WINDOW = 128
SWA_IN = (SWA_HEADS + 2 * SWA_KV_HEADS) * ATTN_HEAD_DIM

N_EXPERTS = 16
EXPERT_DFF = D_MODEL // 2
EC_CAPACITY_FACTOR = 2

N_GLA = (DEPTH + 3) // N_MIXERS
N_GDN = (DEPTH + 2) // N_MIXERS
N_DIFF = (DEPTH + 1) // N_MIXERS
N_SWA = DEPTH // N_MIXERS

kernel_name = "hybrid_bidir_gla_gdn_diff_swa_ecmoe"


def rms_norm(x, gain):
    xf = x.astype(jnp.float32)
    y = xf * lax.rsqrt(jnp.mean(xf * xf, axis=-1, keepdims=True) + RMS_EPS)
    return (y * gain.astype(jnp.float32)).astype(x.dtype)


def l2_norm(x):
    xf = x.astype(jnp.float32)
    return xf * lax.rsqrt(jnp.sum(xf * xf, axis=-1, keepdims=True) + RMS_EPS)


def t5_bucket(rel):
    half = REL_BUCKETS // 2
    max_exact = half // 2
    n = jnp.abs(rel)
    log_ratio = jnp.log(jnp.maximum(n, 1).astype(jnp.float32) / max_exact) / math.log(REL_MAX_DIST / max_exact)
    large = jnp.minimum(max_exact + (log_ratio * (half - max_exact)).astype(jnp.int32), half - 1)
    return jnp.where(rel > 0, half, 0) + jnp.where(n < max_exact, n, large)


def rel_bias_heads(table, rel):
    return jnp.moveaxis(table[t5_bucket(rel)].astype(jnp.float32), -1, 0)


def to_chunks(t, c):
    return t.reshape(t.shape[:2] + (t.shape[2] // c, c) + t.shape[3:])


def from_chunks(t):
    return t.reshape(t.shape[:2] + (t.shape[2] * t.shape[3],) + t.shape[4:])


def flip_seq(t):
    return jnp.flip(t, axis=2)


def gla_chunk_scan(q, k, v, log_a):
    q, k, v, log_a = (to_chunks(t, GLA_CHUNK) for t in (q, k, v, log_a))
    b = jnp.cumsum(log_a, axis=3)
    b_last = b[:, :, :, -1:, :]
    q_d = q * jnp.exp(b)
    lower = jnp.tril(jnp.ones((GLA_CHUNK, GLA_CHUNK), jnp.float32))
    scores = jnp.einsum('bhnik,bhnjk->bhnij', q_d, k * jnp.exp(-b)) * lower
    o_intra = jnp.einsum('bhnij,bhnjv->bhniv', scores, v)
    k_state = k * jnp.exp(b_last - b)
    decay = jnp.exp(b_last[:, :, :, 0, :])

    def step(state, inp):
        qd_c, ks_c, v_c, dec_c = inp
        o = jnp.einsum('bhik,bhkv->bhiv', qd_c, state)
        state = state * dec_c[..., None] + jnp.einsum('bhjk,bhjv->bhkv', ks_c, v_c)
        return state, o

    B, H = q.shape[:2]
    state0 = jnp.zeros((B, H, q.shape[-1], v.shape[-1]), jnp.float32)
    xs = tuple(jnp.moveaxis(t, 2, 0) for t in (q_d, k_state, v, decay))
    _, o_inter = lax.scan(step, state0, xs)
    return from_chunks(o_intra + jnp.moveaxis(o_inter, 0, 2))


def gla_mixer(h, w_in, w_gate_up, b_gate, head_norm, w_out):
    B, S, _ = h.shape
    splits = [GLA_QK_DIM, 2 * GLA_QK_DIM, 2 * GLA_QK_DIM + GLA_V_DIM, 2 * GLA_QK_DIM + 2 * GLA_V_DIM]
    q, k, v, r, g_lo = jnp.split(h @ w_in, splits, axis=-1)
    gate = jnp.einsum('bsdr,dre->bsde', g_lo.reshape(B, S, 2, GLA_GATE_RANK), w_gate_up) + b_gate
    log_a = jax.nn.log_sigmoid(gate.astype(jnp.float32)) / GLA_TAU

    def heads(t, d):
        return t.astype(jnp.float32).reshape(B, S, GLA_HEADS, d).transpose(0, 2, 1, 3)

    qh = heads(q, GLA_DK) * GLA_DK ** -0.5
    kh, vh = heads(k, GLA_DK), heads(v, GLA_DV)
    la_f, la_b = heads(log_a[:, :, 0], GLA_DK), heads(log_a[:, :, 1], GLA_DK)
    o = gla_chunk_scan(qh, kh, vh, la_f) + flip_seq(
        gla_chunk_scan(flip_seq(qh), flip_seq(kh), flip_seq(vh), flip_seq(la_b)))
    o = rms_norm(o, head_norm).transpose(0, 2, 1, 3).reshape(B, S, GLA_V_DIM).astype(h.dtype)
    return (o * jax.nn.silu(r)) @ w_out


def centred_depthwise_conv(x, w):
    K, C = w.shape
    return lax.conv_general_dilated(x, w[:, None, :], window_strides=(1,), padding=[(K // 2, K // 2)],
                                    dimension_numbers=('NWC', 'WIO', 'NWC'), feature_group_count=C)


def gdn_chunk_scan(q, k, v, g, beta):
    C = GDN_CHUNK
    q, k, v, g, beta = (to_chunks(t, C) for t in (q, k, v, g, beta))
    gc = jnp.cumsum(g, axis=-1)
    lower_incl = jnp.tril(jnp.ones((C, C), bool))
    lower_strict = jnp.tril(jnp.ones((C, C), bool), -1)
    gamma = jnp.exp(jnp.where(lower_incl, gc[..., :, None] - gc[..., None, :], -jnp.inf))
    k_beta = k * beta[..., None]
    tri = jnp.where(lower_strict, jnp.einsum('bhnik,bhnjk->bhnij', k_beta, k) * gamma, 0.0) \
        + jnp.eye(C, dtype=jnp.float32)
    solve = functools.partial(lax.linalg.triangular_solve, left_side=True, lower=True, unit_diagonal=True)
    u = solve(tri, v * beta[..., None])
    w = solve(tri, k_beta * jnp.exp(gc)[..., None])
    qk = jnp.einsum('bhnik,bhnjk->bhnij', q, k) * gamma
    q_d = q * jnp.exp(gc)[..., None]
    k_state = k * jnp.exp(gc[..., -1:] - gc)[..., None]
    decay = jnp.exp(gc[..., -1])

    def step(state, inp):
        qd_c, ks_c, u_c, w_c, qk_c, dec_c = inp
        v_new = u_c - jnp.einsum('bhik,bhkv->bhiv', w_c, state)
        o = jnp.einsum('bhik,bhkv->bhiv', qd_c, state) + jnp.einsum('bhij,bhjv->bhiv', qk_c, v_new)
        state = state * dec_c[..., None, None] + jnp.einsum('bhjk,bhjv->bhkv', ks_c, v_new)
        return state, o

    B, H = q.shape[:2]
    state0 = jnp.zeros((B, H, q.shape[-1], v.shape[-1]), jnp.float32)
    xs = tuple(jnp.moveaxis(t, 2, 0) for t in (q_d, k_state, u, w, qk, decay))
    _, o = lax.scan(step, state0, xs)
    return from_chunks(jnp.moveaxis(o, 0, 2))


def gdn_mixer(h, w_in, conv_w, a_log, dt_bias, head_norm, w_out):
    B, S, _ = h.shape
    qkv, z, ab = jnp.split(h @ w_in, [GDN_CONV_DIM, GDN_CONV_DIM + GDN_V_DIM], axis=-1)
    qkv = jax.nn.silu(centred_depthwise_conv(qkv, conv_w))
    q, k, v = jnp.split(qkv, [GDN_QK_DIM, 2 * GDN_QK_DIM], axis=-1)
    rep = GDN_V_HEADS // GDN_QK_HEADS

    def qk_heads(t):
        t = l2_norm(t.reshape(B, S, GDN_QK_HEADS, GDN_HEAD_DIM))
        return jnp.repeat(t, rep, axis=2).transpose(0, 2, 1, 3)

    qh = qk_heads(q) * GDN_HEAD_DIM ** -0.5
    kh = qk_heads(k)
    vh = v.astype(jnp.float32).reshape(B, S, GDN_V_HEADS, GDN_HEAD_DIM).transpose(0, 2, 1, 3)
    ab = ab.astype(jnp.float32).reshape(B, S, 2, 2, GDN_V_HEADS)
    g = -jnp.exp(a_log.astype(jnp.float32)) * jax.nn.softplus(ab[:, :, 0] + dt_bias.astype(jnp.float32))
    beta = jax.nn.sigmoid(ab[:, :, 1])
    g = g.transpose(2, 0, 3, 1)
    beta = beta.transpose(2, 0, 3, 1)
    o = gdn_chunk_scan(qh, kh, vh, g[0], beta[0]) + flip_seq(
        gdn_chunk_scan(flip_seq(qh), flip_seq(kh), flip_seq(vh), flip_seq(g[1]), flip_seq(beta[1])))
    o = rms_norm(o, head_norm).transpose(0, 2, 1, 3).astype(h.dtype)
    o = o * jax.nn.silu(z.reshape(B, S, GDN_V_HEADS, GDN_HEAD_DIM))
    return o.reshape(B, S, GDN_V_DIM) @ w_out


def diff_mixer(h, w_in, q_norm, k_norm, lam, subln, w_out, rel_table, layer_idx):
    B, S, _ = h.shape
    nb = S // Q_BLOCK
    q, k, v = jnp.split(h @ w_in, 3, axis=-1)
    q = rms_norm(q.reshape(B, S, DIFF_HEADS, 2, DIFF_DQK), q_norm) * DIFF_DQK ** -0.5
    k = rms_norm(k.reshape(B, S, DIFF_HEADS, 2, DIFF_DQK), k_norm)
    v = v.reshape(B, S, DIFF_HEADS, DIFF_DV)
    lambda_init = 0.8 - 0.6 * math.exp(-0.3 * layer_idx)
    lamf = lam.astype(jnp.float32)
    lam_full = jnp.exp(jnp.sum(lamf[0] * lamf[1])) - jnp.exp(jnp.sum(lamf[2] * lamf[3])) + lambda_init
    k_t = k.transpose(0, 2, 3, 1, 4)
    q_blocks = jnp.moveaxis(q.reshape(B, nb, Q_BLOCK, DIFF_HEADS, 2, DIFF_DQK), 1, 0)
    k_pos = jnp.arange(S, dtype=jnp.int32)

    def block(args):
        qb, n = args
        s = jnp.einsum('bqhmd,bhmkd->bhmqk', qb, k_t).astype(jnp.float32)
        q_pos = n * Q_BLOCK + jnp.arange(Q_BLOCK, dtype=jnp.int32)
        s = s + rel_bias_heads(rel_table, k_pos[None, :] - q_pos[:, None])[None, :, None]
        p = jax.nn.softmax(s, axis=-1)
        attn = (p[:, :, 0] - lam_full * p[:, :, 1]).astype(v.dtype)
        return jnp.einsum('bhqk,bkhv->bqhv', attn, v)

    o = lax.map(block, (q_blocks, jnp.arange(nb, dtype=jnp.int32)))
    o = jnp.moveaxis(o, 0, 1).reshape(B, S, DIFF_HEADS, DIFF_DV)
    o = rms_norm(o, subln) * (1.0 - lambda_init)
    return o.reshape(B, S, DIFF_HEADS * DIFF_DV).astype(h.dtype) @ w_out


def swa_mixer(h, w_in, q_norm, k_norm, sink, w_out, rel_table):
    B, S, _ = h.shape
    nb = S // Q_BLOCK
    span = Q_BLOCK + 2 * SWA_WINDOW
    q, k, v = jnp.split(h @ w_in, [SWA_HEADS * ATTN_HEAD_DIM, (SWA_HEADS + SWA_KV_HEADS) * ATTN_HEAD_DIM], axis=-1)
    q = rms_norm(q.reshape(B, S, SWA_HEADS, ATTN_HEAD_DIM), q_norm) * ATTN_HEAD_DIM ** -0.5
    k = rms_norm(k.reshape(B, S, SWA_KV_HEADS, ATTN_HEAD_DIM), k_norm)
    v = v.reshape(B, S, SWA_KV_HEADS, ATTN_HEAD_DIM)
    pad = ((0, 0), (SWA_WINDOW, SWA_WINDOW), (0, 0), (0, 0))
    k_pad, v_pad = jnp.pad(k, pad), jnp.pad(v, pad)
    q_blocks = jnp.moveaxis(q.reshape(B, nb, Q_BLOCK, SWA_KV_HEADS, SWA_GROUP, ATTN_HEAD_DIM), 1, 0)
    rel = jnp.arange(span, dtype=jnp.int32)[None, :] - SWA_WINDOW - jnp.arange(Q_BLOCK, dtype=jnp.int32)[:, None]
    bias = rel_bias_heads(rel_table, rel).reshape(SWA_KV_HEADS, SWA_GROUP, Q_BLOCK, span)
    in_window = jnp.abs(rel) <= SWA_WINDOW
    sink_l = sink.astype(jnp.float32).reshape(1, SWA_KV_HEADS, SWA_GROUP, 1, 1)

    def block(args):
        qb, n = args
        start = n * Q_BLOCK
        kb = lax.dynamic_slice_in_dim(k_pad, start, span, axis=1)
        vb = lax.dynamic_slice_in_dim(v_pad, start, span, axis=1)
        key_pos = start - SWA_WINDOW + jnp.arange(span, dtype=jnp.int32)
        valid = in_window & (key_pos >= 0)[None, :] & (key_pos < S)[None, :]
        s = jnp.einsum('bqkgd,bjkd->bkgqj', qb, kb).astype(jnp.float32) + bias[None]
        s = jnp.where(valid, s, -jnp.inf)
        m = jnp.maximum(jnp.max(s, axis=-1, keepdims=True), sink_l)
        p = jnp.exp(s - m)
        p = p / (jnp.sum(p, axis=-1, keepdims=True) + jnp.exp(sink_l - m))
        return jnp.einsum('bkgqj,bjkd->bqkgd', p.astype(vb.dtype), vb)

    o = lax.map(block, (q_blocks, jnp.arange(nb, dtype=jnp.int32)))
    o = jnp.moveaxis(o, 0, 1).reshape(B, S, SWA_HEADS * ATTN_HEAD_DIM)
    return o @ w_out


def ec_moe(h, router, w1, w3, w2):
    B, S, D = h.shape
    cap = EC_CAPACITY_FACTOR * S // N_EXPERTS
    aff = jax.nn.softmax((h @ router).astype(jnp.float32), axis=-1)
    gate, idx = lax.top_k(jnp.swapaxes(aff, 1, 2), cap)
    xs = jax.vmap(lambda hb, ib: hb[ib])(h, idx)
    a = jnp.einsum('becd,edf->becf', xs, w1)
    b = jnp.einsum('becd,edf->becf', xs, w3)
    y = jnp.einsum('becf,efd->becd', jax.nn.silu(a) * b, w2) * gate[..., None].astype(h.dtype)
    out = jax.vmap(lambda yb, ib: jnp.zeros((S, D), yb.dtype).at[ib.reshape(-1)].add(yb.reshape(-1, D)))(y, idx)
    return out.astype(h.dtype)


def setup_inputs(seed: int = 0) -> dict:
    key = jax.random.key(seed)
    keys = jax.random.split(key, 32)
    kit = (keys[i] for i in range(32))
    D = D_MODEL

    def nrm(shape, scale):
        return jax.random.normal(next(kit), shape, jnp.float32) * scale

    def gain(shape):
        return 1.0 + 0.02 * jax.random.normal(next(kit), shape, jnp.float32)

    x = nrm((BATCH, SEQ, D), 1.0)
    rel_bias = nrm((REL_BUCKETS, ATTN_HEADS), 0.5)
    norm_mix = gain((DEPTH, D))
    norm_ffn = gain((DEPTH, D))

    gla_w_in = nrm((N_GLA, D, GLA_IN), D ** -0.5)
    gla_w_gate_up = nrm((N_GLA, 2, GLA_GATE_RANK, GLA_QK_DIM), GLA_GATE_RANK ** -0.5)
    gla_b_gate = nrm((N_GLA, 2, GLA_QK_DIM), 0.1)
    gla_head_norm = gain((N_GLA, GLA_DV))
    gla_w_out = nrm((N_GLA, GLA_V_DIM, D), GLA_V_DIM ** -0.5)

    gdn_w_in = nrm((N_GDN, D, GDN_IN), D ** -0.5)
    gdn_conv = nrm((N_GDN, GDN_CONV, GDN_CONV_DIM), GDN_CONV ** -0.5)
    gdn_a_log = jnp.log(jax.random.uniform(next(kit), (N_GDN, 2, GDN_V_HEADS), jnp.float32, 1.0, 16.0))
    dt = jnp.exp(jax.random.uniform(next(kit), (N_GDN, 2, GDN_V_HEADS), jnp.float32,
                                    math.log(1e-3), math.log(1e-1)))
    gdn_dt_bias = dt + jnp.log(-jnp.expm1(-dt))
    gdn_head_norm = gain((N_GDN, GDN_HEAD_DIM))
    gdn_w_out = nrm((N_GDN, GDN_V_DIM, D), GDN_V_DIM ** -0.5)

    diff_w_in = nrm((N_DIFF, D, DIFF_IN), D ** -0.5)
    diff_q_norm = gain((N_DIFF, DIFF_DQK))
    diff_k_norm = gain((N_DIFF, DIFF_DQK))
    diff_lambda = nrm((N_DIFF, 4, DIFF_DQK), 0.1)
    diff_subln = gain((N_DIFF, DIFF_DV))
    diff_w_out = nrm((N_DIFF, DIFF_HEADS * DIFF_DV, D), (DIFF_HEADS * DIFF_DV) ** -0.5)

    swa_w_in = nrm((N_SWA, D, SWA_IN), D ** -0.5)
    swa_q_norm = gain((N_SWA, ATTN_HEAD_DIM))
    swa_k_norm = gain((N_SWA, ATTN_HEAD_DIM))
    swa_sink = nrm((N_SWA, SWA_HEADS), 0.5)
    swa_w_out = nrm((N_SWA, SWA_HEADS * ATTN_HEAD_DIM, D), (SWA_HEADS * ATTN_HEAD_DIM) ** -0.5)

    moe_router = nrm((DEPTH, D, N_EXPERTS), D ** -0.5)
    moe_w1 = nrm((DEPTH, N_EXPERTS, D, EXPERT_DFF), D ** -0.5)
    moe_w3 = nrm((DEPTH, N_EXPERTS, D, EXPERT_DFF), D ** -0.5)
    moe_w2 = nrm((DEPTH, N_EXPERTS, EXPERT_DFF, D), EXPERT_DFF ** -0.5)

    return {"x": x, "rel_bias": rel_bias, "norm_mix": norm_mix, "norm_ffn": norm_ffn,
            "gla_w_in": gla_w_in, "gla_w_gate_up": gla_w_gate_up, "gla_b_gate": gla_b_gate,
            "gla_head_norm": gla_head_norm, "gla_w_out": gla_w_out,
            "gdn_w_in": gdn_w_in, "gdn_conv": gdn_conv, "gdn_a_log": gdn_a_log, "gdn_dt_bias": gdn_dt_bias,
            "gdn_head_norm": gdn_head_norm, "gdn_w_out": gdn_w_out,
            "diff_w_in": diff_w_in, "diff_q_norm": diff_q_norm, "diff_k_norm": diff_k_norm,
            "diff_lambda": diff_lambda, "diff_subln": diff_subln, "diff_w_out": diff_w_out,
            "swa_w_in": swa_w_in, "swa_q_norm": swa_q_norm, "swa_k_norm": swa_k_norm,
            "swa_sink": swa_sink, "swa_w_out": swa_w_out,
            "moe_router": moe_router, "moe_w1": moe_w1, "moe_w3": moe_w3, "moe_w2": moe_w2}


def reference(x, rel_bias, norm_mix, norm_ffn,
              gla_w_in, gla_w_gate_up, gla_b_gate, gla_head_norm, gla_w_out,
              gdn_w_in, gdn_conv, gdn_a_log, gdn_dt_bias, gdn_head_norm, gdn_w_out,
              diff_w_in, diff_q_norm, diff_k_norm, diff_lambda, diff_subln, diff_w_out,
              swa_w_in, swa_q_norm, swa_k_norm, swa_sink, swa_w_out,
              moe_router, moe_w1, moe_w3, moe_w2):
    h = x
    for i in range(DEPTH):
        m, j = i % N_MIXERS, i // N_MIXERS
        hn = rms_norm(h, norm_mix[i])
        if m == 0:
            mix = gla_mixer(hn, gla_w_in[j], gla_w_gate_up[j], gla_b_gate[j], gla_head_norm[j], gla_w_out[j])
        elif m == 1:
            mix = gdn_mixer(hn, gdn_w_in[j], gdn_conv[j], gdn_a_log[j], gdn_dt_bias[j], gdn_head_norm[j], gdn_w_out[j])
        elif m == 2:
            mix = diff_mixer(hn, diff_w_in[j], diff_q_norm[j], diff_k_norm[j], diff_lambda[j], diff_subln[j],
                             diff_w_out[j], rel_bias, i)
        else:
            mix = swa_mixer(hn, swa_w_in[j], swa_q_norm[j], swa_k_norm[j], swa_sink[j], swa_w_out[j], rel_bias)
        h = h + mix.astype(h.dtype)
        h = h + ec_moe(rms_norm(h, norm_ffn[i]), moe_router[i], moe_w1[i], moe_w3[i], moe_w2[i])
    return h
```

```python
import numpy as np
from contextlib import ExitStack
import concourse.bass as bass
import concourse.mybir as mybir
from concourse.bass_utils import run_bass_kernel_spmd

F32 = mybir.dt.float32
BF16 = mybir.dt.bfloat16
I32 = mybir.dt.int32
U32 = mybir.dt.uint32
AF = mybir.ActivationFunctionType
ALU = mybir.AluOpType
AX = mybir.AxisListType

S = 4096
D = 2048
NCORES = 8
NDS = 40
SAME_SYNC = True


def _key(x):
    if isinstance(x, str):
        return x
    return getattr(x, 'tensor', x).name


class KB:
    def __init__(self, nc, st):
        self.nc = nc
        self.E = {'pe': nc.tensor, 'dve': nc.vector, 'act': nc.scalar, 'pool': nc.gpsimd, 'sp': nc.sync}
        self.sem = {e: st.enter_context(nc.semaphore('sm_' + e)) for e in self.E}
        self.cnt = {e: 0 for e in self.E}
        self.seen = {e: {} for e in self.E}
        self.lastw = {}
        self.readers = {}
        self.dsem = [st.enter_context(nc.semaphore('sd%d' % i)) for i in range(NDS)]
        self.dtarget = [0] * NDS
        self.dnext = 0
        self.ninst = 0

    def _semof(self, sk):
        return self.sem[sk] if isinstance(sk, str) else self.dsem[sk[1]]

    def wait(self, e, tok):
        sk, val = tok
        if val <= 0:
            return
        if sk == e and not (SAME_SYNC and e in ('dve', 'act', 'pool')):
            return
        if self.seen[e].get(sk, 0) >= val:
            return
        self.E[e].wait_ge(self._semof(sk), val)
        self.seen[e][sk] = val

    def _deps(self, e, w, r):
        for k in r:
            k = _key(k)
            if k in self.lastw:
                self.wait(e, self.lastw[k])
        for k in w:
            k = _key(k)
            if k in self.lastw:
                self.wait(e, self.lastw[k])
            for sk, val in self.readers.get(k, {}).items():
                self.wait(e, (sk, val))

    def _commit(self, tok, w, r):
        for k in r:
            k = _key(k)
            d = self.readers.setdefault(k, {})
            d[tok[0]] = max(d.get(tok[0], 0), tok[1])
        for k in w:
            k = _key(k)
            self.lastw[k] = tok
            self.readers[k] = {}

    def op(self, e, fn, w=(), r=()):
        self._deps(e, w, r)
        inst = fn(self.E[e])
        self.cnt[e] += 1
        inst.then_inc(self.sem[e], 1)
        self._commit((e, self.cnt[e]), w, r)
        self.ninst += 1
        return inst

    def dma(self, q, out, in_, w=None, r=None, fn=None, **kw):
        w = [out] if w is None else w
        r = [in_] if r is None else r
        i = self.dnext
        self.dnext = (i + 1) % NDS
        self.wait(q, (('d', i), self.dtarget[i]))
        self._deps(q, w, r)
        if fn is None:
            inst = self.E[q].dma_start(out=out, in_=in_, **kw)
        else:
            inst = fn(self.E[q])
        self.dtarget[i] += 16
        inst.then_inc(self.dsem[i], 16)
        self._commit((('d', i), self.dtarget[i]), w, r)
        self.ninst += 1

    def barrier(self):
        for e in self.E:
            for e2 in self.E:
                if e2 != e:
                    self.wait(e, (e2, self.cnt[e2]))
            for i in range(NDS):
                self.wait(e, (('d', i), self.dtarget[i]))

    def finish(self):
        for i in range(NDS):
            self.wait('sp', (('d', i), self.dtarget[i]))
        for e in self.E:
            if e != 'sp':
                self.wait('sp', (e, self.cnt[e]))

    def mm(self, out, lhsT, rhs, start=True, stop=True, **kw):
        return self.op('pe', lambda E: E.matmul(out, lhsT, rhs, start=start, stop=stop, **kw), w=[out], r=[lhsT, rhs])

    def tr(self, out, in_, ident):
        return self.op('pe', lambda E: E.transpose(out, in_, ident), w=[out], r=[in_, ident])

    def act(self, out, in_, func, bias=None, scale=None, accum=None, e='act'):
        kw = {}
        r = [in_]
        w = [out]
        if bias is not None:
            kw['bias'] = bias
            if not isinstance(bias, (int, float)):
                r.append(bias)
        if scale is not None:
            kw['scale'] = scale
            if not isinstance(scale, (int, float)):
                r.append(scale)
        if accum is not None:
            kw['accum_out'] = accum
            w.append(accum)
        return self.op('act', lambda E: E.activation(out, in_, func, **kw), w=w, r=r)

    def tt(self, out, a, b, op, e='dve'):
        return self.op(e, lambda E: E.tensor_tensor(out, a, b, op), w=[out], r=[a, b])

    def ts(self, out, a, s1, s2=None, op0=ALU.mult, op1=None, e='dve', accum=None):
        r = [a] + [s for s in (s1, s2) if s is not None and not isinstance(s, (int, float))]
        w = [out] + ([accum] if accum is not None else [])
        kw = {}
        if op1 is not None:
            kw['op1'] = op1
        if accum is not None:
            kw['accum_out'] = accum
        return self.op(e, lambda E: E.tensor_scalar(out, a, s1, s2, op0, **kw), w=w, r=r)

    def stt(self, out, a, sc, b, op0, op1):
        r = [a, b] + ([] if isinstance(sc, (int, float)) else [sc])
        return self.op('dve', lambda E: E.scalar_tensor_tensor(out, a, sc, b, op0, op1), w=[out], r=r)

    def copy(self, out, in_, e='dve'):
        if e == 'act':
            return self.op('act', lambda E: E.copy(out, in_), w=[out], r=[in_])
        return self.op(e, lambda E: E.tensor_copy(out, in_), w=[out], r=[in_])

    def memset(self, out, val, e='dve'):
        return self.op(e, lambda E: E.memset(out, val), w=[out], r=[])


WCOLS = 2048


class WTable:
    def __init__(self):
        self.ents = {}
        self.R = 0

    def add(self, name, K, N):
        n = K * N
        assert n % WCOLS == 0
        rows = n // WCOLS
        self.ents[name] = (self.R, rows, K, N)
        self.R += rows

    def host_shards(self, arrays):
        out = np.empty((self.R, WCOLS), np.float32)
        for name, (ro, rows, K, N) in self.ents.items():
            out[ro:ro + rows] = np.ascontiguousarray(arrays[name]).reshape(rows, WCOLS)
        return out

    def ap(self, gathered_flat, name, k0, kk, n0, nn):
        ro, rows, K, N = self.ents[name]
        base = ro * WCOLS + k0 * N
        return gathered_flat[base: base + kk * N].rearrange("(k n) -> k n", n=N)[:, n0:n0 + nn]


def build_wtable(layers):
    wt = WTable()
    if 0 in layers:
        wt.add('gla_w_in', D, 6176)
        wt.add('gla_w_out', D, D)
    if 1 in layers:
        wt.add('gdn_w_in', D, 12416)
        wt.add('gdn_w_out', 4096, D)
    if 2 in layers:
        wt.add('diff_w_in', D, 6144)
        wt.add('diff_w_out', D, D)
    if 3 in layers:
        wt.add('swa_w_in', D, 3072)
        wt.add('swa_w_out', D, D)
    return wt


class Ctx:
    pass


_UNIQ = [0]


_KB = [None]


def mk_sb(nc, st):
    st.callback(lambda: _KB[0].barrier())

    def sb(name, shape, dt):
        _UNIQ[0] += 1
        return st.enter_context(nc.sbuf_tensor('%s_u%d' % (name, _UNIQ[0]), shape, dt))
    return sb


def make_consts():
    ident = np.eye(128, dtype=np.float32)
    masku = np.triu(np.ones((128, 128), np.float32))
    maskl = np.tril(np.ones((128, 128), np.float32))
    d = {'c_ident': ident, 'c_masku': masku, 'c_maskl': maskl}
    d.update(attn_consts())
    return d


def rows_bcast(ap_row, n=128):
    return ap_row.broadcast_to([n, ap_row.shape[-1]])


def load_consts(kb, cx, st):
    nc = cx.nc
    cx.ident_f = st.enter_context(nc.sbuf_tensor('ident_f', [128, 128], F32))
    cx.ident_b = st.enter_context(nc.sbuf_tensor('ident_b', [128, 128], BF16))
    kb.dma('sp', cx.ident_f[:], cx.dram['c_ident'][:, :])
    kb.copy(cx.ident_b[:], cx.ident_f[:])
    cx.ps = [st.enter_context(nc.psum_tensor('ps%d' % i, [128, 512], F32)) for i in range(3)]
    cx.acc = [st.enter_context(nc.psum_tensor('acc%d' % i, [128, 512], F32)) for i in range(4)]
    cx.pb = [st.enter_context(nc.psum_tensor('pb%d' % i, [128, 1024], BF16)) for i in range(1)]
    cx.psi = 0
    cx.pbi = 0


def next_ps(cx):
    t = cx.ps[cx.psi % len(cx.ps)]
    cx.psi += 1
    return t


def next_pb(cx):
    t = cx.pb[cx.pbi % len(cx.pb)]
    cx.pbi += 1
    return t


def norm_tile(kb, cx, bufs, h_rows, gain_bc, i):
    ht = bufs['h'][i % 2]
    xn = bufs['xn'][i % 2]
    ss = bufs['ss'][i % 2]
    kb.dma('sp', ht[:], h_rows, r=['h'])
    kb.act(bufs['junk'][:], ht[:], AF.Square, accum=ss[:, 0:1])
    kb.act(ss[:, 1:2], ss[:, 0:1], AF.Sqrt, bias=cx.eps_t[:, 0:1], scale=1.0 / D)
    kb.op('dve', lambda E: E.reciprocal(ss[:, 2:3], ss[:, 1:2]), w=[ss], r=[ss])
    kb.stt(xn[:], ht[:], ss[:, 2:3], gain_bc[:], ALU.mult, ALU.mult)
    return xn


def transpose_to(kb, cx, src_bf, dstT, tok0, ntok=128, nchunks=16):
    for c0 in range(0, nchunks, 8):
        pb = next_pb(cx)
        n = min(8, nchunks - c0)
        for c in range(n):
            kb.tr(pb[:, c * 128:(c + 1) * 128], src_bf[:, (c0 + c) * 128:(c0 + c + 1) * 128], cx.ident_b[:])
        kb.copy(dstT[:, c0:c0 + n, tok0:tok0 + 128],
                pb[:, 0:n * 128].rearrange("p (c t) -> p c t", t=128), e='act' if (c0 // 8) % 2 else 'dve')


def moe_layer(kb, cx, li):
    nc = cx.nc
    S = cx.S
    NE, DFF = 16, 1024
    CAP = 2 * S // NE
    NJ = CAP // 128
    h = cx.h
    hn = cx.hn_bf
    with ExitStack() as st:
        sb = mk_sb(nc, st)
        bufs = {'h': [sb('m_h%d' % i, [128, D], F32) for i in range(2)],
                'xn': [sb('m_xn%d' % i, [128, D], BF16) for i in range(2)],
                'ss': [sb('m_ss%d' % i, [128, 4], F32) for i in range(2)],
                'junk': sb('m_junk', [128, D], BF16)}
        gain = sb('m_gain', [128, D], F32)
        rt_f = sb('m_rtf', [128, 16, NE], F32)
        rt_b = sb('m_rtb', [128, 16, NE], BF16)
        hnT = sb('m_hnT', [128, 16, 128], BF16)
        affT = sb('m_affT', [NE, S], F32)
        lg = [sb('m_lg%d' % i, [128, NE + 4], F32) for i in range(2)]
        kb.dma('sp', gain[:], rows_bcast(cx.dram['norm_ffn'][li:li + 1, :]))
        kb.dma('sp', rt_f[:], cx.dram['moe_router'][li].rearrange("(c p) e -> p c e", p=128))
        kb.copy(rt_b[:], rt_f[:])
        for t in range(S // 128):
            xn = norm_tile(kb, cx, bufs, h[t * 128:(t + 1) * 128, :], gain, t)
            kb.dma('sp', hn[t * 128:(t + 1) * 128, :], xn[:], w=['hn'])
            transpose_to(kb, cx, xn, hnT, 0)
            ps = next_ps(cx)
            for c in range(16):
                kb.mm(ps[:, 0:NE], hnT[:, c, :], rt_b[:, c, :], start=(c == 0), stop=(c == 15))
            l = lg[t % 2]
            kb.op('dve', lambda E: E.reduce_max(l[:, NE:NE + 1], ps[:, 0:NE], AX.X), w=[l], r=[ps])
            kb.ts(l[:, NE + 1:NE + 2], l[:, NE:NE + 1], -1.0, None, op0=ALU.mult)
            kb.act(l[:, 0:NE], ps[:, 0:NE], AF.Exp, bias=l[:, NE + 1:NE + 2], scale=1.0, accum=l[:, NE + 2:NE + 3])
            kb.op('dve', lambda E: E.reciprocal(l[:, NE + 3:NE + 4], l[:, NE + 2:NE + 3]), w=[l], r=[l])
            kb.ts(l[:, 0:NE], l[:, 0:NE], l[:, NE + 3:NE + 4], None, op0=ALU.mult)
            ps2 = next_ps(cx)
            kb.tr(ps2[0:NE, 0:128], l[:, 0:NE], cx.ident_f[:])
            kb.copy(affT[:, t * 128:(t + 1) * 128], ps2[0:NE, 0:128])
        gate = sb('m_gate', [NE, CAP], F32)
        idx = sb('m_idx', [NE, CAP], U32)
        for r8 in range(CAP // 8):
            g8 = gate[:, r8 * 8:(r8 + 1) * 8]
            kb.op('dve', lambda E: E.max(g8, affT[:]), w=[gate], r=[affT])
            kb.op('dve', lambda E: E.max_index(idx[:, r8 * 8:(r8 + 1) * 8], g8, affT[:]), w=[idx], r=[gate, affT])
            kb.op('dve', lambda E: E.match_replace(affT[:], g8, affT[:], -1.0), w=[affT], r=[gate, affT])
        kb.dma('sp', cx.sc_gate[:, :], gate[:], w=['sc_gate'])
        kb.dma('sp', cx.sc_idx[:, :], idx[:], w=['sc_idx'])
        gateT = sb('m_gateT', [128, NE * NJ], F32)
        idxT = sb('m_idxT', [128, NE * NJ], U32)
        for e in range(NE):
            kb.dma('sp', gateT[:, e * NJ:(e + 1) * NJ], cx.sc_gate[e].rearrange("(j p) -> p j", p=128),
                   r=['sc_gate'], allow_slow_non_contiguous=True)
            kb.dma('sp', idxT[:, e * NJ:(e + 1) * NJ], cx.sc_idx[e].rearrange("(j p) -> p j", p=128),
                   r=['sc_idx'], allow_slow_non_contiguous=True)
        xs = [sb('m_xs%d' % i, [128, D], BF16) for i in range(2)]
        xsT = sb('m_xsT', [128, 16, CAP], BF16)
        wa = [sb('m_wa%d' % i, [128, 16, 256], BF16) for i in range(2)]
        wb = [sb('m_wb%d' % i, [128, 16, 256], BF16) for i in range(2)]
        w2t = [sb('m_w2%d' % i, [128, 8, 512], BF16) for i in range(2)]
        gT = sb('m_gT', [128, 8, CAP], BF16)
        tmp = [sb('m_tmp%d' % i, [128, 512], F32) for i in range(2)]
        ybuf = [sb('m_y%d' % i, [128, D], F32) for i in range(NJ)]
        W1 = cx.wmoe[('w1', li)]
        W3 = cx.wmoe[('w3', li)]
        W2 = cx.wmoe[('w2', li)]
        nld = 0
        for e in range(NE):
            for j in range(NJ):
                x_ = xs[j % 2]
                col = e * NJ + j
                kb.dma('pool', x_[:], hn[:, :], r=['hn', idxT], w=[x_],
                       fn=lambda E, x_=x_, col=col: E.indirect_dma_start(
                           out=x_[:], out_offset=None, in_=hn[:, :],
                           in_offset=bass.IndirectOffsetOnAxis(ap=idxT[:, col:col + 1], axis=0)))
                transpose_to(kb, cx, x_, xsT, j * 128)
            for fg in range(4):
                a_ = wa[nld % 2]
                b_ = wb[nld % 2]
                nld += 1
                for c in range(16):
                    kb.dma('sp', a_[:, c, :], W1(e, c * 128, 128, fg * 256, 256), r=['wg'])
                    kb.dma('sp', b_[:, c, :], W3(e, c * 128, 128, fg * 256, 256), r=['wg'])
                for f in range(2):
                    fc = fg * 2 + f
                    pa = next_ps(cx)
                    pbm = next_ps(cx)
                    for c in range(16):
                        kb.mm(pa[:, 0:CAP], a_[:, c, f * 128:(f + 1) * 128], xsT[:, c, :], start=(c == 0), stop=(c == 15))
                    for c in range(16):
                        kb.mm(pbm[:, 0:CAP], b_[:, c, f * 128:(f + 1) * 128], xsT[:, c, :], start=(c == 0), stop=(c == 15))
                    tm = tmp[fc % 2]
                    kb.act(tm[:, 0:CAP], pa[:, 0:CAP], AF.Silu)
                    kb.tt(gT[:, fc, :], tm[:, 0:CAP], pbm[:, 0:CAP], ALU.mult)
            for dc in range(4):
                w_ = w2t[dc % 2]
                for c in range(8):
                    kb.dma('sp', w_[:, c, :], W2(e, c * 128, 128, dc * 512, 512), r=['wg'])
                for j in range(NJ):
                    py = next_ps(cx)
                    for c in range(8):
                        kb.mm(py[:, :], gT[:, c, j * 128:(j + 1) * 128], w_[:, c, :], start=(c == 0), stop=(c == 7))
                    kb.ts(ybuf[j][:, dc * 512:(dc + 1) * 512], py[:, :], gateT[:, e * NJ + j:e * NJ + j + 1], None, op0=ALU.mult)
            for j in range(NJ):
                col = e * NJ + j
                yb = ybuf[j]
                kb.dma('pool', h[:, :], yb[:], w=['h'], r=[yb, idxT],
                       fn=lambda E, yb=yb, col=col: E.indirect_dma_start(
                           out=h[:, :], out_offset=bass.IndirectOffsetOnAxis(ap=idxT[:, col:col + 1], axis=0),
                           in_=yb[:], in_offset=None, compute_op=ALU.add))


def prologue_weights(kb, cx):
    nc = cx.nc
    wsh = cx.dram['wsh']
    with ExitStack() as st:
        f = [st.enter_context(nc.sbuf_tensor('pw_f%d' % i, [128, 4096], F32)) for i in range(2)]
        b = [st.enter_context(nc.sbuf_tensor('pw_b%d' % i, [128, 4096], BF16)) for i in range(2)]
        i = 0
        for name, (ro, rows, K_, N_) in cx.wt.ents.items():
            per = rows * WCOLS // 128
            src = wsh[ro:ro + rows, :].rearrange("r c -> (r c)").rearrange("(p n) -> p n", p=128)
            dst = cx.wbf[name].rearrange("r c -> (r c)").rearrange("(p n) -> p n", p=128)
            for c0 in range(0, per, 4096):
                n = min(4096, per - c0)
                kb.dma('sp', f[i % 2][:, 0:n], src[:, c0:c0 + n])
                kb.copy(b[i % 2][:, 0:n], f[i % 2][:, 0:n], e=('dve' if i % 2 == 0 else 'pool'))
                kb.dma('sp', dst[:, c0:c0 + n], b[i % 2][:, 0:n], w=['wg'])
                i += 1
        kb.barrier()


def build_program(cfg):
    nc = bass.Bass("TRN2", target_bir_lowering=False)
    cx = Ctx()
    cx.nc = nc
    cx.S = cfg['S']
    cx.ncores = cfg.get('ncores', NCORES)
    cx.debug_og = cfg.get('debug_og', False)
    cx.stop = cfg.get('stop', 0)
    Sx = cx.S
    layers = cfg['layers']
    wt = WTable()
    for li in layers:
        if cfg.get('mixers', True):
            if li == 0:
                wt.add('gla_w_in', D, 6176); wt.add('gla_w_out', D, D)
            if li == 1:
                wt.add('gdn_w_in', D, 12416); wt.add('gdn_w_out', 4096, D)
            if li == 2:
                wt.add('diff_w_in', D, 6144); wt.add('diff_w_out', D, D)
            if li == 3:
                wt.add('swa_w_in', D, 3072); wt.add('swa_w_out', D, D)
        if cfg.get('moe', True):
            wt.add('moe_w1_%d' % li, 16 * D, 1024)
            wt.add('moe_w3_%d' % li, 16 * D, 1024)
            wt.add('moe_w2_%d' % li, 16 * 1024, D)
    cx.wt = wt
    dram = {}

    def din(name, shape, dt=F32):
        dram[name] = nc.dram_tensor(name, list(shape), dt, kind="ExternalInput").ap()

    din('x', [Sx, D])
    din('wsh', [wt.R, WCOLS])
    din('c_ident', [128, 128]); din('c_masku', [128, 128]); din('c_maskl', [128, 128]); din('c_bk', [128, 384]); din('c_mneg', [128, 384])
    cx.scr = {}
    din('norm_mix', [4, D]); din('norm_ffn', [4, D]); din('moe_router', [4, D, 16]); din('rel_bias', [32, 16])
    for name, shape in cfg.get('small_inputs', {}).items():
        din(name, shape)
    out = nc.dram_tensor('out', [Sx, D], F32, kind="ExternalOutput").ap()
    cx.dram = dram
    cx.h = out
    cx.hn_bf = nc.dram_tensor('hn_bf', [Sx, D], BF16, kind="Internal").ap()
    cx.sc_gate = nc.dram_tensor('sc_gate', [16, 2 * Sx // 16], F32, kind="Internal").ap()
    cx.sc_idx = nc.dram_tensor('sc_idx', [16, 2 * Sx // 16], U32, kind="Internal").ap()
    cx.wbf = {}
    for name, (ro, rows, K_, N_) in wt.ents.items():
        cx.wbf[name] = nc.dram_tensor('wbf_' + name, [rows, WCOLS], BF16, kind="Internal").ap()

    def Wap(name, k0, kk, n0, nn):
        ro, rows, K_, N_ = wt.ents[name]
        flat = cx.wbf[name].rearrange("r c -> (r c)")
        return flat[k0 * N_: (k0 + kk) * N_].rearrange("(k n) -> k n", n=N_)[:, n0:n0 + nn]
    cx.W = Wap
    cx.wmoe = {}
    for li in layers:
        cx.wmoe[('w1', li)] = (lambda e, k0, kk, n0, nn, li=li: cx.W('moe_w1_%d' % li, e * D + k0, kk, n0, nn))
        cx.wmoe[('w3', li)] = (lambda e, k0, kk, n0, nn, li=li: cx.W('moe_w3_%d' % li, e * D + k0, kk, n0, nn))
        cx.wmoe[('w2', li)] = (lambda e, k0, kk, n0, nn, li=li: cx.W('moe_w2_%d' % li, e * 1024 + k0, kk, n0, nn))

    with ExitStack() as st:
        kb = KB(nc, st)
        cx.kb = kb
        _KB[0] = kb
        load_consts(kb, cx, st)
        cx.eps_t = st.enter_context(nc.sbuf_tensor('eps_t', [128, 1], F32))
        kb.memset(cx.eps_t[:], 1e-6)
        prologue_weights(kb, cx)
        with ExitStack() as st2:
            tb = [st2.enter_context(nc.sbuf_tensor('cp%d' % i, [128, D], F32)) for i in range(2)]
            for t in range(Sx // 128):
                kb.dma('sp', tb[t % 2][:], dram['x'][t * 128:(t + 1) * 128, :])
                kb.dma('sp', cx.h[t * 128:(t + 1) * 128, :], tb[t % 2][:], w=['h'])
            kb.barrier()
        for li in layers:
            if cfg.get('mixers', True):
                MIXERS[li](kb, cx, li)
            if cfg.get('moe', True):
                moe_layer(kb, cx, li)
        kb.finish()
    cx.ninst = kb.ninst
    return nc, cx


MIXERS = {}


def host_weights(inputs, cx):
    arrs = {}
    for name in cx.wt.ents:
        if name.startswith('moe_'):
            kind, li = name[4:6], int(name.split('_')[-1])
            a = inputs['moe_' + kind][li]
            arrs[name] = a.reshape(-1, a.shape[-1])
        else:
            a = inputs[name][0]
            arrs[name] = a
    return cx.wt.host_shards(arrs)


def run(cfg, inputs, xs):
    nc, cx = build_program(cfg)
    shards = host_weights(inputs, cx)
    consts = make_consts()
    in_maps = []
    for c in range(cx.ncores):
        m = {'x': np.ascontiguousarray(xs[c]), 'wsh': shards}
        m.update(consts)
        for k in ('norm_mix', 'norm_ffn', 'moe_router', 'rel_bias'):
            m[k] = np.ascontiguousarray(inputs[k])
        for name in cfg.get('small_inputs', {}):
            m[name] = np.ascontiguousarray(inputs[name]).reshape(cfg['small_inputs'][name])
        in_maps.append(m)
    res = run_bass_kernel_spmd(nc, in_maps, core_ids=list(range(cx.ncores)))
    return [r['out'] for r in res.results]


def proj_in(kb, cx, li, wname, groups):
    nc = cx.nc
    S = cx.S
    TT = 512
    with ExitStack() as st:
        sb = mk_sb(nc, st)
        bufs = {'h': [sb('p_h%d' % i, [128, D], F32) for i in range(2)],
                'xn': [sb('p_xn%d' % i, [128, D], BF16) for i in range(2)],
                'ss': [sb('p_ss%d' % i, [128, 4], F32) for i in range(2)],
                'junk': sb('p_junk', [128, D], BF16)}
        gain = sb('p_gain', [128, D], F32)
        hnT = sb('p_hnT', [128, 16, TT], BF16)
        wts = [sb('p_w%d' % i, [128, 16, 512], BF16) for i in range(2)]
        stg_b = [sb('p_sb%d' % i, [128, 512], BF16) for i in range(3)]
        stg_f = [sb('p_sf%d' % i, [128, 512], F32) for i in range(2)]
        kb.dma('sp', gain[:], rows_bcast(cx.dram['norm_mix'][li:li + 1, :]))
        nw = 0
        ns = 0
        for s0 in range(0, S, TT):
            for j in range(TT // 128):
                xn = norm_tile(kb, cx, bufs, cx.h[s0 + j * 128:s0 + (j + 1) * 128, :], gain, j)
                transpose_to(kb, cx, xn, hnT, j * 128)
            for (col0, ncols, mode, dst, scale) in groups:
                for c0 in range(0, ncols, 512):
                    cw = min(512, ncols - c0)
                    wt = wts[nw % 2]
                    nw += 1
                    for c in range(16):
                        kb.dma('sp', wt[:, c, 0:cw], cx.W(wname, c * 128, 128, col0 + c0, cw), r=['wg'])
                    isb = (dst.dtype == BF16)
                    if mode == 'T':
                        for f0 in range(0, cw, 128):
                            fw = min(128, cw - f0)
                            ps = next_ps(cx)
                            for c in range(16):
                                kb.mm(ps[0:fw, :], wt[:, c, f0:f0 + fw], hnT[:, c, :], start=(c == 0), stop=(c == 15))
                            sg = (stg_b[ns % 3] if isb else stg_f[ns % 2])
                            ns += 1
                            kb.act(sg[0:fw, :], ps[0:fw, :], AF.Copy, scale=float(scale))
                            kb.dma('sp', dst[c0 + f0:c0 + f0 + fw, s0:s0 + TT], sg[0:fw, :], w=[dst])
                    else:
                        for j in range(TT // 128):
                            ps = next_ps(cx)
                            for c in range(16):
                                kb.mm(ps[:, 0:cw], hnT[:, c, j * 128:(j + 1) * 128], wt[:, c, 0:cw], start=(c == 0), stop=(c == 15))
                            sg = (stg_b[ns % 3] if isb else stg_f[ns % 2])
                            ns += 1
                            if ns % 2:
                                kb.act(sg[:, 0:cw], ps[:, 0:cw], AF.Copy, scale=float(scale))
                            else:
                                kb.ts(sg[:, 0:cw], ps[:, 0:cw], float(scale), None, op0=ALU.mult)
                            kb.dma('sp', dst[s0 + j * 128:s0 + (j + 1) * 128, c0:c0 + cw], sg[:, 0:cw], w=[dst])


def proj_out(kb, cx, wname, og, KD):
    nc = cx.nc
    S = cx.S
    KC = KD // 128
    TT = 512
    if getattr(cx, 'debug_og', False):
        with ExitStack() as st:
            sb = mk_sb(nc, st)
            a = [sb('dbg_a%d' % i, [128, 2048], BF16) for i in range(2)]
            b = [sb('dbg_b%d' % i, [128, 2048], F32) for i in range(2)]
            for t in range(S // 128):
                kb.dma('sp', a[t % 2][:], og[t * 128:(t + 1) * 128, 0:2048])
                kb.copy(b[t % 2][:], a[t % 2][:])
                kb.dma('sp', cx.h[t * 128:(t + 1) * 128, :], b[t % 2][:], w=['h'])
        return
    with ExitStack() as st:
        sb = mk_sb(nc, st)
        ogt = [sb('o_og%d' % i, [128, KD], BF16) for i in range(2)]
        ogT = sb('o_ogT', [128, KC, TT], BF16)
        wts = [sb('o_w%d' % i, [128, KC, 512], BF16) for i in range(2)]
        hb = [sb('o_h%d' % i, [128, D], F32) for i in range(4)]
        nw = 0
        for s0 in range(0, S, TT):
            for j in range(4):
                o_ = ogt[j % 2]
                kb.dma('sp', o_[:], og[s0 + j * 128:s0 + (j + 1) * 128, :])
                transpose_to(kb, cx, o_, ogT, j * 128, nchunks=KC)
                kb.dma('sp', hb[j][:], cx.h[s0 + j * 128:s0 + (j + 1) * 128, :], r=['h'])
            for dc in range(4):
                wt = wts[nw % 2]
                nw += 1
                for c in range(KC):
                    kb.dma('sp', wt[:, c, :], cx.W(wname, c * 128, 128, dc * 512, 512), r=['wg'])
                for j in range(4):
                    ps = next_ps(cx)
                    for c in range(KC):
                        kb.mm(ps[:, :], ogT[:, c, j * 128:(j + 1) * 128], wt[:, c, :], start=(c == 0), stop=(c == KC - 1))
                    kb.tt(hb[j][:, dc * 512:(dc + 1) * 512], hb[j][:, dc * 512:(dc + 1) * 512], ps[:, :], ALU.add)
            for j in range(4):
                kb.dma('sp', cx.h[s0 + j * 128:s0 + (j + 1) * 128, :], hb[j][:], w=['h'])


def dscr(cx, name, shape, dt):
    if name not in cx.scr:
        cx.scr[name] = cx.nc.dram_tensor(name, list(shape), dt, kind="Internal").ap()
    return cx.scr[name]


def gla_mixer(kb, cx, li):
    nc = cx.nc
    S = cx.S
    NCH = S // 128
    qT = dscr(cx, 'gla_qT', [1024, S], BF16)
    kT = dscr(cx, 'gla_kT', [1024, S], BF16)
    vv = dscr(cx, 'gla_v', [S, 2048], BF16)
    rr = dscr(cx, 'gla_r', [S, 2048], BF16)
    gl = dscr(cx, 'gla_glo', [32, S], BF16)
    o1 = dscr(cx, 'gla_o1', [S, 2048], F32)
    og = dscr(cx, 'mix_og', [S, 4096], BF16)
    proj_in(kb, cx, li, 'gla_w_in', [(0, 1024, 'T', qT, 1.0 / 16.0), (1024, 1024, 'T', kT, 1.0),
                                     (2048, 2048, 'N', vv, 1.0), (4096, 2048, 'N', rr, 1.0),
                                     (6144, 32, 'T', gl, 1.0)])
    with ExitStack() as st:
        sb = mk_sb(nc, st)
        glo1 = sb('g_glo', [16, S], BF16)
        wup_f = sb('g_wupf', [16, 2, 1024], F32)
        wup = sb('g_wup', [16, 2, 1024], BF16)
        negb = sb('g_negb', [128, 2, 8], F32)
        hng = sb('g_hng', [128, 512], F32)
        mask = [sb('g_mask%d' % d, [128, 128], F32) for d in range(2)]
        qh = sb('g_q', [128, 2, S], BF16)
        kh = sb('g_k', [128, 2, S], BF16)
        qd = sb('g_qd', [128, 2, S], BF16)
        ki = sb('g_ki', [128, 2, S], BF16)
        kstT = sb('g_kstT', [128, S], BF16)
        kst = sb('g_kst', [128, NCH, 256], BF16)
        CSx = sb('g_cs', [128, S + 1], F32)
        T1 = sb('g_t1', [128, S], F32)
        T2 = sb('g_t2', [128, S], F32)
        T3 = sb('g_t3', [128, S], F32)
        dec = sb('g_dec', [128, 2, NCH], F32)
        Sf = sb('g_Sf', [128, 2, 512], F32)
        Sb = sb('g_Sb', [128, 2, 512], BF16)
        vt = [sb('g_v%d' % i, [128, 512], BF16) for i in range(2)]
        rt = [sb('g_r%d' % i, [128, 512], BF16) for i in range(2)]
        AT = [sb('g_AT%d' % i, [128, 128], BF16) for i in range(2)]
        of = [sb('g_of%d' % i, [128, 512], F32) for i in range(2)]
        op_ = [sb('g_op%d' % i, [128, 512], F32) for i in range(2)]
        sr = [sb('g_sr%d' % i, [128, 512], F32) for i in range(2)]
        ob = [sb('g_ob%d' % i, [128, 512], BF16) for i in range(2)]
        ss = [sb('g_ss%d' % i, [128, 4], F32) for i in range(2)]
        junk = sb('g_junk', [128, 512], BF16)
        kb.dma('sp', wup_f[:], cx.dram['gla_w_gate_up'].rearrange("d r e -> r d e"))
        kb.copy(wup[:], wup_f[:])
        for d in range(2):
            kb.dma('sp', negb[:, d, :], cx.dram['gla_b_gate'][d].rearrange("(c p) -> p c", p=128), allow_slow_non_contiguous=True)
        kb.ts(negb[:], negb[:], -1.0, None, op0=ALU.mult)
        kb.dma('sp', hng[:], rows_bcast(cx.dram['gla_head_norm'][0:1, :]))
        kb.dma('sp', mask[0][:], cx.dram['c_masku'][:, :])
        kb.dma('sp', mask[1][:], cx.dram['c_maskl'][:, :])
        kb.memset(CSx[:, 0:1], 0.0)
        ch = lambda t: t.rearrange("p (n c) -> p n c", c=128)
        for hh in range(4):
            kb.dma('sp', qh[:], qT[hh * 256:(hh + 1) * 256, :].rearrange("(c p) s -> p c s", p=128))
            kb.dma('sp', kh[:], kT[hh * 256:(hh + 1) * 256, :].rearrange("(c p) s -> p c s", p=128))
            for d in range(2):
                kb.dma('sp', glo1[:], gl[d * 16:(d + 1) * 16, :])
                for fc in range(2):
                    f0 = hh * 256 + fc * 128
                    for b0 in range(0, S, 512):
                        ps = next_ps(cx)
                        kb.mm(ps[:, :], wup[:, d, f0:f0 + 128], glo1[:, b0:b0 + 512])
                        kb.act(T1[:, b0:b0 + 512], ps[:, :], AF.Exp, bias=negb[:, d, hh * 2 + fc:hh * 2 + fc + 1], scale=-1.0)
                    kb.act(T1[:], T1[:], AF.Ln, bias=1.0, scale=1.0)
                    kb.op('dve', lambda E: E.tensor_tensor_scan(CSx[:, 1:S + 1], T1[:], T1[:], 0.0, ALU.add, ALU.max),
                          w=[CSx], r=[T1])
                    if d == 0:
                        kb.tt(ch(T2[:]), ch(CSx[:, 1:S + 1]), ch(CSx[:, 0:S])[:, :, 0:1].broadcast_to([128, NCH, 128]), ALU.subtract)
                        li_ = 127
                    else:
                        kb.tt(ch(T2[:]), ch(CSx[:, 1:S + 1])[:, :, 127:128].broadcast_to([128, NCH, 128]), ch(CSx[:, 0:S]), ALU.subtract)
                        li_ = 0
                    kb.act(T3[:], T2[:], AF.Exp, scale=-1.0 / 16.0)
                    kb.act(CSx[:, 1:S + 1], T2[:], AF.Exp, scale=1.0 / 16.0)
                    kb.tt(qd[:, fc, :], qh[:, fc, :], T3[:], ALU.mult)
                    kb.tt(T1[:], kh[:, fc, :], CSx[:, 1:S + 1], ALU.mult)
                    kb.copy(ki[:, fc, :], T1[:], e='pool')
                    kb.tt(ch(kstT[:]), ch(T1[:]), ch(T3[:])[:, :, li_:li_ + 1].broadcast_to([128, NCH, 128]), ALU.mult)
                    kb.copy(dec[:, fc, :], ch(T3[:])[:, :, li_])
                    for n0 in range(0, NCH, 8):
                        pb = next_pb(cx)
                        for n in range(8):
                            kb.tr(pb[:, n * 128:(n + 1) * 128], kstT[:, (n0 + n) * 128:(n0 + n + 1) * 128], cx.ident_b[:])
                        kb.copy(kst[:, n0:n0 + 8, fc * 128:(fc + 1) * 128], pb[:, :].rearrange("p (n t) -> p n t", t=128), e='act')
                kb.memset(Sf[:], 0.0)
                kb.memset(Sb[:], 0.0)
                order = list(range(NCH)) if d == 0 else list(range(NCH - 1, -1, -1))
                for it, n in enumerate(order):
                    tk = slice(n * 128, (n + 1) * 128)
                    v_ = vt[it % 2]
                    kb.dma('sp', v_[:], vv[tk, hh * 512:(hh + 1) * 512])
                    ps1 = next_ps(cx)
                    for fc in range(2):
                        kb.mm(ps1[:, 0:128], ki[:, fc, tk], qd[:, fc, tk], start=(fc == 0), stop=(fc == 1))
                    a_ = AT[it % 2]
                    kb.tt(a_[:], ps1[:, 0:128], mask[d][:], ALU.mult)
                    ps2 = next_ps(cx)
                    kb.mm(ps2[:, :], a_[:], v_[:], start=True, stop=False)
                    for fc in range(2):
                        kb.mm(ps2[:, :], qd[:, fc, tk], Sb[:, fc, :], start=False, stop=(fc == 1))
                    if d == 0:
                        o_ = of[it % 2]
                        kb.copy(o_[:], ps2[:, :], e='act')
                        kb.dma('sp', o1[tk, hh * 512:(hh + 1) * 512], o_[:], w=[o1])
                    else:
                        p_ = op_[it % 2]
                        r_ = rt[it % 2]
                        kb.dma('sp', p_[:], o1[tk, hh * 512:(hh + 1) * 512])
                        kb.dma('sp', r_[:], rr[tk, hh * 512:(hh + 1) * 512])
                        o_ = of[it % 2]
                        kb.tt(o_[:], ps2[:, :], p_[:], ALU.add)
                        s_ = ss[it % 2]
                        kb.act(junk[:], o_[:], AF.Square, accum=s_[:, 0:1])
                        kb.act(s_[:, 1:2], s_[:, 0:1], AF.Sqrt, bias=cx.eps_t[:, 0:1], scale=1.0 / 512)
                        kb.op('dve', lambda E, s_=s_: E.reciprocal(s_[:, 2:3], s_[:, 1:2]), w=[s_], r=[s_])
                        kb.stt(o_[:], o_[:], s_[:, 2:3], hng[:], ALU.mult, ALU.mult)
                        sr_ = sr[it % 2]
                        kb.act(sr_[:], r_[:], AF.Silu)
                        b_ = ob[it % 2]
                        kb.tt(b_[:], o_[:], sr_[:], ALU.mult)
                        kb.dma('sp', og[tk, hh * 512:(hh + 1) * 512], b_[:], w=[og])
                    for fc in range(2):
                        ps3 = next_ps(cx)
                        kb.mm(ps3[:, :], kst[:, n, fc * 128:(fc + 1) * 128], v_[:])
                        kb.stt(Sf[:, fc, :], Sf[:, fc, :], dec[:, fc, n:n + 1], ps3[:, :], ALU.mult, ALU.add)
                        kb.copy(Sb[:, fc, :], Sf[:, fc, :], e='act')
    proj_out(kb, cx, 'gla_w_out', og[:, 0:2048], 2048)


MIXERS[0] = gla_mixer


def t5_bucket_np(rel):
    import math
    n = np.abs(rel)
    lr = np.log(np.maximum(n, 1).astype(np.float32) / np.float32(8)) / np.float32(math.log(128 / 8))
    large = np.minimum(8 + (lr * np.float32(8)).astype(np.int32), 15)
    return np.where(rel > 0, 16, 0) + np.where(n < 8, n, large)


def attn_consts():
    kp = np.arange(128)[:, None, None]
    dl = np.arange(-1, 2)[None, :, None]
    qf = np.arange(128)[None, None, :]
    rel = dl * 128 + kp - qf
    bk = t5_bucket_np(rel).astype(np.float32)
    mneg = np.where(np.abs(rel) <= 128, 0.0, -30000.0).astype(np.float32)
    return {'c_bk': np.ascontiguousarray(bk.reshape(128, 384)), 'c_mneg': np.ascontiguousarray(mneg.reshape(128, 384))}


def build_bias_tiles(kb, cx, st, swa):
    nc = cx.nc
    sb0 = mk_sb(nc, st)
    T = sb0('a_T', [128, 16, 384], F32)
    cfar = sb0('a_cfar', [128, 32], F32)
    with ExitStack() as s2:
        sb = mk_sb(nc, s2)
        bk = sb('a_bk', [128, 384], F32)
        mk = sb('a_mk', [128, 32, 384], BF16)
        tb = sb('a_tb', [128, 512], F32)
        mn = sb('a_mn', [128, 384], F32)
        kb.dma('sp', bk[:], cx.dram['c_bk'][:, :])
        kb.dma('sp', mn[:], cx.dram['c_mneg'][:, :])
        kb.dma('sp', tb[:], cx.dram['rel_bias'].rearrange("b h -> (b h)").rearrange("(o n) -> o n", o=1).broadcast_to([128, 512]))
        for b in range(32):
            kb.ts(mk[:, b, :], bk[:], float(b), None, op0=ALU.is_equal)
        for h in range(16):
            if swa:
                kb.copy(T[:, h, :], mn[:])
            else:
                kb.memset(T[:, h, :], 0.0)
            for b in range(32):
                kb.stt(T[:, h, :], mk[:, b, :], tb[:, b * 16 + h:b * 16 + h + 1], T[:, h, :], ALU.mult, ALU.add)
            kb.copy(cfar[:, h * 2:h * 2 + 1], tb[:, 15 * 16 + h:15 * 16 + h + 1])
            kb.copy(cfar[:, h * 2 + 1:h * 2 + 2], tb[:, 31 * 16 + h:31 * 16 + h + 1])
    return T, cfar


def qknorm_pass(kb, cx, src, ncols, G, gain_ap, scale, dstT):
    nc = cx.nc
    S = cx.S
    NG = ncols // G
    NCk = ncols // 128
    with ExitStack() as st:
        sb = mk_sb(nc, st)
        xt = [sb('n_x%d' % i, [128, ncols], F32) for i in range(2)]
        sq = sb('n_sq', [128, ncols], F32)
        xb = [sb('n_xb%d' % i, [128, ncols], BF16) for i in range(2)]
        ss = [sb('n_ss%d' % i, [128, 3, NG], F32) for i in range(2)]
        gn = sb('n_g', [128, G], F32)
        stg = [sb('n_st%d' % i, [128, NCk, 128], BF16) for i in range(2)]
        kb.dma('sp', gn[:], gain_ap.broadcast_to([128, G]))
        kb.ts(gn[:], gn[:], float(scale), None, op0=ALU.mult)
        g3 = lambda t: t.rearrange("p (g d) -> p g d", d=G)
        for t in range(S // 128):
            x_ = xt[t % 2]
            s_ = ss[t % 2]
            b_ = xb[t % 2]
            kb.dma('sp', x_[:], src[t * 128:(t + 1) * 128, 0:ncols])
            kb.tt(sq[:], x_[:], x_[:], ALU.mult)
            kb.op('dve', lambda E, s_=s_: E.tensor_reduce(s_[:, 0, :], g3(sq[:]), AX.X, ALU.add), w=[s_], r=[sq])
            kb.act(s_[:, 1, :], s_[:, 0, :], AF.Sqrt, bias=cx.eps_t[:, 0:1], scale=1.0 / G)
            kb.op('dve', lambda E, s_=s_: E.reciprocal(s_[:, 2, :], s_[:, 1, :]), w=[s_], r=[s_])
            kb.tt(g3(sq[:]), g3(x_[:]), s_[:, 2, :].rearrange("p (g o) -> p g o", o=1).broadcast_to([128, NG, G]), ALU.mult)
            kb.tt(g3(b_[:]), g3(sq[:]), gn[:].rearrange("p (o d) -> p o d", o=1).broadcast_to([128, NG, G]), ALU.mult)
            sg = stg[t % 2]
            transpose_to(kb, cx, b_, sg, 0, nchunks=NCk)
            kb.dma('sp', dstT[0:ncols, :].rearrange("(c p) s -> p c s", p=128)[:, :, t * 128:(t + 1) * 128], sg[:], w=[dstT])


def acc_slots(cx):
    return [(cx.acc[i], 0, True) for i in range(4)]


def diff_mixer(kb, cx, li):
    import math
    nc = cx.nc
    S = cx.S
    NCH = S // 128
    NQT = S // 512
    lam_init = 0.8 - 0.6 * math.exp(-0.3 * li)
    qf = dscr(cx, 'at_q', [S, 2048], F32)
    kf = dscr(cx, 'at_k', [S, 2048], F32)
    vv = dscr(cx, 'at_v', [S, 2048], BF16)
    qT = dscr(cx, 'at_qT', [2048, S], BF16)
    kT = dscr(cx, 'at_kT', [2048, S], BF16)
    og = dscr(cx, 'mix_og', [S, 4096], BF16)
    proj_in(kb, cx, li, 'diff_w_in', [(0, 2048, 'N', qf, 1.0), (2048, 2048, 'N', kf, 1.0), (4096, 2048, 'N', vv, 1.0)])
    qknorm_pass(kb, cx, qf, 2048, 64, cx.dram['diff_q_norm'][0:1, :], 0.125, qT)
    qknorm_pass(kb, cx, kf, 2048, 64, cx.dram['diff_k_norm'][0:1, :], 1.0, kT)
    with ExitStack() as st:
        sb = mk_sb(nc, st)
        T, cfar = build_bias_tiles(kb, cx, st, swa=False)
        lam = sb('d_lam', [128, 4, 64], F32)
        lw = sb('d_lw', [128, 8], F32)
        sub = sb('d_sub', [128, 128], F32)
        qh = sb('d_q', [128, S], BF16)
        kh = sb('d_k', [128, S], BF16)
        v1 = sb('d_v1', [128, NCH, 130], BF16)
        tmp = [sb('d_tmp%d' % i, [128, 512], F32) for i in range(2)]
        PT = [sb('d_PT%d' % i, [128, 512], BF16) for i in range(3)]
        res = [sb('d_res%d' % i, [128, 4, 130], F32) for i in range(2)]
        rc = [sb('d_rc%d' % i, [128, 8], F32) for i in range(2)]
        ot = [sb('d_ot%d' % i, [128, 128], F32) for i in range(2)]
        ob = [sb('d_ob%d' % i, [128, 128], BF16) for i in range(2)]
        junk = sb('d_junk', [128, 128], BF16)
        kb.dma('sp', lam[:], cx.dram['diff_lambda'].rearrange("a d -> (a d)").rearrange("(o n) -> o n", o=1).broadcast_to([128, 256]))
        kb.tt(lam[:, 0, :], lam[:, 0, :], lam[:, 1, :], ALU.mult)
        kb.tt(lam[:, 2, :], lam[:, 2, :], lam[:, 3, :], ALU.mult)
        kb.op('dve', lambda E: E.reduce_sum(lw[:, 0:1], lam[:, 0, :], AX.X), w=[lw], r=[lam])
        kb.op('dve', lambda E: E.reduce_sum(lw[:, 1:2], lam[:, 2, :], AX.X), w=[lw], r=[lam])
        kb.act(lw[:, 2:4], lw[:, 0:2], AF.Exp)
        kb.tt(lw[:, 4:5], lw[:, 3:4], lw[:, 2:3], ALU.subtract)
        kb.ts(lw[:, 5:6], lw[:, 4:5], -lam_init, None, op0=ALU.add)
        kb.dma('sp', sub[:], cx.dram['diff_subln'][0:1, :].broadcast_to([128, 128]))
        kb.ts(sub[:], sub[:], float(1.0 - lam_init), None, op0=ALU.mult)
        kb.memset(v1[:], 1.0)
        slots = acc_slots(cx)
        npt = 0
        for h in range(16):
            kb.dma('sp', qh[:], qT[h * 128:(h + 1) * 128, :])
            kb.dma('sp', kh[:], kT[h * 128:(h + 1) * 128, :])
            kb.dma('sp', v1[:, :, 0:128], vv[:, h * 128:(h + 1) * 128].rearrange("(n p) d -> p n d", p=128))
            for qt in range(NQT):
                for m in range(2):
                    pr = slice(m * 64, (m + 1) * 64)
                    for kc in range(NCH):
                        ps = next_ps(cx)
                        kb.mm(ps[:, :], kh[pr, kc * 128:(kc + 1) * 128], qh[pr, qt * 512:(qt + 1) * 512])
                        p_ = PT[npt % 3]
                        npt += 1
                        dls = [kc - (qt * 4 + qb) for qb in range(4)]
                        if all(abs(dl) >= 2 for dl in dls):
                            sgn = 1 if dls[0] > 0 else 0
                            kb.act(p_[:], ps[:, :], AF.Exp, bias=cfar[:, h * 2 + sgn:h * 2 + sgn + 1], scale=1.0)
                        else:
                            t_ = tmp[npt % 2]
                            for qb, dl in enumerate(dls):
                                blk = slice(qb * 128, (qb + 1) * 128)
                                if abs(dl) <= 1:
                                    kb.tt(t_[:, blk], ps[:, blk], T[:, h, (dl + 1) * 128:(dl + 2) * 128], ALU.add)
                                else:
                                    sgn = 1 if dl > 0 else 0
                                    kb.ts(t_[:, blk], ps[:, blk], cfar[:, h * 2 + sgn:h * 2 + sgn + 1], None, op0=ALU.add)
                            kb.act(p_[:], t_[:], AF.Exp)
                        for qb in range(4):
                            bank, c0, first = slots[qb]
                            kb.mm(bank[:, c0:c0 + 130], p_[:, qb * 128:(qb + 1) * 128], v1[:, kc, :],
                                  start=(kc == 0 and first), stop=(kc == NCH - 1), skip_group_check=True)
                    r_ = res[m]
                    for qb in range(4):
                        bank, c0, first = slots[qb]
                        kb.copy(r_[:, qb, :], bank[:, c0:c0 + 130], e='act' if qb % 2 else 'dve')
                for qb in range(4):
                    c_ = rc[qb % 2]
                    o_ = ot[qb % 2]
                    kb.op('dve', lambda E, c_=c_, qb=qb: E.reciprocal(c_[:, 0:1], res[0][:, qb, 128:129]), w=[c_], r=[res[0]])
                    kb.op('dve', lambda E, c_=c_, qb=qb: E.reciprocal(c_[:, 1:2], res[1][:, qb, 128:129]), w=[c_], r=[res[1]])
                    kb.tt(c_[:, 2:3], c_[:, 1:2], lw[:, 5:6], ALU.mult)
                    kb.ts(o_[:], res[0][:, qb, 0:128], c_[:, 0:1], None, op0=ALU.mult)
                    kb.stt(o_[:], res[1][:, qb, 0:128], c_[:, 2:3], o_[:], ALU.mult, ALU.add)
                    kb.act(junk[:], o_[:], AF.Square, accum=c_[:, 3:4])
                    kb.act(c_[:, 4:5], c_[:, 3:4], AF.Sqrt, bias=cx.eps_t[:, 0:1], scale=1.0 / 128)
                    kb.op('dve', lambda E, c_=c_: E.reciprocal(c_[:, 5:6], c_[:, 4:5]), w=[c_], r=[c_])
                    b_ = ob[qb % 2]
                    kb.stt(b_[:], o_[:], c_[:, 5:6], sub[:], ALU.mult, ALU.mult)
                    t0 = (qt * 4 + qb) * 128
                    kb.dma('sp', og[t0:t0 + 128, h * 128:(h + 1) * 128], b_[:], w=[og])
    proj_out(kb, cx, 'diff_w_out', og[:, 0:2048], 2048)


def swa_mixer(kb, cx, li):
    nc = cx.nc
    S = cx.S
    NCH = S // 128
    qf = dscr(cx, 'at_q', [S, 2048], F32)
    kf = dscr(cx, 'at_k', [S, 2048], F32)
    vv = dscr(cx, 'at_v', [S, 2048], BF16)
    qT = dscr(cx, 'at_qT', [2048, S], BF16)
    kT = dscr(cx, 'at_kT', [2048, S], BF16)
    og = dscr(cx, 'mix_og', [S, 4096], BF16)
    proj_in(kb, cx, li, 'swa_w_in', [(0, 2048, 'N', qf, 1.0), (2048, 512, 'N', kf, 1.0), (2560, 512, 'N', vv, 1.0)])
    qknorm_pass(kb, cx, qf, 2048, 128, cx.dram['swa_q_norm'][0:1, :], 128 ** -0.5, qT)
    qknorm_pass(kb, cx, kf, 512, 128, cx.dram['swa_k_norm'][0:1, :], 1.0, kT)
    with ExitStack() as st:
        sb = mk_sb(nc, st)
        T, cfar = build_bias_tiles(kb, cx, st, swa=True)
        snk = sb('w_snk', [128, 16], F32)
        q4 = sb('w_q4', [128, 4, S], BF16)
        kg = sb('w_kg', [128, S], BF16)
        v1 = sb('w_v1', [128, NCH, 130], BF16)
        tmp = [sb('w_tmp%d' % i, [128, 512], F32) for i in range(2)]
        PT = [sb('w_PT%d' % i, [128, 512], BF16) for i in range(3)]
        rc = [sb('w_rc%d' % i, [128, 4], F32) for i in range(2)]
        ob = [sb('w_ob%d' % i, [128, 128], BF16) for i in range(2)]
        kb.dma('sp', snk[:], cx.dram['swa_sink'][0:1, :].broadcast_to([128, 16]))
        kb.act(snk[:], snk[:], AF.Exp)
        kb.memset(v1[:], 1.0)
        slots = acc_slots(cx)
        npt = 0
        for g in range(4):
            kb.dma('sp', q4[:], qT[g * 512:(g + 1) * 512, :].rearrange("(c p) s -> p c s", p=128))
            kb.dma('sp', kg[:], kT[g * 128:(g + 1) * 128, :])
            kb.dma('sp', v1[:, :, 0:128], vv[:, g * 128:(g + 1) * 128].rearrange("(n p) d -> p n d", p=128))
            for qb in range(NCH):
                kcs = [kc for kc in (qb - 1, qb, qb + 1) if 0 <= kc < NCH]
                for ik, kc in enumerate(kcs):
                    dl = kc - qb
                    ps = next_ps(cx)
                    kb.mm(ps[:, :], kg[:, kc * 128:(kc + 1) * 128], q4[:, :, qb * 128:(qb + 1) * 128])
                    t_ = tmp[npt % 2]
                    p_ = PT[npt % 3]
                    npt += 1
                    for hh in range(4):
                        blk = slice(hh * 128, (hh + 1) * 128)
                        kb.tt(t_[:, blk], ps[:, blk], T[:, g * 4 + hh, (dl + 1) * 128:(dl + 2) * 128], ALU.add)
                    kb.act(p_[:], t_[:], AF.Exp)
                    for hh in range(4):
                        bank, c0, first = slots[hh]
                        kb.mm(bank[:, c0:c0 + 130], p_[:, hh * 128:(hh + 1) * 128], v1[:, kc, :],
                              start=(ik == 0 and first), stop=(ik == len(kcs) - 1), skip_group_check=True)
                for hh in range(4):
                    bank, c0, first = slots[hh]
                    h = g * 4 + hh
                    c_ = rc[hh % 2]
                    kb.tt(c_[:, 0:1], bank[:, c0 + 128:c0 + 129], snk[:, h:h + 1], ALU.add)
                    kb.op('dve', lambda E, c_=c_: E.reciprocal(c_[:, 1:2], c_[:, 0:1]), w=[c_], r=[c_])
                    b_ = ob[hh % 2]
                    kb.ts(b_[:], bank[:, c0:c0 + 128], c_[:, 1:2], None, op0=ALU.mult)
                    kb.dma('sp', og[qb * 128:(qb + 1) * 128, h * 128:(h + 1) * 128], b_[:], w=[og])
    proj_out(kb, cx, 'swa_w_out', og[:, 0:2048], 2048)


MIXERS[2] = diff_mixer
MIXERS[3] = swa_mixer


def gdn_mixer(kb, cx, li):
    nc = cx.nc
    S = cx.S
    NCH = S // 128
    preT = dscr(cx, 'gd_preT', [8192, S], BF16)
    qkT = dscr(cx, 'gd_qkT', [4096, S], BF16)
    vT = dscr(cx, 'gd_vT', [4096, S], BF16)
    zz = dscr(cx, 'gd_z', [S, 4096], BF16)
    abt = dscr(cx, 'gd_ab', [S, 128], F32)
    o1 = dscr(cx, 'gd_o1', [S, 4096], F32)
    og = dscr(cx, 'mix_og', [S, 4096], BF16)
    proj_in(kb, cx, li, 'gdn_w_in', [(0, 8192, 'T', preT, 1.0), (8192, 4096, 'N', zz, 1.0), (12288, 128, 'N', abt, 1.0)])
    with ExitStack() as st:
        sb = mk_sb(nc, st)
        cwl = [sb('c_w%d' % i, [128, 5], F32) for i in range(2)]
        xp = [sb('c_xp%d' % i, [128, S + 4], BF16) for i in range(2)]
        y = sb('c_y', [128, S], F32)
        ys = sb('c_ys', [128, S], F32)
        sq = sb('c_sq', [128, S], BF16)
        rs = sb('c_rs', [128, 512], F32)
        ob = [sb('c_ob%d' % i, [128, S], BF16) for i in range(2)]
        onesb = sb('c_ones', [128, 128], BF16)
        kb.memset(onesb[:], 1.0)
        for i in range(2):
            kb.memset(xp[i][:, 0:2], 0.0)
            kb.memset(xp[i][:, S + 2:S + 4], 0.0)
        for c in range(64):
            x_ = xp[c % 2]
            kb.dma('sp', x_[:, 2:S + 2], preT[c * 128:(c + 1) * 128, :])
            cw = cwl[c % 2]
            kb.dma('sp', cw[:], cx.dram['gdn_conv'][:, c * 128:(c + 1) * 128].rearrange("j p -> p j"), allow_slow_non_contiguous=True)
            kb.ts(y[:], x_[:, 0:S], cw[:, 0:1], None, op0=ALU.mult)
            for j in range(1, 5):
                kb.stt(y[:], x_[:, j:j + S], cw[:, j:j + 1], y[:], ALU.mult, ALU.add)
            kb.act(ys[:], y[:], AF.Silu)
            o_ = ob[c % 2]
            if c < 32:
                kb.tt(sq[:], ys[:], ys[:], ALU.mult, e='pool')
                for b0 in range(0, S, 512):
                    ps = next_ps(cx)
                    kb.mm(ps[:, :], onesb[:], sq[:, b0:b0 + 512])
                    kb.act(rs[:], ps[:, :], AF.Sqrt, bias=cx.eps_t[:, 0:1], scale=1.0)
                    kb.op('dve', lambda E: E.reciprocal(rs[:], rs[:]), w=[rs], r=[rs])
                    if c < 16:
                        kb.stt(o_[:, b0:b0 + 512], ys[:, b0:b0 + 512], 128 ** -0.5, rs[:], ALU.mult, ALU.mult)
                    else:
                        kb.tt(o_[:, b0:b0 + 512], ys[:, b0:b0 + 512], rs[:], ALU.mult)
                kb.dma('sp', qkT[c * 128:(c + 1) * 128, :], o_[:], w=[qkT])
            else:
                kb.copy(o_[:], ys[:], e='pool')
                kb.dma('sp', vT[(c - 32) * 128:(c - 31) * 128, :], o_[:], w=[vT])
    with ExitStack() as st:
        sb = mk_sb(nc, st)
        psl = cx.ps + cx.acc
        pidx = [0]

        def nps():
            t = psl[pidx[0] % len(psl)]
            pidx[0] += 1
            return t
        g_all = sb('s_g', [128, NCH, 64], F32)
        nb_all = sb('s_nb', [128, NCH, 64], F32)
        gc_all = sb('s_gc', [128, NCH, 64], F32)
        ngc_all = sb('s_ngc', [128, NCH, 64], F32)
        alog = sb('s_alog', [128, 64], F32)
        dtb = sb('s_dtb', [128, 64], F32)
        hng = sb('s_hng', [128, 128], F32)
        UT = sb('s_ut', [128, 128], F32)
        LT = sb('s_lt', [128, 128], F32)
        MSU = sb('s_msu', [128, 128], F32)
        MSL = sb('s_msl', [128, 128], F32)
        abl = [sb('s_ab%d' % i, [128, 128], F32) for i in range(2)]
        tmpa = sb('s_tmpa', [128, 64], F32)
        kb.dma('sp', UT[:], cx.dram['c_masku'][:, :])
        kb.dma('sp', LT[:], cx.dram['c_maskl'][:, :])
        kb.tt(MSU[:], UT[:], cx.ident_f[:], ALU.subtract)
        kb.tt(MSL[:], LT[:], cx.ident_f[:], ALU.subtract)
        kb.dma('sp', alog[:], cx.dram['gdn_a_log'].rearrange("d h -> (d h)").rearrange("(o n) -> o n", o=1).broadcast_to([128, 64]))
        kb.dma('sp', dtb[:], cx.dram['gdn_dt_bias'].rearrange("d h -> (d h)").rearrange("(o n) -> o n", o=1).broadcast_to([128, 64]))
        kb.dma('sp', hng[:], cx.dram['gdn_head_norm'][0:1, :].broadcast_to([128, 128]))
        kb.act(alog[:], alog[:], AF.Exp)
        kb.ts(alog[:], alog[:], -1.0, None, op0=ALU.mult)
        for n in range(NCH):
            a_ = abl[n % 2]
            kb.dma('sp', a_[:], abt[n * 128:(n + 1) * 128, :])
            kb.tt(tmpa[:], a_[:, 0:64], dtb[:], ALU.add)
            kb.act(tmpa[:], tmpa[:], AF.Exp)
            kb.act(tmpa[:], tmpa[:], AF.Ln, bias=1.0, scale=1.0)
            kb.tt(g_all[:, n, :], tmpa[:], alog[:], ALU.mult)
            kb.act(tmpa[:], a_[:, 64:128], AF.Exp, scale=-1.0)
            kb.ts(tmpa[:], tmpa[:], 1.0, None, op0=ALU.add)
            kb.op('dve', lambda E: E.reciprocal(tmpa[:], tmpa[:]), w=[tmpa], r=[tmpa])
            kb.ts(nb_all[:, n, :], tmpa[:], -1.0, None, op0=ALU.mult)
            ps = nps()
            kb.mm(ps[:, 0:32], UT[:], g_all[:, n, 0:32])
            kb.mm(ps[:, 32:64], LT[:], g_all[:, n, 32:64])
            kb.copy(gc_all[:, n, :], ps[:, 0:64])
            kb.ts(ngc_all[:, n, :], gc_all[:, n, :], -1.0, None, op0=ALU.mult, e='dve')
        qh = sb('s_q', [128, S], BF16)
        kh = sb('s_k', [128, S], BF16)
        ktok = sb('s_ktok', [128, NCH, 128], BF16)
        vth = sb('s_vth', [128, S], BF16)
        vtok = [sb('s_vtok%d' % i, [128, NCH, 128], BF16) for i in range(2)]
        bv = [sb('s_bv%d' % i, [128, NCH, 128], BF16) for i in range(4)]
        Sf = [sb('s_Sf%d' % i, [128, 128], F32) for i in range(4)]
        Sb = [sb('s_Sb%d' % i, [128, 128], BF16) for i in range(4)]
        KKs = [sb('s_kk%d' % i, [128, 128], F32) for i in range(2)]
        QKr = [sb('s_qkr%d' % i, [128, 128], F32) for i in range(2)]
        R = 2
        mkf = lambda nm: [sb('s_%s%d' % (nm, i), [128, 128], F32) for i in range(R)]
        mkb = lambda nm: [sb('s_%s%d' % (nm, i), [128, 128], BF16) for i in range(R)]
        gB, gR, GT, GG, Er = mkf('gB'), mkf('gR'), mkf('GT'), mkf('GG'), mkf('Er')
        Xa, Xb_, Ya, Yb_, Pa = mkf('Xa'), mkf('Xb'), mkf('Ya'), mkf('Yb'), mkf('Pa')
        QKT, MT, kdT, qdT, kst, Xv, vnb = mkb('QKT'), mkb('MT'), mkb('kdT'), mkb('qdT'), mkb('kst'), mkb('Xv'), mkb('vnb')
        sc = [sb('s_sc%d' % i, [128, 8], F32) for i in range(R)]
        of = [sb('s_of%d' % i, [128, 128], F32) for i in range(2)]
        op_ = [sb('s_op%d' % i, [128, 128], F32) for i in range(2)]
        zt = [sb('s_z%d' % i, [128, 128], BF16) for i in range(2)]
        zs = [sb('s_zs%d' % i, [128, 128], F32) for i in range(2)]
        obf = [sb('s_obf%d' % i, [128, 128], BF16) for i in range(2)]
        junk = sb('s_junk', [128, 128], BF16)
        rot = [0]
        for hq in range(16):
            kb.dma('sp', qh[:], qkT[hq * 128:(hq + 1) * 128, :])
            kb.dma('sp', kh[:], qkT[2048 + hq * 128:2048 + (hq + 1) * 128, :])
            for n0 in range(0, NCH, 8):
                pb = next_pb(cx)
                for n in range(8):
                    kb.tr(pb[:, n * 128:(n + 1) * 128], kh[:, (n0 + n) * 128:(n0 + n + 1) * 128], cx.ident_b[:])
                kb.copy(ktok[:, n0:n0 + 8, :], pb[:, :].rearrange("p (n t) -> p n t", t=128), e='act')
            chains = []
            for v2 in range(2):
                hv = hq * 2 + v2
                kb.dma('sp', vth[:], vT[hv * 128:(hv + 1) * 128, :])
                for n0 in range(0, NCH, 8):
                    pb = next_pb(cx)
                    for n in range(8):
                        kb.tr(pb[:, n * 128:(n + 1) * 128], vth[:, (n0 + n) * 128:(n0 + n + 1) * 128], cx.ident_b[:])
                    kb.copy(vtok[v2][:, n0:n0 + 8, :], pb[:, :].rearrange("p (n t) -> p n t", t=128), e='act')
                for d in range(2):
                    ci = v2 * 2 + d
                    col = d * 32 + hv
                    kb.tt(bv[ci][:], vtok[v2][:], nb_all[:, :, col:col + 1].broadcast_to([128, NCH, 128]), ALU.mult)
                    kb.ts(bv[ci][:], bv[ci][:], -1.0, None, op0=ALU.mult, e='pool')
                    kb.memset(Sf[ci][:], 0.0)
                    kb.memset(Sb[ci][:], 0.0)
                    chains.append((ci, v2, hv, d, col))
            done = {}
            for s_ in range(NCH):
                raw = {}
                for d, n in ((0, s_), (1, NCH - 1 - s_)):
                    tk = slice(n * 128, (n + 1) * 128)
                    ps = nps()
                    kb.mm(ps[:, 0:128], kh[:, tk], kh[:, tk])
                    kb.mm(ps[:, 128:256], kh[:, tk], qh[:, tk])
                    kb.tt(KKs[d][:], ps[:, 0:128], (MSL if d == 0 else MSU)[:], ALU.mult)
                    kb.copy(QKr[d][:], ps[:, 128:256], e='dve')
                    raw[d] = n
                for (ci, v2, hv, d, col) in chains:
                    n = raw[d]
                    tk = slice(n * 128, (n + 1) * 128)
                    r = rot[0] % R
                    rot[0] += 1
                    li_ = 127 if d == 0 else 0
                    gcol = gc_all[:, n, col:col + 1]
                    ngcol = ngc_all[:, n, col:col + 1]
                    nbcol = nb_all[:, n, col:col + 1]
                    kb.copy(gB[r][:], g_all[:, n, col:col + 1].broadcast_to([128, 128]), e='dve')
                    pg = nps()
                    kb.mm(pg[:, 0:128], gB[r][:], (UT if d == 0 else LT)[:])
                    kb.copy(gR[r][:], pg[:, 0:128], e='dve')
                    kb.ts(GT[r][:], gR[r][:], ngcol, 0.0, op0=ALU.add, op1=ALU.min)
                    kb.act(GT[r][:], GT[r][:], AF.Exp)
                    kb.ts(GG[r][:], gR[r][:], gcol, 0.0, op0=ALU.subtract, op1=ALU.max)
                    kb.act(GG[r][:], GG[r][:], AF.Exp, scale=-1.0)
                    kb.act(Er[r][:], gR[r][:], AF.Exp)
                    kb.copy(sc[r][:, 0:1], gR[r][:, li_:li_ + 1])
                    kb.act(sc[r][:, 1:2], sc[r][:, 0:1], AF.Exp)
                    kb.act(sc[r][:, 2:3], gcol, AF.Exp, bias=sc[r][:, 0:1], scale=-1.0)
                    kb.tt(GT[r][:], GT[r][:], (UT if d == 0 else LT)[:], ALU.mult)
                    kb.tt(QKT[r][:], QKr[d][:], GT[r][:], ALU.mult)
                    kb.stt(Ya[r][:], KKs[d][:], nbcol, GG[r][:], ALU.mult, ALU.mult)
                    px = nps()
                    kb.tr(px[:, 0:128], Ya[r][:], cx.ident_f[:])
                    kb.copy(Xa[r][:], px[:, 0:128], e='dve')
                    kb.tt(Pa[r][:], Xa[r][:], cx.ident_f[:], ALU.add)
                    X, Y, Xn, Yn = Xa[r], Ya[r], Xb_[r], Yb_[r]
                    for k_ in range(1, 7):
                        pyy = nps()
                        kb.mm(pyy[:, 0:128], X[:], Y[:])
                        if k_ < 6:
                            pxx = nps()
                            kb.mm(pxx[:, 0:128], Y[:], X[:])
                        kb.copy(Yn[:], pyy[:, 0:128], e='act')
                        if k_ < 6:
                            kb.copy(Xn[:], pxx[:, 0:128], e='dve')
                        pp = nps()
                        kb.mm(pp[:, 0:128], Yn[:], Pa[r][:])
                        kb.tt(Pa[r][:], Pa[r][:], pp[:, 0:128], ALU.add)
                        X, Y, Xn, Yn = Xn, Yn, X, Y
                    kb.copy(MT[r][:], Pa[r][:], e='act')
                    kb.tt(kdT[r][:], kh[:, tk], Er[r][:], ALU.mult)
                    kb.tt(qdT[r][:], qh[:, tk], Er[r][:], ALU.mult)
                    kb.ts(kst[r][:], ktok[:, n, :], sc[r][:, 2:3], None, op0=ALU.mult)
                    p1 = nps()
                    kb.mm(p1[:, 0:128], kdT[r][:], Sb[ci][:])
                    kb.stt(Xv[r][:], p1[:, 0:128], nbcol, bv[ci][:, n, :], ALU.mult, ALU.add)
                    p2 = nps()
                    kb.mm(p2[:, 0:128], MT[r][:], Xv[r][:])
                    kb.copy(vnb[r][:], p2[:, 0:128], e='act')
                    p3 = nps()
                    kb.mm(p3[:, 0:128], qdT[r][:], Sb[ci][:], start=True, stop=False)
                    kb.mm(p3[:, 0:128], QKT[r][:], vnb[r][:], start=False, stop=True)
                    key = (hv, n)
                    i2 = rot[0] % 2
                    if key not in done:
                        done[key] = 1
                        kb.copy(of[i2][:], p3[:, 0:128], e='act')
                        kb.dma('sp', o1[tk, hv * 128:(hv + 1) * 128], of[i2][:], w=[o1])
                    else:
                        kb.dma('sp', op_[i2][:], o1[tk, hv * 128:(hv + 1) * 128])
                        kb.dma('sp', zt[i2][:], zz[tk, hv * 128:(hv + 1) * 128])
                        kb.tt(of[i2][:], p3[:, 0:128], op_[i2][:], ALU.add)
                        kb.act(junk[:], of[i2][:], AF.Square, accum=sc[r][:, 3:4])
                        kb.act(sc[r][:, 4:5], sc[r][:, 3:4], AF.Sqrt, bias=cx.eps_t[:, 0:1], scale=1.0 / 128)
                        kb.op('dve', lambda E, r=r: E.reciprocal(sc[r][:, 5:6], sc[r][:, 4:5]), w=[sc[r]], r=[sc[r]])
                        kb.stt(of[i2][:], of[i2][:], sc[r][:, 5:6], hng[:], ALU.mult, ALU.mult)
                        kb.act(zs[i2][:], zt[i2][:], AF.Silu)
                        kb.tt(obf[i2][:], of[i2][:], zs[i2][:], ALU.mult)
                        kb.dma('sp', og[tk, hv * 128:(hv + 1) * 128], obf[i2][:], w=[og])
                    p4 = nps()
                    kb.mm(p4[:, 0:128], kst[r][:], vnb[r][:])
                    kb.stt(Sf[ci][:], Sf[ci][:], sc[r][:, 1:2], p4[:, 0:128], ALU.mult, ALU.add)
                    kb.copy(Sb[ci][:], Sf[ci][:], e='act')
    proj_out(kb, cx, 'gdn_w_out', og[:, 0:4096], 4096)


MIXERS[1] = gdn_mixer


SMALL_INPUTS = {
    'gla_w_gate_up': [2, 16, 1024], 'gla_b_gate': [2, 1024], 'gla_head_norm': [1, 512],
    'gdn_conv': [5, 8192], 'gdn_a_log': [2, 32], 'gdn_dt_bias': [2, 32], 'gdn_head_norm': [1, 128],
    'diff_q_norm': [1, 64], 'diff_k_norm': [1, 64], 'diff_lambda': [4, 64], 'diff_subln': [1, 128],
    'swa_q_norm': [1, 128], 'swa_k_norm': [1, 128], 'swa_sink': [1, 16],
}


def kernel(**inputs):
    x = np.asarray(inputs['x'])
    ncores = NCORES
    cfg = dict(S=S, layers=[0, 1, 2, 3], ncores=ncores, small_inputs=SMALL_INPUTS)
    outs = run(cfg, inputs, [x[b] for b in range(ncores)])
    return np.stack(outs, axis=0).astype(np.float32)
```

```python
import numpy as np
from contextlib import ExitStack
import concourse.bass as bass
import concourse.mybir as mybir
from concourse.bass_utils import run_bass_kernel_spmd

F32 = mybir.dt.float32
BF16 = mybir.dt.bfloat16
I32 = mybir.dt.int32
U32 = mybir.dt.uint32
AF = mybir.ActivationFunctionType
ALU = mybir.AluOpType
AX = mybir.AxisListType

S = 4096
D = 2048
NCORES = 8
NDS = 40
SAME_SYNC = True
SQ = 'pool'


def _key(x):
    if isinstance(x, str):
        return x
    return getattr(x, 'tensor', x).name


class KB:
    def __init__(self, nc, st):
        self.nc = nc
        self.E = {'pe': nc.tensor, 'dve': nc.vector, 'act': nc.scalar, 'pool': nc.gpsimd, 'sp': nc.sync}
        self.sem = {e: st.enter_context(nc.semaphore('sm_' + e)) for e in self.E}
        self.cnt = {e: 0 for e in self.E}
        self.seen = {e: {} for e in self.E}
        self.lastw = {}
        self.readers = {}
        self.dsem = [st.enter_context(nc.semaphore('sd%d' % i)) for i in range(NDS)]
        self.dtarget = [0] * NDS
        self.dnext = 0
        self.ninst = 0

    def _semof(self, sk):
        return self.sem[sk] if isinstance(sk, str) else self.dsem[sk[1]]

    def wait(self, e, tok):
        sk, val = tok
        if val <= 0:
            return
        if sk == e and not (SAME_SYNC and e in ('dve', 'act', 'pool')):
            return
        if self.seen[e].get(sk, 0) >= val:
            return
        self.E[e].wait_ge(self._semof(sk), val)
        self.seen[e][sk] = val

    def _deps(self, e, w, r):
        for k in r:
            k = _key(k)
            if k in self.lastw:
                self.wait(e, self.lastw[k])
        for k in w:
            k = _key(k)
            if k in self.lastw:
                self.wait(e, self.lastw[k])
            for sk, val in self.readers.get(k, {}).items():
                self.wait(e, (sk, val))

    def _commit(self, tok, w, r):
        for k in r:
            k = _key(k)
            d = self.readers.setdefault(k, {})
            d[tok[0]] = max(d.get(tok[0], 0), tok[1])
        for k in w:
            k = _key(k)
            self.lastw[k] = tok
            self.readers[k] = {}

    def op(self, e, fn, w=(), r=()):
        self._deps(e, w, r)
        inst = fn(self.E[e])
        self.cnt[e] += 1
        inst.then_inc(self.sem[e], 1)
        self._commit((e, self.cnt[e]), w, r)
        self.ninst += 1
        return inst

    def dma(self, q, out, in_, w=None, r=None, fn=None, **kw):
        w = [out] if w is None else w
        r = [in_] if r is None else r
        i = self.dnext
        self.dnext = (i + 1) % NDS
        self.wait(q, (('d', i), self.dtarget[i]))
        self._deps(q, w, r)
        if fn is None:
            inst = self.E[q].dma_start(out=out, in_=in_, **kw)
        else:
            inst = fn(self.E[q])
        self.dtarget[i] += 16
        inst.then_inc(self.dsem[i], 16)
        self._commit((('d', i), self.dtarget[i]), w, r)
        self.ninst += 1

    def barrier(self):
        for e in self.E:
            for e2 in self.E:
                if e2 != e:
                    self.wait(e, (e2, self.cnt[e2]))
            for i in range(NDS):
                self.wait(e, (('d', i), self.dtarget[i]))

    def finish(self):
        for i in range(NDS):
            self.wait('sp', (('d', i), self.dtarget[i]))
        for e in self.E:
            if e != 'sp':
                self.wait('sp', (e, self.cnt[e]))

    def mm(self, out, lhsT, rhs, start=True, stop=True, **kw):
        return self.op('pe', lambda E: E.matmul(out, lhsT, rhs, start=start, stop=stop, **kw), w=[out], r=[lhsT, rhs])

    def tr(self, out, in_, ident):
        return self.op('pe', lambda E: E.transpose(out, in_, ident), w=[out], r=[in_, ident])

    def act(self, out, in_, func, bias=None, scale=None, accum=None, e='act'):
        kw = {}
        r = [in_]
        w = [out]
        if bias is not None:
            kw['bias'] = bias
            if not isinstance(bias, (int, float)):
                r.append(bias)
        if scale is not None:
            kw['scale'] = scale
            if not isinstance(scale, (int, float)):
                r.append(scale)
        if accum is not None:
            kw['accum_out'] = accum
            w.append(accum)
        return self.op('act', lambda E: E.activation(out, in_, func, **kw), w=w, r=r)

    def tt(self, out, a, b, op, e='dve'):
        return self.op(e, lambda E: E.tensor_tensor(out, a, b, op), w=[out], r=[a, b])

    def ts(self, out, a, s1, s2=None, op0=ALU.mult, op1=None, e='dve', accum=None):
        r = [a] + [s for s in (s1, s2) if s is not None and not isinstance(s, (int, float))]
        w = [out] + ([accum] if accum is not None else [])
        kw = {}
        if op1 is not None:
            kw['op1'] = op1
        if accum is not None:
            kw['accum_out'] = accum
        return self.op(e, lambda E: E.tensor_scalar(out, a, s1, s2, op0, **kw), w=w, r=r)

    def stt(self, out, a, sc, b, op0, op1):
        r = [a, b] + ([] if isinstance(sc, (int, float)) else [sc])
        return self.op('dve', lambda E: E.scalar_tensor_tensor(out, a, sc, b, op0, op1), w=[out], r=r)

    def copy(self, out, in_, e='dve'):
        if e == 'act':
            return self.op('act', lambda E: E.copy(out, in_), w=[out], r=[in_])
        return self.op(e, lambda E: E.tensor_copy(out, in_), w=[out], r=[in_])

    def memset(self, out, val, e='dve'):
        return self.op(e, lambda E: E.memset(out, val), w=[out], r=[])


WCOLS = 2048


class WTable:
    def __init__(self):
        self.ents = {}
        self.R = 0

    def add(self, name, K, N):
        n = K * N
        assert n % WCOLS == 0
        rows = n // WCOLS
        self.ents[name] = (self.R, rows, K, N)
        self.R += rows

    def host_shards(self, arrays):
        out = np.empty((self.R, WCOLS), np.float32)
        for name, (ro, rows, K, N) in self.ents.items():
            out[ro:ro + rows] = np.ascontiguousarray(arrays[name]).reshape(rows, WCOLS)
        return out

    def ap(self, gathered_flat, name, k0, kk, n0, nn):
        ro, rows, K, N = self.ents[name]
        base = ro * WCOLS + k0 * N
        return gathered_flat[base: base + kk * N].rearrange("(k n) -> k n", n=N)[:, n0:n0 + nn]


def build_wtable(layers):
    wt = WTable()
    if 0 in layers:
        wt.add('gla_w_in', D, 6176)
        wt.add('gla_w_out', D, D)
    if 1 in layers:
        wt.add('gdn_w_in', D, 12416)
        wt.add('gdn_w_out', 4096, D)
    if 2 in layers:
        wt.add('diff_w_in', D, 6144)
        wt.add('diff_w_out', D, D)
    if 3 in layers:
        wt.add('swa_w_in', D, 3072)
        wt.add('swa_w_out', D, D)
    return wt


class Ctx:
    pass


_UNIQ = [0]


_KB = [None]


def mk_sb(nc, st):
    st.callback(lambda: _KB[0].barrier())

    def sb(name, shape, dt):
        _UNIQ[0] += 1
        return st.enter_context(nc.sbuf_tensor('%s_u%d' % (name, _UNIQ[0]), shape, dt))
    return sb


def make_consts():
    ident = np.eye(128, dtype=np.float32)
    masku = np.triu(np.ones((128, 128), np.float32))
    maskl = np.tril(np.ones((128, 128), np.float32))
    d = {'c_ident': ident, 'c_masku': masku, 'c_maskl': maskl}
    d.update(attn_consts())
    return d


def rows_bcast(ap_row, n=128):
    return ap_row.broadcast_to([n, ap_row.shape[-1]])


def load_consts(kb, cx, st):
    nc = cx.nc
    cx.ident_f = st.enter_context(nc.sbuf_tensor('ident_f', [128, 128], F32))
    cx.ident_b = st.enter_context(nc.sbuf_tensor('ident_b', [128, 128], BF16))
    kb.dma('sp', cx.ident_f[:], cx.dram['c_ident'][:, :])
    kb.copy(cx.ident_b[:], cx.ident_f[:])
    cx.ps = [st.enter_context(nc.psum_tensor('ps%d' % i, [128, 512], F32)) for i in range(3)]
    cx.acc = [st.enter_context(nc.psum_tensor('acc%d' % i, [128, 512], F32)) for i in range(4)]
    cx.pb = [st.enter_context(nc.psum_tensor('pb%d' % i, [128, 1024], BF16)) for i in range(1)]
    cx.psi = 0
    cx.pbi = 0


def next_ps(cx):
    t = cx.ps[cx.psi % len(cx.ps)]
    cx.psi += 1
    return t


def next_pb(cx):
    t = cx.pb[cx.pbi % len(cx.pb)]
    cx.pbi += 1
    return t


def norm_tile(kb, cx, bufs, h_rows, gain_bc, i):
    ht = bufs['h'][i % 2]
    xn = bufs['xn'][i % 2]
    ss = bufs['ss'][i % 2]
    kb.dma('sp', ht[:], h_rows, r=['h'])
    kb.act(bufs['junk'][:], ht[:], AF.Square, accum=ss[:, 0:1])
    kb.act(ss[:, 1:2], ss[:, 0:1], AF.Sqrt, bias=cx.eps_t[:, 0:1], scale=1.0 / D)
    kb.op('dve', lambda E: E.reciprocal(ss[:, 2:3], ss[:, 1:2]), w=[ss], r=[ss])
    kb.stt(xn[:], ht[:], ss[:, 2:3], gain_bc[:], ALU.mult, ALU.mult)
    return xn


def transpose_to(kb, cx, src_bf, dstT, tok0, ntok=128, nchunks=16):
    for c0 in range(0, nchunks, 8):
        pb = next_pb(cx)
        n = min(8, nchunks - c0)
        for c in range(n):
            kb.tr(pb[:, c * 128:(c + 1) * 128], src_bf[:, (c0 + c) * 128:(c0 + c + 1) * 128], cx.ident_b[:])
        kb.copy(dstT[:, c0:c0 + n, tok0:tok0 + 128],
                pb[:, 0:n * 128].rearrange("p (c t) -> p c t", t=128), e='act' if (c0 // 8) % 2 else 'dve')


def moe_layer(kb, cx, li):
    nc = cx.nc
    S = cx.S
    NE, DFF = 16, 1024
    CAP = 2 * S // NE
    NJ = CAP // 128
    h = cx.h
    hn = cx.hn_bf
    with ExitStack() as st:
        sb = mk_sb(nc, st)
        bufs = {'h': [sb('m_h%d' % i, [128, D], F32) for i in range(2)],
                'xn': [sb('m_xn%d' % i, [128, D], BF16) for i in range(2)],
                'ss': [sb('m_ss%d' % i, [128, 4], F32) for i in range(2)],
                'junk': sb('m_junk', [128, D], BF16)}
        gain = sb('m_gain', [128, D], F32)
        rt_f = sb('m_rtf', [128, 16, NE], F32)
        rt_b = sb('m_rtb', [128, 16, NE], BF16)
        hnT = sb('m_hnT', [128, 16, 128], BF16)
        affT = sb('m_affT', [NE, S], F32)
        lg = [sb('m_lg%d' % i, [128, NE + 4], F32) for i in range(2)]
        kb.dma('sp', gain[:], rows_bcast(cx.dram['norm_ffn'][li:li + 1, :]))
        kb.dma('sp', rt_f[:], cx.dram['moe_router'][li].rearrange("(c p) e -> p c e", p=128))
        kb.copy(rt_b[:], rt_f[:])
        for t in range(S // 128):
            xn = norm_tile(kb, cx, bufs, h[t * 128:(t + 1) * 128, :], gain, t)
            kb.dma(SQ, hn[t * 128:(t + 1) * 128, :], xn[:], w=['hn'])
            transpose_to(kb, cx, xn, hnT, 0)
            ps = next_ps(cx)
            for c in range(16):
                kb.mm(ps[:, 0:NE], hnT[:, c, :], rt_b[:, c, :], start=(c == 0), stop=(c == 15))
            l = lg[t % 2]
            kb.op('dve', lambda E: E.reduce_max(l[:, NE:NE + 1], ps[:, 0:NE], AX.X), w=[l], r=[ps])
            kb.ts(l[:, NE + 1:NE + 2], l[:, NE:NE + 1], -1.0, None, op0=ALU.mult)
            kb.act(l[:, 0:NE], ps[:, 0:NE], AF.Exp, bias=l[:, NE + 1:NE + 2], scale=1.0, accum=l[:, NE + 2:NE + 3])
            kb.op('dve', lambda E: E.reciprocal(l[:, NE + 3:NE + 4], l[:, NE + 2:NE + 3]), w=[l], r=[l])
            kb.ts(l[:, 0:NE], l[:, 0:NE], l[:, NE + 3:NE + 4], None, op0=ALU.mult)
            ps2 = next_ps(cx)
            kb.tr(ps2[0:NE, 0:128], l[:, 0:NE], cx.ident_f[:])
            kb.copy(affT[:, t * 128:(t + 1) * 128], ps2[0:NE, 0:128])
        gate = sb('m_gate', [NE, CAP], F32)
        idx = sb('m_idx', [NE, CAP], U32)
        for r8 in range(CAP // 8):
            g8 = gate[:, r8 * 8:(r8 + 1) * 8]
            kb.op('dve', lambda E: E.max(g8, affT[:]), w=[gate], r=[affT])
            kb.op('dve', lambda E: E.max_index(idx[:, r8 * 8:(r8 + 1) * 8], g8, affT[:]), w=[idx], r=[gate, affT])
            kb.op('dve', lambda E: E.match_replace(affT[:], g8, affT[:], -1.0), w=[affT], r=[gate, affT])
        kb.dma('sp', cx.sc_gate[:, :], gate[:], w=['sc_gate'])
        kb.dma('sp', cx.sc_idx[:, :], idx[:], w=['sc_idx'])
        gateT = sb('m_gateT', [128, NE * NJ], F32)
        idxT = sb('m_idxT', [128, NE * NJ], U32)
        for e in range(NE):
            kb.dma('sp', gateT[:, e * NJ:(e + 1) * NJ], cx.sc_gate[e].rearrange("(j p) -> p j", p=128),
                   r=['sc_gate'], allow_slow_non_contiguous=True)
            kb.dma('sp', idxT[:, e * NJ:(e + 1) * NJ], cx.sc_idx[e].rearrange("(j p) -> p j", p=128),
                   r=['sc_idx'], allow_slow_non_contiguous=True)
        xs = [sb('m_xs%d' % i, [128, D], BF16) for i in range(2)]
        xsT = sb('m_xsT', [128, 16, CAP], BF16)
        wa = [sb('m_wa%d' % i, [128, 16, 256], BF16) for i in range(2)]
        wb = [sb('m_wb%d' % i, [128, 16, 256], BF16) for i in range(2)]
        w2t = [sb('m_w2%d' % i, [128, 8, 512], BF16) for i in range(2)]
        gT = sb('m_gT', [128, 8, CAP], BF16)
        tmp = [sb('m_tmp%d' % i, [128, 512], F32) for i in range(2)]
        ybuf = [sb('m_y%d' % i, [128, D], F32) for i in range(NJ)]
        W1 = cx.wmoe[('w1', li)]
        W3 = cx.wmoe[('w3', li)]
        W2 = cx.wmoe[('w2', li)]
        nld = 0
        for e in range(NE):
            for j in range(NJ):
                x_ = xs[j % 2]
                col = e * NJ + j
                kb.dma('pool', x_[:], hn[:, :], r=['hn', idxT], w=[x_],
                       fn=lambda E, x_=x_, col=col: E.indirect_dma_start(
                           out=x_[:], out_offset=None, in_=hn[:, :],
                           in_offset=bass.IndirectOffsetOnAxis(ap=idxT[:, col:col + 1], axis=0)))
                transpose_to(kb, cx, x_, xsT, j * 128)
            for fg in range(4):
                a_ = wa[nld % 2]
                b_ = wb[nld % 2]
                nld += 1
                kb.dma('sp', a_[:], cx.Wtile('moe_w1_%d' % li, e * D, 16, fg * 256, 256), r=['wg'])
                kb.dma('sp', b_[:], cx.Wtile('moe_w3_%d' % li, e * D, 16, fg * 256, 256), r=['wg'])
                for f in range(2):
                    fc = fg * 2 + f
                    pa = next_ps(cx)
                    pbm = next_ps(cx)
                    for c in range(16):
                        kb.mm(pa[:, 0:CAP], a_[:, c, f * 128:(f + 1) * 128], xsT[:, c, :], start=(c == 0), stop=(c == 15))
                    for c in range(16):
                        kb.mm(pbm[:, 0:CAP], b_[:, c, f * 128:(f + 1) * 128], xsT[:, c, :], start=(c == 0), stop=(c == 15))
                    tm = tmp[fc % 2]
                    kb.act(tm[:, 0:CAP], pa[:, 0:CAP], AF.Silu)
                    kb.tt(gT[:, fc, :], tm[:, 0:CAP], pbm[:, 0:CAP], ALU.mult)
            for dc in range(4):
                w_ = w2t[dc % 2]
                kb.dma('sp', w_[:], cx.Wtile('moe_w2_%d' % li, e * 1024, 8, dc * 512, 512), r=['wg'])
                for j in range(NJ):
                    py = next_ps(cx)
                    for c in range(8):
                        kb.mm(py[:, :], gT[:, c, j * 128:(j + 1) * 128], w_[:, c, :], start=(c == 0), stop=(c == 7))
                    kb.ts(ybuf[j][:, dc * 512:(dc + 1) * 512], py[:, :], gateT[:, e * NJ + j:e * NJ + j + 1], None, op0=ALU.mult)
            for j in range(NJ):
                col = e * NJ + j
                yb = ybuf[j]
                kb.dma('pool', h[:, :], yb[:], w=['h'], r=[yb, idxT],
                       fn=lambda E, yb=yb, col=col: E.indirect_dma_start(
                           out=h[:, :], out_offset=bass.IndirectOffsetOnAxis(ap=idxT[:, col:col + 1], axis=0),
                           in_=yb[:], in_offset=None, compute_op=ALU.add))


def prologue_weights(kb, cx):
    nc = cx.nc
    wsh = cx.dram['wsh']
    with ExitStack() as st:
        f = [st.enter_context(nc.sbuf_tensor('pw_f%d' % i, [128, 4096], F32)) for i in range(2)]
        b = [st.enter_context(nc.sbuf_tensor('pw_b%d' % i, [128, 4096], BF16)) for i in range(2)]
        i = 0
        for name, (ro, rows, K_, N_) in cx.wt.ents.items():
            per = rows * WCOLS // 128
            src = wsh[ro:ro + rows, :].rearrange("r c -> (r c)").rearrange("(p n) -> p n", p=128)
            dst = cx.wbf[name].rearrange("r c -> (r c)").rearrange("(p n) -> p n", p=128)
            for c0 in range(0, per, 4096):
                n = min(4096, per - c0)
                kb.dma('sp', f[i % 2][:, 0:n], src[:, c0:c0 + n])
                kb.copy(b[i % 2][:, 0:n], f[i % 2][:, 0:n], e=('dve' if i % 2 == 0 else 'pool'))
                kb.dma(SQ, dst[:, c0:c0 + n], b[i % 2][:, 0:n], w=['wg'])
                i += 1
        kb.barrier()


def build_program(cfg):
    nc = bass.Bass("TRN2", target_bir_lowering=False)
    cx = Ctx()
    cx.nc = nc
    cx.S = cfg['S']
    cx.ncores = cfg.get('ncores', NCORES)
    cx.debug_og = cfg.get('debug_og', False)
    cx.stop = cfg.get('stop', 0)
    Sx = cx.S
    layers = cfg['layers']
    wt = WTable()
    for li in layers:
        if cfg.get('mixers', True):
            if li == 0:
                wt.add('gla_w_in', D, 6176); wt.add('gla_w_out', D, D)
            if li == 1:
                wt.add('gdn_w_in', D, 12416); wt.add('gdn_w_out', 4096, D)
            if li == 2:
                wt.add('diff_w_in', D, 6144); wt.add('diff_w_out', D, D)
            if li == 3:
                wt.add('swa_w_in', D, 3072); wt.add('swa_w_out', D, D)
        if cfg.get('moe', True):
            wt.add('moe_w1_%d' % li, 16 * D, 1024)
            wt.add('moe_w3_%d' % li, 16 * D, 1024)
            wt.add('moe_w2_%d' % li, 16 * 1024, D)
    cx.wt = wt
    dram = {}

    def din(name, shape, dt=F32):
        dram[name] = nc.dram_tensor(name, list(shape), dt, kind="ExternalInput").ap()

    din('x', [Sx, D])
    din('wsh', [wt.R, WCOLS])
    din('c_ident', [128, 128]); din('c_masku', [128, 128]); din('c_maskl', [128, 128]); din('c_bk', [128, 384]); din('c_mneg', [128, 384])
    cx.scr = {}
    din('norm_mix', [4, D]); din('norm_ffn', [4, D]); din('moe_router', [4, D, 16]); din('rel_bias', [32, 16])
    for name, shape in cfg.get('small_inputs', {}).items():
        din(name, shape)
    out = nc.dram_tensor('out', [Sx, D], F32, kind="ExternalOutput").ap()
    cx.dram = dram
    cx.h = out
    cx.hn_bf = nc.dram_tensor('hn_bf', [Sx, D], BF16, kind="Internal").ap()
    cx.sc_gate = nc.dram_tensor('sc_gate', [16, 2 * Sx // 16], F32, kind="Internal").ap()
    cx.sc_idx = nc.dram_tensor('sc_idx', [16, 2 * Sx // 16], U32, kind="Internal").ap()
    cx.wbf = {}
    for name, (ro, rows, K_, N_) in wt.ents.items():
        cx.wbf[name] = nc.dram_tensor('wbf_' + name, [rows, WCOLS], BF16, kind="Internal").ap()

    def Wap(name, k0, kk, n0, nn):
        ro, rows, K_, N_ = wt.ents[name]
        flat = cx.wbf[name].rearrange("r c -> (r c)")
        return flat[k0 * N_: (k0 + kk) * N_].rearrange("(k n) -> k n", n=N_)[:, n0:n0 + nn]
    cx.W = Wap

    def Wtile(name, k0, KC, n0, nn):
        ro, rows, K_, N_ = wt.ents[name]
        flat = cx.wbf[name].rearrange("r c -> (r c)")
        return flat[k0 * N_: (k0 + KC * 128) * N_].rearrange("(c p n) -> p c n", p=128, n=N_)[:, :, n0:n0 + nn]
    cx.Wtile = Wtile
    cx.wmoe = {}
    for li in layers:
        cx.wmoe[('w1', li)] = (lambda e, k0, kk, n0, nn, li=li: cx.W('moe_w1_%d' % li, e * D + k0, kk, n0, nn))
        cx.wmoe[('w3', li)] = (lambda e, k0, kk, n0, nn, li=li: cx.W('moe_w3_%d' % li, e * D + k0, kk, n0, nn))
        cx.wmoe[('w2', li)] = (lambda e, k0, kk, n0, nn, li=li: cx.W('moe_w2_%d' % li, e * 1024 + k0, kk, n0, nn))

    with ExitStack() as st:
        kb = KB(nc, st)
        cx.kb = kb
        _KB[0] = kb
        load_consts(kb, cx, st)
        cx.eps_t = st.enter_context(nc.sbuf_tensor('eps_t', [128, 1], F32))
        kb.memset(cx.eps_t[:], 1e-6)
        prologue_weights(kb, cx)
        with ExitStack() as st2:
            tb = [st2.enter_context(nc.sbuf_tensor('cp%d' % i, [128, D], F32)) for i in range(2)]
            for t in range(Sx // 128):
                kb.dma('sp', tb[t % 2][:], dram['x'][t * 128:(t + 1) * 128, :])
                kb.dma(SQ, cx.h[t * 128:(t + 1) * 128, :], tb[t % 2][:], w=['h'])
            kb.barrier()
        for li in layers:
            if cfg.get('mixers', True):
                MIXERS[li](kb, cx, li)
            if cfg.get('moe', True):
                moe_layer(kb, cx, li)
        kb.finish()
    cx.ninst = kb.ninst
    return nc, cx


MIXERS = {}


def host_weights(inputs, cx):
    arrs = {}
    for name in cx.wt.ents:
        if name.startswith('moe_'):
            kind, li = name[4:6], int(name.split('_')[-1])
            a = inputs['moe_' + kind][li]
            arrs[name] = a.reshape(-1, a.shape[-1])
        else:
            a = inputs[name][0]
            arrs[name] = a
    return cx.wt.host_shards(arrs)


def run(cfg, inputs, xs):
    nc, cx = build_program(cfg)
    shards = host_weights(inputs, cx)
    consts = make_consts()
    in_maps = []
    for c in range(cx.ncores):
        m = {'x': np.ascontiguousarray(xs[c]), 'wsh': shards}
        m.update(consts)
        for k in ('norm_mix', 'norm_ffn', 'moe_router', 'rel_bias'):
            m[k] = np.ascontiguousarray(inputs[k])
        for name in cfg.get('small_inputs', {}):
            m[name] = np.ascontiguousarray(inputs[name]).reshape(cfg['small_inputs'][name])
        in_maps.append(m)
    res = run_bass_kernel_spmd(nc, in_maps, core_ids=list(range(cx.ncores)))
    return [r['out'] for r in res.results]


def proj_in(kb, cx, li, wname, groups):
    nc = cx.nc
    S = cx.S
    TT = 512
    with ExitStack() as st:
        sb = mk_sb(nc, st)
        bufs = {'h': [sb('p_h%d' % i, [128, D], F32) for i in range(2)],
                'xn': [sb('p_xn%d' % i, [128, D], BF16) for i in range(2)],
                'ss': [sb('p_ss%d' % i, [128, 4], F32) for i in range(2)],
                'junk': sb('p_junk', [128, D], BF16)}
        gain = sb('p_gain', [128, D], F32)
        hnT = sb('p_hnT', [128, 16, TT], BF16)
        wts = [sb('p_w%d' % i, [128, 16, 512], BF16) for i in range(2)]
        stg_b = [sb('p_sb%d' % i, [128, 512], BF16) for i in range(3)]
        stg_f = [sb('p_sf%d' % i, [128, 512], F32) for i in range(2)]
        kb.dma('sp', gain[:], rows_bcast(cx.dram['norm_mix'][li:li + 1, :]))
        nw = 0
        ns = 0
        for s0 in range(0, S, TT):
            for j in range(TT // 128):
                xn = norm_tile(kb, cx, bufs, cx.h[s0 + j * 128:s0 + (j + 1) * 128, :], gain, j)
                transpose_to(kb, cx, xn, hnT, j * 128)
            for (col0, ncols, mode, dst, scale) in groups:
                for c0 in range(0, ncols, 512):
                    cw = min(512, ncols - c0)
                    wt = wts[nw % 2]
                    nw += 1
                    kb.dma('sp', wt[:, :, 0:cw], cx.Wtile(wname, 0, 16, col0 + c0, cw), r=['wg'])
                    isb = (dst.dtype == BF16)
                    if mode == 'T':
                        for f0 in range(0, cw, 128):
                            fw = min(128, cw - f0)
                            ps = next_ps(cx)
                            for c in range(16):
                                kb.mm(ps[0:fw, :], wt[:, c, f0:f0 + fw], hnT[:, c, :], start=(c == 0), stop=(c == 15))
                            sg = (stg_b[ns % 3] if isb else stg_f[ns % 2])
                            ns += 1
                            kb.act(sg[0:fw, :], ps[0:fw, :], AF.Copy, scale=float(scale))
                            kb.dma(SQ, dst[c0 + f0:c0 + f0 + fw, s0:s0 + TT], sg[0:fw, :], w=[dst])
                    else:
                        for j in range(TT // 128):
                            ps = next_ps(cx)
                            for c in range(16):
                                kb.mm(ps[:, 0:cw], hnT[:, c, j * 128:(j + 1) * 128], wt[:, c, 0:cw], start=(c == 0), stop=(c == 15))
                            sg = (stg_b[ns % 3] if isb else stg_f[ns % 2])
                            ns += 1
                            if ns % 2:
                                kb.act(sg[:, 0:cw], ps[:, 0:cw], AF.Copy, scale=float(scale))
                            else:
                                kb.ts(sg[:, 0:cw], ps[:, 0:cw], float(scale), None, op0=ALU.mult)
                            kb.dma(SQ, dst[s0 + j * 128:s0 + (j + 1) * 128, c0:c0 + cw], sg[:, 0:cw], w=[dst])


def proj_out(kb, cx, wname, og, KD):
    nc = cx.nc
    S = cx.S
    KC = KD // 128
    TT = 512
    if getattr(cx, 'debug_og', False):
        with ExitStack() as st:
            sb = mk_sb(nc, st)
            a = [sb('dbg_a%d' % i, [128, 2048], BF16) for i in range(2)]
            b = [sb('dbg_b%d' % i, [128, 2048], F32) for i in range(2)]
            for t in range(S // 128):
                kb.dma('sp', a[t % 2][:], og[t * 128:(t + 1) * 128, 0:2048])
                kb.copy(b[t % 2][:], a[t % 2][:])
                kb.dma(SQ, cx.h[t * 128:(t + 1) * 128, :], b[t % 2][:], w=['h'])
        return
    with ExitStack() as st:
        sb = mk_sb(nc, st)
        ogt = [sb('o_og%d' % i, [128, KD], BF16) for i in range(2)]
        ogT = sb('o_ogT', [128, KC, TT], BF16)
        wts = [sb('o_w%d' % i, [128, KC, 512], BF16) for i in range(2)]
        hb = [sb('o_h%d' % i, [128, D], F32) for i in range(4)]
        nw = 0
        for s0 in range(0, S, TT):
            for j in range(4):
                o_ = ogt[j % 2]
                kb.dma('sp', o_[:], og[s0 + j * 128:s0 + (j + 1) * 128, :])
                transpose_to(kb, cx, o_, ogT, j * 128, nchunks=KC)
                kb.dma('sp', hb[j][:], cx.h[s0 + j * 128:s0 + (j + 1) * 128, :], r=['h'])
            for dc in range(4):
                wt = wts[nw % 2]
                nw += 1
                for c in range(0, KC, 16):
                    kb.dma('sp', wt[:, c:c + 16, :], cx.Wtile(wname, c * 128, 16, dc * 512, 512), r=['wg'])
                for j in range(4):
                    ps = next_ps(cx)
                    for c in range(KC):
                        kb.mm(ps[:, :], ogT[:, c, j * 128:(j + 1) * 128], wt[:, c, :], start=(c == 0), stop=(c == KC - 1))
                    kb.tt(hb[j][:, dc * 512:(dc + 1) * 512], hb[j][:, dc * 512:(dc + 1) * 512], ps[:, :], ALU.add)
            for j in range(4):
                kb.dma(SQ, cx.h[s0 + j * 128:s0 + (j + 1) * 128, :], hb[j][:], w=['h'])


def dscr(cx, name, shape, dt):
    if name not in cx.scr:
        cx.scr[name] = cx.nc.dram_tensor(name, list(shape), dt, kind="Internal").ap()
    return cx.scr[name]


def gla_mixer(kb, cx, li):
    nc = cx.nc
    S = cx.S
    NCH = S // 128
    qT = dscr(cx, 'gla_qT', [1024, S], BF16)
    kT = dscr(cx, 'gla_kT', [1024, S], BF16)
    vv = dscr(cx, 'gla_v', [S, 2048], BF16)
    rr = dscr(cx, 'gla_r', [S, 2048], BF16)
    gl = dscr(cx, 'gla_glo', [32, S], BF16)
    o1 = dscr(cx, 'gla_o1', [S, 2048], F32)
    og = dscr(cx, 'mix_og', [S, 4096], BF16)
    proj_in(kb, cx, li, 'gla_w_in', [(0, 1024, 'T', qT, 1.0 / 16.0), (1024, 1024, 'T', kT, 1.0),
                                     (2048, 2048, 'N', vv, 1.0), (4096, 2048, 'N', rr, 1.0),
                                     (6144, 32, 'T', gl, 1.0)])
    with ExitStack() as st:
        sb = mk_sb(nc, st)
        glo1 = sb('g_glo', [16, S], BF16)
        wup_f = sb('g_wupf', [16, 2, 1024], F32)
        wup = sb('g_wup', [16, 2, 1024], BF16)
        negb = sb('g_negb', [128, 2, 8], F32)
        hng = sb('g_hng', [128, 512], F32)
        mask = [sb('g_mask%d' % d, [128, 128], F32) for d in range(2)]
        qh = sb('g_q', [128, 2, S], BF16)
        kh = sb('g_k', [128, 2, S], BF16)
        qd = sb('g_qd', [128, 2, S], BF16)
        ki = sb('g_ki', [128, 2, S], BF16)
        kstT = sb('g_kstT', [128, S], BF16)
        kst = sb('g_kst', [128, NCH, 256], BF16)
        CSx = sb('g_cs', [128, S + 1], F32)
        T1 = sb('g_t1', [128, S], F32)
        T2 = sb('g_t2', [128, S], F32)
        T3 = sb('g_t3', [128, S], F32)
        dec = sb('g_dec', [128, 2, NCH], F32)
        Sf = sb('g_Sf', [128, 2, 512], F32)
        Sb = sb('g_Sb', [128, 2, 512], BF16)
        vt = [sb('g_v%d' % i, [128, 512], BF16) for i in range(2)]
        rt = [sb('g_r%d' % i, [128, 512], BF16) for i in range(2)]
        AT = [sb('g_AT%d' % i, [128, 128], BF16) for i in range(2)]
        of = [sb('g_of%d' % i, [128, 512], F32) for i in range(2)]
        op_ = [sb('g_op%d' % i, [128, 512], F32) for i in range(2)]
        sr = [sb('g_sr%d' % i, [128, 512], F32) for i in range(2)]
        ob = [sb('g_ob%d' % i, [128, 512], BF16) for i in range(2)]
        ss = [sb('g_ss%d' % i, [128, 4], F32) for i in range(2)]
        junk = sb('g_junk', [128, 512], BF16)
        kb.dma('sp', wup_f[:], cx.dram['gla_w_gate_up'].rearrange("d r e -> r d e"))
        kb.copy(wup[:], wup_f[:])
        for d in range(2):
            kb.dma('sp', negb[:, d, :], cx.dram['gla_b_gate'][d].rearrange("(c p) -> p c", p=128), allow_slow_non_contiguous=True)
        kb.ts(negb[:], negb[:], -1.0, None, op0=ALU.mult)
        kb.dma('sp', hng[:], rows_bcast(cx.dram['gla_head_norm'][0:1, :]))
        kb.dma('sp', mask[0][:], cx.dram['c_masku'][:, :])
        kb.dma('sp', mask[1][:], cx.dram['c_maskl'][:, :])
        kb.memset(CSx[:, 0:1], 0.0)
        ch = lambda t: t.rearrange("p (n c) -> p n c", c=128)
        for hh in range(4):
            kb.dma('sp', qh[:], qT[hh * 256:(hh + 1) * 256, :].rearrange("(c p) s -> p c s", p=128))
            kb.dma('sp', kh[:], kT[hh * 256:(hh + 1) * 256, :].rearrange("(c p) s -> p c s", p=128))
            for d in range(2):
                kb.dma('sp', glo1[:], gl[d * 16:(d + 1) * 16, :])
                for fc in range(2):
                    f0 = hh * 256 + fc * 128
                    for b0 in range(0, S, 512):
                        ps = next_ps(cx)
                        kb.mm(ps[:, :], wup[:, d, f0:f0 + 128], glo1[:, b0:b0 + 512])
                        kb.act(T1[:, b0:b0 + 512], ps[:, :], AF.Exp, bias=negb[:, d, hh * 2 + fc:hh * 2 + fc + 1], scale=-1.0)
                    kb.act(T1[:], T1[:], AF.Ln, bias=1.0, scale=1.0)
                    kb.op('dve', lambda E: E.tensor_tensor_scan(CSx[:, 1:S + 1], T1[:], T1[:], 0.0, ALU.add, ALU.max),
                          w=[CSx], r=[T1])
                    if d == 0:
                        kb.tt(ch(T2[:]), ch(CSx[:, 1:S + 1]), ch(CSx[:, 0:S])[:, :, 0:1].broadcast_to([128, NCH, 128]), ALU.subtract)
                        li_ = 127
                    else:
                        kb.tt(ch(T2[:]), ch(CSx[:, 1:S + 1])[:, :, 127:128].broadcast_to([128, NCH, 128]), ch(CSx[:, 0:S]), ALU.subtract)
                        li_ = 0
                    kb.act(T3[:], T2[:], AF.Exp, scale=-1.0 / 16.0)
                    kb.act(CSx[:, 1:S + 1], T2[:], AF.Exp, scale=1.0 / 16.0)
                    kb.tt(qd[:, fc, :], qh[:, fc, :], T3[:], ALU.mult)
                    kb.tt(T1[:], kh[:, fc, :], CSx[:, 1:S + 1], ALU.mult)
                    kb.copy(ki[:, fc, :], T1[:], e='pool')
                    kb.tt(ch(kstT[:]), ch(T1[:]), ch(T3[:])[:, :, li_:li_ + 1].broadcast_to([128, NCH, 128]), ALU.mult)
                    kb.copy(dec[:, fc, :], ch(T3[:])[:, :, li_])
                    for n0 in range(0, NCH, 8):
                        pb = next_pb(cx)
                        for n in range(8):
                            kb.tr(pb[:, n * 128:(n + 1) * 128], kstT[:, (n0 + n) * 128:(n0 + n + 1) * 128], cx.ident_b[:])
                        kb.copy(kst[:, n0:n0 + 8, fc * 128:(fc + 1) * 128], pb[:, :].rearrange("p (n t) -> p n t", t=128), e='act')
                kb.memset(Sf[:], 0.0)
                kb.memset(Sb[:], 0.0)
                order = list(range(NCH)) if d == 0 else list(range(NCH - 1, -1, -1))
                for it, n in enumerate(order):
                    tk = slice(n * 128, (n + 1) * 128)
                    v_ = vt[it % 2]
                    kb.dma('sp', v_[:], vv[tk, hh * 512:(hh + 1) * 512])
                    ps1 = next_ps(cx)
                    for fc in range(2):
                        kb.mm(ps1[:, 0:128], ki[:, fc, tk], qd[:, fc, tk], start=(fc == 0), stop=(fc == 1))
                    a_ = AT[it % 2]
                    kb.tt(a_[:], ps1[:, 0:128], mask[d][:], ALU.mult)
                    ps2 = next_ps(cx)
                    kb.mm(ps2[:, :], a_[:], v_[:], start=True, stop=False)
                    for fc in range(2):
                        kb.mm(ps2[:, :], qd[:, fc, tk], Sb[:, fc, :], start=False, stop=(fc == 1))
                    if d == 0:
                        o_ = of[it % 2]
                        kb.copy(o_[:], ps2[:, :], e='act')
                        kb.dma(SQ, o1[tk, hh * 512:(hh + 1) * 512], o_[:], w=[o1])
                    else:
                        p_ = op_[it % 2]
                        r_ = rt[it % 2]
                        kb.dma('sp', p_[:], o1[tk, hh * 512:(hh + 1) * 512])
                        kb.dma('sp', r_[:], rr[tk, hh * 512:(hh + 1) * 512])
                        o_ = of[it % 2]
                        kb.tt(o_[:], ps2[:, :], p_[:], ALU.add)
                        s_ = ss[it % 2]
                        kb.act(junk[:], o_[:], AF.Square, accum=s_[:, 0:1])
                        kb.act(s_[:, 1:2], s_[:, 0:1], AF.Sqrt, bias=cx.eps_t[:, 0:1], scale=1.0 / 512)
                        kb.op('dve', lambda E, s_=s_: E.reciprocal(s_[:, 2:3], s_[:, 1:2]), w=[s_], r=[s_])
                        kb.stt(o_[:], o_[:], s_[:, 2:3], hng[:], ALU.mult, ALU.mult)
                        sr_ = sr[it % 2]
                        kb.act(sr_[:], r_[:], AF.Silu)
                        b_ = ob[it % 2]
                        kb.tt(b_[:], o_[:], sr_[:], ALU.mult)
                        kb.dma(SQ, og[tk, hh * 512:(hh + 1) * 512], b_[:], w=[og])
                    for fc in range(2):
                        ps3 = next_ps(cx)
                        kb.mm(ps3[:, :], kst[:, n, fc * 128:(fc + 1) * 128], v_[:])
                        kb.stt(Sf[:, fc, :], Sf[:, fc, :], dec[:, fc, n:n + 1], ps3[:, :], ALU.mult, ALU.add)
                        kb.copy(Sb[:, fc, :], Sf[:, fc, :], e='act')
    proj_out(kb, cx, 'gla_w_out', og[:, 0:2048], 2048)


MIXERS[0] = gla_mixer


def t5_bucket_np(rel):
    import math
    n = np.abs(rel)
    lr = np.log(np.maximum(n, 1).astype(np.float32) / np.float32(8)) / np.float32(math.log(128 / 8))
    large = np.minimum(8 + (lr * np.float32(8)).astype(np.int32), 15)
    return np.where(rel > 0, 16, 0) + np.where(n < 8, n, large)


def attn_consts():
    kp = np.arange(128)[:, None, None]
    dl = np.arange(-1, 2)[None, :, None]
    qf = np.arange(128)[None, None, :]
    rel = dl * 128 + kp - qf
    bk = t5_bucket_np(rel).astype(np.float32)
    mneg = np.where(np.abs(rel) <= 128, 0.0, -30000.0).astype(np.float32)
    return {'c_bk': np.ascontiguousarray(bk.reshape(128, 384)), 'c_mneg': np.ascontiguousarray(mneg.reshape(128, 384))}


def build_bias_tiles(kb, cx, st, swa):
    nc = cx.nc
    sb0 = mk_sb(nc, st)
    T = sb0('a_T', [128, 16, 384], F32)
    cfar = sb0('a_cfar', [128, 32], F32)
    with ExitStack() as s2:
        sb = mk_sb(nc, s2)
        bk = sb('a_bk', [128, 384], F32)
        mk = sb('a_mk', [128, 32, 384], BF16)
        tb = sb('a_tb', [128, 512], F32)
        mn = sb('a_mn', [128, 384], F32)
        kb.dma('sp', bk[:], cx.dram['c_bk'][:, :])
        kb.dma('sp', mn[:], cx.dram['c_mneg'][:, :])
        kb.dma('sp', tb[:], cx.dram['rel_bias'].rearrange("b h -> (b h)").rearrange("(o n) -> o n", o=1).broadcast_to([128, 512]))
        for b in range(32):
            kb.ts(mk[:, b, :], bk[:], float(b), None, op0=ALU.is_equal)
        for h in range(16):
            if swa:
                kb.copy(T[:, h, :], mn[:])
            else:
                kb.memset(T[:, h, :], 0.0)
            for b in range(32):
                kb.stt(T[:, h, :], mk[:, b, :], tb[:, b * 16 + h:b * 16 + h + 1], T[:, h, :], ALU.mult, ALU.add)
            kb.copy(cfar[:, h * 2:h * 2 + 1], tb[:, 15 * 16 + h:15 * 16 + h + 1])
            kb.copy(cfar[:, h * 2 + 1:h * 2 + 2], tb[:, 31 * 16 + h:31 * 16 + h + 1])
    return T, cfar


def qknorm_pass(kb, cx, src, ncols, G, gain_ap, scale, dstT):
    nc = cx.nc
    S = cx.S
    NG = ncols // G
    NCk = ncols // 128
    with ExitStack() as st:
        sb = mk_sb(nc, st)
        xt = [sb('n_x%d' % i, [128, ncols], F32) for i in range(2)]
        sq = sb('n_sq', [128, ncols], F32)
        xb = [sb('n_xb%d' % i, [128, ncols], BF16) for i in range(2)]
        ss = [sb('n_ss%d' % i, [128, 3, NG], F32) for i in range(2)]
        gn = sb('n_g', [128, G], F32)
        stg = [sb('n_st%d' % i, [128, NCk, 128], BF16) for i in range(2)]
        kb.dma('sp', gn[:], gain_ap.broadcast_to([128, G]))
        kb.ts(gn[:], gn[:], float(scale), None, op0=ALU.mult)
        g3 = lambda t: t.rearrange("p (g d) -> p g d", d=G)
        for t in range(S // 128):
            x_ = xt[t % 2]
            s_ = ss[t % 2]
            b_ = xb[t % 2]
            kb.dma('sp', x_[:], src[t * 128:(t + 1) * 128, 0:ncols])
            kb.tt(sq[:], x_[:], x_[:], ALU.mult)
            kb.op('dve', lambda E, s_=s_: E.tensor_reduce(s_[:, 0, :], g3(sq[:]), AX.X, ALU.add), w=[s_], r=[sq])
            kb.act(s_[:, 1, :], s_[:, 0, :], AF.Sqrt, bias=cx.eps_t[:, 0:1], scale=1.0 / G)
            kb.op('dve', lambda E, s_=s_: E.reciprocal(s_[:, 2, :], s_[:, 1, :]), w=[s_], r=[s_])
            kb.tt(g3(sq[:]), g3(x_[:]), s_[:, 2, :].rearrange("p (g o) -> p g o", o=1).broadcast_to([128, NG, G]), ALU.mult)
            kb.tt(g3(b_[:]), g3(sq[:]), gn[:].rearrange("p (o d) -> p o d", o=1).broadcast_to([128, NG, G]), ALU.mult)
            sg = stg[t % 2]
            transpose_to(kb, cx, b_, sg, 0, nchunks=NCk)
            kb.dma(SQ, dstT[0:ncols, :].rearrange("(c p) s -> p c s", p=128)[:, :, t * 128:(t + 1) * 128], sg[:], w=[dstT])


def acc_slots(cx):
    return [(cx.acc[i], 0, True) for i in range(4)]


def diff_mixer(kb, cx, li):
    import math
    nc = cx.nc
    S = cx.S
    NCH = S // 128
    NQT = S // 512
    lam_init = 0.8 - 0.6 * math.exp(-0.3 * li)
    qf = dscr(cx, 'at_q', [S, 2048], F32)
    kf = dscr(cx, 'at_k', [S, 2048], F32)
    vv = dscr(cx, 'at_v', [S, 2048], BF16)
    qT = dscr(cx, 'at_qT', [2048, S], BF16)
    kT = dscr(cx, 'at_kT', [2048, S], BF16)
    og = dscr(cx, 'mix_og', [S, 4096], BF16)
    proj_in(kb, cx, li, 'diff_w_in', [(0, 2048, 'N', qf, 1.0), (2048, 2048, 'N', kf, 1.0), (4096, 2048, 'N', vv, 1.0)])
    qknorm_pass(kb, cx, qf, 2048, 64, cx.dram['diff_q_norm'][0:1, :], 0.125, qT)
    qknorm_pass(kb, cx, kf, 2048, 64, cx.dram['diff_k_norm'][0:1, :], 1.0, kT)
    with ExitStack() as st:
        sb = mk_sb(nc, st)
        T, cfar = build_bias_tiles(kb, cx, st, swa=False)
        lam = sb('d_lam', [128, 4, 64], F32)
        lw = sb('d_lw', [128, 8], F32)
        sub = sb('d_sub', [128, 128], F32)
        qh = sb('d_q', [128, S], BF16)
        kh = sb('d_k', [128, S], BF16)
        v1 = sb('d_v1', [128, NCH, 130], BF16)
        tmp = [sb('d_tmp%d' % i, [128, 512], F32) for i in range(2)]
        PT = [sb('d_PT%d' % i, [128, 512], BF16) for i in range(3)]
        res = [sb('d_res%d' % i, [128, 4, 130], F32) for i in range(2)]
        rc = [sb('d_rc%d' % i, [128, 8], F32) for i in range(2)]
        ot = [sb('d_ot%d' % i, [128, 128], F32) for i in range(2)]
        ob = [sb('d_ob%d' % i, [128, 128], BF16) for i in range(2)]
        junk = sb('d_junk', [128, 128], BF16)
        kb.dma('sp', lam[:], cx.dram['diff_lambda'].rearrange("a d -> (a d)").rearrange("(o n) -> o n", o=1).broadcast_to([128, 256]))
        kb.tt(lam[:, 0, :], lam[:, 0, :], lam[:, 1, :], ALU.mult)
        kb.tt(lam[:, 2, :], lam[:, 2, :], lam[:, 3, :], ALU.mult)
        kb.op('dve', lambda E: E.reduce_sum(lw[:, 0:1], lam[:, 0, :], AX.X), w=[lw], r=[lam])
        kb.op('dve', lambda E: E.reduce_sum(lw[:, 1:2], lam[:, 2, :], AX.X), w=[lw], r=[lam])
        kb.act(lw[:, 2:4], lw[:, 0:2], AF.Exp)
        kb.tt(lw[:, 4:5], lw[:, 3:4], lw[:, 2:3], ALU.subtract)
        kb.ts(lw[:, 5:6], lw[:, 4:5], -lam_init, None, op0=ALU.add)
        kb.dma('sp', sub[:], cx.dram['diff_subln'][0:1, :].broadcast_to([128, 128]))
        kb.ts(sub[:], sub[:], float(1.0 - lam_init), None, op0=ALU.mult)
        kb.memset(v1[:], 1.0)
        slots = acc_slots(cx)
        npt = 0
        for h in range(16):
            kb.dma('sp', qh[:], qT[h * 128:(h + 1) * 128, :])
            kb.dma('sp', kh[:], kT[h * 128:(h + 1) * 128, :])
            kb.dma('sp', v1[:, :, 0:128], vv[:, h * 128:(h + 1) * 128].rearrange("(n p) d -> p n d", p=128))
            for qt in range(NQT):
                for m in range(2):
                    pr = slice(m * 64, (m + 1) * 64)
                    for kc in range(NCH):
                        ps = next_ps(cx)
                        kb.mm(ps[:, :], kh[pr, kc * 128:(kc + 1) * 128], qh[pr, qt * 512:(qt + 1) * 512])
                        p_ = PT[npt % 3]
                        npt += 1
                        dls = [kc - (qt * 4 + qb) for qb in range(4)]
                        if all(abs(dl) >= 2 for dl in dls):
                            sgn = 1 if dls[0] > 0 else 0
                            kb.act(p_[:], ps[:, :], AF.Exp, bias=cfar[:, h * 2 + sgn:h * 2 + sgn + 1], scale=1.0)
                        else:
                            t_ = tmp[npt % 2]
                            for qb, dl in enumerate(dls):
                                blk = slice(qb * 128, (qb + 1) * 128)
                                if abs(dl) <= 1:
                                    kb.tt(t_[:, blk], ps[:, blk], T[:, h, (dl + 1) * 128:(dl + 2) * 128], ALU.add)
                                else:
                                    sgn = 1 if dl > 0 else 0
                                    kb.ts(t_[:, blk], ps[:, blk], cfar[:, h * 2 + sgn:h * 2 + sgn + 1], None, op0=ALU.add)
                            kb.act(p_[:], t_[:], AF.Exp)
                        for qb in range(4):
                            bank, c0, first = slots[qb]
                            kb.mm(bank[:, c0:c0 + 130], p_[:, qb * 128:(qb + 1) * 128], v1[:, kc, :],
                                  start=(kc == 0 and first), stop=(kc == NCH - 1), skip_group_check=True)
                    r_ = res[m]
                    for qb in range(4):
                        bank, c0, first = slots[qb]
                        kb.copy(r_[:, qb, :], bank[:, c0:c0 + 130], e='act' if qb % 2 else 'dve')
                for qb in range(4):
                    c_ = rc[qb % 2]
                    o_ = ot[qb % 2]
                    kb.op('dve', lambda E, c_=c_, qb=qb: E.reciprocal(c_[:, 0:1], res[0][:, qb, 128:129]), w=[c_], r=[res[0]])
                    kb.op('dve', lambda E, c_=c_, qb=qb: E.reciprocal(c_[:, 1:2], res[1][:, qb, 128:129]), w=[c_], r=[res[1]])
                    kb.tt(c_[:, 2:3], c_[:, 1:2], lw[:, 5:6], ALU.mult)
                    kb.ts(o_[:], res[0][:, qb, 0:128], c_[:, 0:1], None, op0=ALU.mult)
                    kb.stt(o_[:], res[1][:, qb, 0:128], c_[:, 2:3], o_[:], ALU.mult, ALU.add)
                    kb.act(junk[:], o_[:], AF.Square, accum=c_[:, 3:4])
                    kb.act(c_[:, 4:5], c_[:, 3:4], AF.Sqrt, bias=cx.eps_t[:, 0:1], scale=1.0 / 128)
                    kb.op('dve', lambda E, c_=c_: E.reciprocal(c_[:, 5:6], c_[:, 4:5]), w=[c_], r=[c_])
                    b_ = ob[qb % 2]
                    kb.stt(b_[:], o_[:], c_[:, 5:6], sub[:], ALU.mult, ALU.mult)
                    t0 = (qt * 4 + qb) * 128
                    kb.dma(SQ, og[t0:t0 + 128, h * 128:(h + 1) * 128], b_[:], w=[og])
    proj_out(kb, cx, 'diff_w_out', og[:, 0:2048], 2048)


def swa_mixer(kb, cx, li):
    nc = cx.nc
    S = cx.S
    NCH = S // 128
    qf = dscr(cx, 'at_q', [S, 2048], F32)
    kf = dscr(cx, 'at_k', [S, 2048], F32)
    vv = dscr(cx, 'at_v', [S, 2048], BF16)
    qT = dscr(cx, 'at_qT', [2048, S], BF16)
    kT = dscr(cx, 'at_kT', [2048, S], BF16)
    og = dscr(cx, 'mix_og', [S, 4096], BF16)
    proj_in(kb, cx, li, 'swa_w_in', [(0, 2048, 'N', qf, 1.0), (2048, 512, 'N', kf, 1.0), (2560, 512, 'N', vv, 1.0)])
    qknorm_pass(kb, cx, qf, 2048, 128, cx.dram['swa_q_norm'][0:1, :], 128 ** -0.5, qT)
    qknorm_pass(kb, cx, kf, 512, 128, cx.dram['swa_k_norm'][0:1, :], 1.0, kT)
    with ExitStack() as st:
        sb = mk_sb(nc, st)
        T, cfar = build_bias_tiles(kb, cx, st, swa=True)
        snk = sb('w_snk', [128, 16], F32)
        q4 = sb('w_q4', [128, 4, S], BF16)
        kg = sb('w_kg', [128, S], BF16)
        v1 = sb('w_v1', [128, NCH, 130], BF16)
        tmp = [sb('w_tmp%d' % i, [128, 512], F32) for i in range(2)]
        PT = [sb('w_PT%d' % i, [128, 512], BF16) for i in range(3)]
        rc = [sb('w_rc%d' % i, [128, 4], F32) for i in range(2)]
        ob = [sb('w_ob%d' % i, [128, 128], BF16) for i in range(2)]
        kb.dma('sp', snk[:], cx.dram['swa_sink'][0:1, :].broadcast_to([128, 16]))
        kb.act(snk[:], snk[:], AF.Exp)
        kb.memset(v1[:], 1.0)
        slots = acc_slots(cx)
        npt = 0
        for g in range(4):
            kb.dma('sp', q4[:], qT[g * 512:(g + 1) * 512, :].rearrange("(c p) s -> p c s", p=128))
            kb.dma('sp', kg[:], kT[g * 128:(g + 1) * 128, :])
            kb.dma('sp', v1[:, :, 0:128], vv[:, g * 128:(g + 1) * 128].rearrange("(n p) d -> p n d", p=128))
            for qb in range(NCH):
                kcs = [kc for kc in (qb - 1, qb, qb + 1) if 0 <= kc < NCH]
                for ik, kc in enumerate(kcs):
                    dl = kc - qb
                    ps = next_ps(cx)
                    kb.mm(ps[:, :], kg[:, kc * 128:(kc + 1) * 128], q4[:, :, qb * 128:(qb + 1) * 128])
                    t_ = tmp[npt % 2]
                    p_ = PT[npt % 3]
                    npt += 1
                    for hh in range(4):
                        blk = slice(hh * 128, (hh + 1) * 128)
                        kb.tt(t_[:, blk], ps[:, blk], T[:, g * 4 + hh, (dl + 1) * 128:(dl + 2) * 128], ALU.add)
                    kb.act(p_[:], t_[:], AF.Exp)
                    for hh in range(4):
                        bank, c0, first = slots[hh]
                        kb.mm(bank[:, c0:c0 + 130], p_[:, hh * 128:(hh + 1) * 128], v1[:, kc, :],
                              start=(ik == 0 and first), stop=(ik == len(kcs) - 1), skip_group_check=True)
                for hh in range(4):
                    bank, c0, first = slots[hh]
                    h = g * 4 + hh
                    c_ = rc[hh % 2]
                    kb.tt(c_[:, 0:1], bank[:, c0 + 128:c0 + 129], snk[:, h:h + 1], ALU.add)
                    kb.op('dve', lambda E, c_=c_: E.reciprocal(c_[:, 1:2], c_[:, 0:1]), w=[c_], r=[c_])
                    b_ = ob[hh % 2]
                    kb.ts(b_[:], bank[:, c0:c0 + 128], c_[:, 1:2], None, op0=ALU.mult)
                    kb.dma(SQ, og[qb * 128:(qb + 1) * 128, h * 128:(h + 1) * 128], b_[:], w=[og])
    proj_out(kb, cx, 'swa_w_out', og[:, 0:2048], 2048)


MIXERS[2] = diff_mixer
MIXERS[3] = swa_mixer


def gdn_mixer(kb, cx, li):
    nc = cx.nc
    S = cx.S
    NCH = S // 128
    preT = dscr(cx, 'gd_preT', [8192, S], BF16)
    qkT = dscr(cx, 'gd_qkT', [4096, S], BF16)
    vT = dscr(cx, 'gd_vT', [4096, S], BF16)
    zz = dscr(cx, 'gd_z', [S, 4096], BF16)
    abt = dscr(cx, 'gd_ab', [S, 128], F32)
    o1 = dscr(cx, 'gd_o1', [S, 4096], F32)
    og = dscr(cx, 'mix_og', [S, 4096], BF16)
    proj_in(kb, cx, li, 'gdn_w_in', [(0, 8192, 'T', preT, 1.0), (8192, 4096, 'N', zz, 1.0), (12288, 128, 'N', abt, 1.0)])
    with ExitStack() as st:
        sb = mk_sb(nc, st)
        cwl = [sb('c_w%d' % i, [128, 5], F32) for i in range(2)]
        xp = [sb('c_xp%d' % i, [128, S + 4], BF16) for i in range(2)]
        y = sb('c_y', [128, S], F32)
        ys = sb('c_ys', [128, S], F32)
        sq = sb('c_sq', [128, S], BF16)
        rs = sb('c_rs', [128, 512], F32)
        ob = [sb('c_ob%d' % i, [128, S], BF16) for i in range(2)]
        onesb = sb('c_ones', [128, 128], BF16)
        kb.memset(onesb[:], 1.0)
        for i in range(2):
            kb.memset(xp[i][:, 0:2], 0.0)
            kb.memset(xp[i][:, S + 2:S + 4], 0.0)
        for c in range(64):
            x_ = xp[c % 2]
            kb.dma('sp', x_[:, 2:S + 2], preT[c * 128:(c + 1) * 128, :])
            cw = cwl[c % 2]
            kb.dma('sp', cw[:], cx.dram['gdn_conv'][:, c * 128:(c + 1) * 128].rearrange("j p -> p j"), allow_slow_non_contiguous=True)
            kb.ts(y[:], x_[:, 0:S], cw[:, 0:1], None, op0=ALU.mult)
            for j in range(1, 5):
                kb.stt(y[:], x_[:, j:j + S], cw[:, j:j + 1], y[:], ALU.mult, ALU.add)
            kb.act(ys[:], y[:], AF.Silu)
            o_ = ob[c % 2]
            if c < 32:
                kb.tt(sq[:], ys[:], ys[:], ALU.mult, e='pool')
                for b0 in range(0, S, 512):
                    ps = next_ps(cx)
                    kb.mm(ps[:, :], onesb[:], sq[:, b0:b0 + 512])
                    kb.act(rs[:], ps[:, :], AF.Sqrt, bias=cx.eps_t[:, 0:1], scale=1.0)
                    kb.op('dve', lambda E: E.reciprocal(rs[:], rs[:]), w=[rs], r=[rs])
                    if c < 16:
                        kb.stt(o_[:, b0:b0 + 512], ys[:, b0:b0 + 512], 128 ** -0.5, rs[:], ALU.mult, ALU.mult)
                    else:
                        kb.tt(o_[:, b0:b0 + 512], ys[:, b0:b0 + 512], rs[:], ALU.mult)
                kb.dma(SQ, qkT[c * 128:(c + 1) * 128, :], o_[:], w=[qkT])
            else:
                kb.copy(o_[:], ys[:], e='pool')
                kb.dma(SQ, vT[(c - 32) * 128:(c - 31) * 128, :], o_[:], w=[vT])
    with ExitStack() as st:
        sb = mk_sb(nc, st)
        psl = cx.ps + cx.acc
        pidx = [0]

        def nps():
            t = psl[pidx[0] % len(psl)]
            pidx[0] += 1
            return t
        g_all = sb('s_g', [128, NCH, 64], F32)
        nb_all = sb('s_nb', [128, NCH, 64], F32)
        gc_all = sb('s_gc', [128, NCH, 64], F32)
        ngc_all = sb('s_ngc', [128, NCH, 64], F32)
        alog = sb('s_alog', [128, 64], F32)
        dtb = sb('s_dtb', [128, 64], F32)
        hng = sb('s_hng', [128, 128], F32)
        UT = sb('s_ut', [128, 128], F32)
        LT = sb('s_lt', [128, 128], F32)
        MSU = sb('s_msu', [128, 128], F32)
        MSL = sb('s_msl', [128, 128], F32)
        abl = [sb('s_ab%d' % i, [128, 128], F32) for i in range(2)]
        tmpa = sb('s_tmpa', [128, 64], F32)
        kb.dma('sp', UT[:], cx.dram['c_masku'][:, :])
        kb.dma('sp', LT[:], cx.dram['c_maskl'][:, :])
        kb.tt(MSU[:], UT[:], cx.ident_f[:], ALU.subtract)
        kb.tt(MSL[:], LT[:], cx.ident_f[:], ALU.subtract)
        kb.dma('sp', alog[:], cx.dram['gdn_a_log'].rearrange("d h -> (d h)").rearrange("(o n) -> o n", o=1).broadcast_to([128, 64]))
        kb.dma('sp', dtb[:], cx.dram['gdn_dt_bias'].rearrange("d h -> (d h)").rearrange("(o n) -> o n", o=1).broadcast_to([128, 64]))
        kb.dma('sp', hng[:], cx.dram['gdn_head_norm'][0:1, :].broadcast_to([128, 128]))
        kb.act(alog[:], alog[:], AF.Exp)
        kb.ts(alog[:], alog[:], -1.0, None, op0=ALU.mult)
        for n in range(NCH):
            a_ = abl[n % 2]
            kb.dma('sp', a_[:], abt[n * 128:(n + 1) * 128, :])
            kb.tt(tmpa[:], a_[:, 0:64], dtb[:], ALU.add)
            kb.act(tmpa[:], tmpa[:], AF.Exp)
            kb.act(tmpa[:], tmpa[:], AF.Ln, bias=1.0, scale=1.0)
            kb.tt(g_all[:, n, :], tmpa[:], alog[:], ALU.mult)
            kb.act(tmpa[:], a_[:, 64:128], AF.Exp, scale=-1.0)
            kb.ts(tmpa[:], tmpa[:], 1.0, None, op0=ALU.add)
            kb.op('dve', lambda E: E.reciprocal(tmpa[:], tmpa[:]), w=[tmpa], r=[tmpa])
            kb.ts(nb_all[:, n, :], tmpa[:], -1.0, None, op0=ALU.mult)
            ps = nps()
            kb.mm(ps[:, 0:32], UT[:], g_all[:, n, 0:32])
            kb.mm(ps[:, 32:64], LT[:], g_all[:, n, 32:64])
            kb.copy(gc_all[:, n, :], ps[:, 0:64])
            kb.ts(ngc_all[:, n, :], gc_all[:, n, :], -1.0, None, op0=ALU.mult, e='dve')
        qh = sb('s_q', [128, S], BF16)
        kh = sb('s_k', [128, S], BF16)
        ktok = sb('s_ktok', [128, NCH, 128], BF16)
        vth = sb('s_vth', [128, S], BF16)
        vtok = [sb('s_vtok%d' % i, [128, NCH, 128], BF16) for i in range(2)]
        bv = [sb('s_bv%d' % i, [128, NCH, 128], BF16) for i in range(4)]
        Sf = [sb('s_Sf%d' % i, [128, 128], F32) for i in range(4)]
        Sb = [sb('s_Sb%d' % i, [128, 128], BF16) for i in range(4)]
        KKs = [sb('s_kk%d' % i, [128, 128], F32) for i in range(2)]
        QKr = [sb('s_qkr%d' % i, [128, 128], F32) for i in range(2)]
        R = 2
        mkf = lambda nm: [sb('s_%s%d' % (nm, i), [128, 128], F32) for i in range(R)]
        mkb = lambda nm: [sb('s_%s%d' % (nm, i), [128, 128], BF16) for i in range(R)]
        gB, gR, GT, GG, Er = mkf('gB'), mkf('gR'), mkf('GT'), mkf('GG'), mkf('Er')
        Xa, Xb_, Ya, Yb_, Pa = mkf('Xa'), mkf('Xb'), mkf('Ya'), mkf('Yb'), mkf('Pa')
        QKT, MT, kdT, qdT, kst, Xv, vnb = mkb('QKT'), mkb('MT'), mkb('kdT'), mkb('qdT'), mkb('kst'), mkb('Xv'), mkb('vnb')
        sc = [sb('s_sc%d' % i, [128, 8], F32) for i in range(R)]
        of = [sb('s_of%d' % i, [128, 128], F32) for i in range(2)]
        op_ = [sb('s_op%d' % i, [128, 128], F32) for i in range(2)]
        zt = [sb('s_z%d' % i, [128, 128], BF16) for i in range(2)]
        zs = [sb('s_zs%d' % i, [128, 128], F32) for i in range(2)]
        obf = [sb('s_obf%d' % i, [128, 128], BF16) for i in range(2)]
        junk = sb('s_junk', [128, 128], BF16)
        rot = [0]
        for hq in range(16):
            kb.dma('sp', qh[:], qkT[hq * 128:(hq + 1) * 128, :])
            kb.dma('sp', kh[:], qkT[2048 + hq * 128:2048 + (hq + 1) * 128, :])
            for n0 in range(0, NCH, 8):
                pb = next_pb(cx)
                for n in range(8):
                    kb.tr(pb[:, n * 128:(n + 1) * 128], kh[:, (n0 + n) * 128:(n0 + n + 1) * 128], cx.ident_b[:])
                kb.copy(ktok[:, n0:n0 + 8, :], pb[:, :].rearrange("p (n t) -> p n t", t=128), e='act')
            chains = []
            for v2 in range(2):
                hv = hq * 2 + v2
                kb.dma('sp', vth[:], vT[hv * 128:(hv + 1) * 128, :])
                for n0 in range(0, NCH, 8):
                    pb = next_pb(cx)
                    for n in range(8):
                        kb.tr(pb[:, n * 128:(n + 1) * 128], vth[:, (n0 + n) * 128:(n0 + n + 1) * 128], cx.ident_b[:])
                    kb.copy(vtok[v2][:, n0:n0 + 8, :], pb[:, :].rearrange("p (n t) -> p n t", t=128), e='act')
                for d in range(2):
                    ci = v2 * 2 + d
                    col = d * 32 + hv
                    kb.tt(bv[ci][:], vtok[v2][:], nb_all[:, :, col:col + 1].broadcast_to([128, NCH, 128]), ALU.mult)
                    kb.ts(bv[ci][:], bv[ci][:], -1.0, None, op0=ALU.mult, e='pool')
                    kb.memset(Sf[ci][:], 0.0)
                    kb.memset(Sb[ci][:], 0.0)
                    chains.append((ci, v2, hv, d, col))
            done = {}
            for s_ in range(NCH):
                raw = {}
                for d, n in ((0, s_), (1, NCH - 1 - s_)):
                    tk = slice(n * 128, (n + 1) * 128)
                    ps = nps()
                    kb.mm(ps[:, 0:128], kh[:, tk], kh[:, tk])
                    kb.mm(ps[:, 128:256], kh[:, tk], qh[:, tk])
                    kb.tt(KKs[d][:], ps[:, 0:128], (MSL if d == 0 else MSU)[:], ALU.mult)
                    kb.copy(QKr[d][:], ps[:, 128:256], e='dve')
                    raw[d] = n
                for (ci, v2, hv, d, col) in chains:
                    n = raw[d]
                    tk = slice(n * 128, (n + 1) * 128)
                    r = rot[0] % R
                    rot[0] += 1
                    li_ = 127 if d == 0 else 0
                    gcol = gc_all[:, n, col:col + 1]
                    ngcol = ngc_all[:, n, col:col + 1]
                    nbcol = nb_all[:, n, col:col + 1]
                    kb.copy(gB[r][:], g_all[:, n, col:col + 1].broadcast_to([128, 128]), e='dve')
                    pg = nps()
                    kb.mm(pg[:, 0:128], gB[r][:], (UT if d == 0 else LT)[:])
                    kb.copy(gR[r][:], pg[:, 0:128], e='dve')
                    kb.ts(GT[r][:], gR[r][:], ngcol, 0.0, op0=ALU.add, op1=ALU.min)
                    kb.act(GT[r][:], GT[r][:], AF.Exp)
                    kb.ts(GG[r][:], gR[r][:], gcol, 0.0, op0=ALU.subtract, op1=ALU.max)
                    kb.act(GG[r][:], GG[r][:], AF.Exp, scale=-1.0)
                    kb.act(Er[r][:], gR[r][:], AF.Exp)
                    kb.copy(sc[r][:, 0:1], gR[r][:, li_:li_ + 1])
                    kb.act(sc[r][:, 1:2], sc[r][:, 0:1], AF.Exp)
                    kb.act(sc[r][:, 2:3], gcol, AF.Exp, bias=sc[r][:, 0:1], scale=-1.0)
                    kb.tt(GT[r][:], GT[r][:], (UT if d == 0 else LT)[:], ALU.mult)
                    kb.tt(QKT[r][:], QKr[d][:], GT[r][:], ALU.mult)
                    kb.stt(Ya[r][:], KKs[d][:], nbcol, GG[r][:], ALU.mult, ALU.mult)
                    px = nps()
                    kb.tr(px[:, 0:128], Ya[r][:], cx.ident_f[:])
                    kb.copy(Xa[r][:], px[:, 0:128], e='dve')
                    kb.tt(Pa[r][:], Xa[r][:], cx.ident_f[:], ALU.add)
                    X, Y, Xn, Yn = Xa[r], Ya[r], Xb_[r], Yb_[r]
                    for k_ in range(1, 7):
                        pyy = nps()
                        kb.mm(pyy[:, 0:128], X[:], Y[:])
                        if k_ < 6:
                            pxx = nps()
                            kb.mm(pxx[:, 0:128], Y[:], X[:])
                        kb.copy(Yn[:], pyy[:, 0:128], e='act')
                        if k_ < 6:
                            kb.copy(Xn[:], pxx[:, 0:128], e='dve')
                        pp = nps()
                        kb.mm(pp[:, 0:128], Yn[:], Pa[r][:])
                        kb.tt(Pa[r][:], Pa[r][:], pp[:, 0:128], ALU.add)
                        X, Y, Xn, Yn = Xn, Yn, X, Y
                    kb.copy(MT[r][:], Pa[r][:], e='act')
                    kb.tt(kdT[r][:], kh[:, tk], Er[r][:], ALU.mult)
                    kb.tt(qdT[r][:], qh[:, tk], Er[r][:], ALU.mult)
                    kb.ts(kst[r][:], ktok[:, n, :], sc[r][:, 2:3], None, op0=ALU.mult)
                    p1 = nps()
                    kb.mm(p1[:, 0:128], kdT[r][:], Sb[ci][:])
                    kb.stt(Xv[r][:], p1[:, 0:128], nbcol, bv[ci][:, n, :], ALU.mult, ALU.add)
                    p2 = nps()
                    kb.mm(p2[:, 0:128], MT[r][:], Xv[r][:])
                    kb.copy(vnb[r][:], p2[:, 0:128], e='act')
                    p3 = nps()
                    kb.mm(p3[:, 0:128], qdT[r][:], Sb[ci][:], start=True, stop=False)
                    kb.mm(p3[:, 0:128], QKT[r][:], vnb[r][:], start=False, stop=True)
                    key = (hv, n)
                    i2 = rot[0] % 2
                    if key not in done:
                        done[key] = 1
                        kb.copy(of[i2][:], p3[:, 0:128], e='act')
                        kb.dma(SQ, o1[tk, hv * 128:(hv + 1) * 128], of[i2][:], w=[o1])
                    else:
                        kb.dma('sp', op_[i2][:], o1[tk, hv * 128:(hv + 1) * 128])
                        kb.dma('sp', zt[i2][:], zz[tk, hv * 128:(hv + 1) * 128])
                        kb.tt(of[i2][:], p3[:, 0:128], op_[i2][:], ALU.add)
                        kb.act(junk[:], of[i2][:], AF.Square, accum=sc[r][:, 3:4])
                        kb.act(sc[r][:, 4:5], sc[r][:, 3:4], AF.Sqrt, bias=cx.eps_t[:, 0:1], scale=1.0 / 128)
                        kb.op('dve', lambda E, r=r: E.reciprocal(sc[r][:, 5:6], sc[r][:, 4:5]), w=[sc[r]], r=[sc[r]])
                        kb.stt(of[i2][:], of[i2][:], sc[r][:, 5:6], hng[:], ALU.mult, ALU.mult)
                        kb.act(zs[i2][:], zt[i2][:], AF.Silu)
                        kb.tt(obf[i2][:], of[i2][:], zs[i2][:], ALU.mult)
                        kb.dma(SQ, og[tk, hv * 128:(hv + 1) * 128], obf[i2][:], w=[og])
                    p4 = nps()
                    kb.mm(p4[:, 0:128], kst[r][:], vnb[r][:])
                    kb.stt(Sf[ci][:], Sf[ci][:], sc[r][:, 1:2], p4[:, 0:128], ALU.mult, ALU.add)
                    kb.copy(Sb[ci][:], Sf[ci][:], e='act')
    proj_out(kb, cx, 'gdn_w_out', og[:, 0:4096], 4096)


MIXERS[1] = gdn_mixer


SMALL_INPUTS = {
    'gla_w_gate_up': [2, 16, 1024], 'gla_b_gate': [2, 1024], 'gla_head_norm': [1, 512],
    'gdn_conv': [5, 8192], 'gdn_a_log': [2, 32], 'gdn_dt_bias': [2, 32], 'gdn_head_norm': [1, 128],
    'diff_q_norm': [1, 64], 'diff_k_norm': [1, 64], 'diff_lambda': [4, 64], 'diff_subln': [1, 128],
    'swa_q_norm': [1, 128], 'swa_k_norm': [1, 128], 'swa_sink': [1, 16],
}


def kernel(**inputs):
    x = np.asarray(inputs['x'])
    ncores = NCORES
    cfg = dict(S=S, layers=[0, 1, 2, 3], ncores=ncores, small_inputs=SMALL_INPUTS)
    outs = run(cfg, inputs, [x[b] for b in range(ncores)])
    return np.stack(outs, axis=0).astype(np.float32)
```

```python
import numpy as np
from contextlib import ExitStack
import concourse.bass as bass
import concourse.mybir as mybir
from concourse.bass_utils import run_bass_kernel_spmd

F32 = mybir.dt.float32
BF16 = mybir.dt.bfloat16
I32 = mybir.dt.int32
U32 = mybir.dt.uint32
AF = mybir.ActivationFunctionType
ALU = mybir.AluOpType
AX = mybir.AxisListType

S = 4096
D = 2048
NCORES = 8
NDS = 40
SAME_SYNC = True
SQ = 'pool'


def _key(x):
    if isinstance(x, str):
        return x
    return getattr(x, 'tensor', x).name


class KB:
    def __init__(self, nc, st):
        self.nc = nc
        self.E = {'pe': nc.tensor, 'dve': nc.vector, 'act': nc.scalar, 'pool': nc.gpsimd, 'sp': nc.sync}
        self.sem = {e: st.enter_context(nc.semaphore('sm_' + e)) for e in self.E}
        self.cnt = {e: 0 for e in self.E}
        self.seen = {e: {} for e in self.E}
        self.lastw = {}
        self.readers = {}
        self.dsem = [st.enter_context(nc.semaphore('sd%d' % i)) for i in range(NDS)]
        self.dtarget = [0] * NDS
        self.dnext = 0
        self.ninst = 0

    def _semof(self, sk):
        return self.sem[sk] if isinstance(sk, str) else self.dsem[sk[1]]

    def wait(self, e, tok):
        sk, val = tok
        if val <= 0:
            return
        if sk == e and not (SAME_SYNC and e in ('dve', 'act', 'pool')):
            return
        if self.seen[e].get(sk, 0) >= val:
            return
        self.E[e].wait_ge(self._semof(sk), val)
        self.seen[e][sk] = val

    def _deps(self, e, w, r):
        for k in r:
            k = _key(k)
            if k in self.lastw:
                self.wait(e, self.lastw[k])
        for k in w:
            k = _key(k)
            if k in self.lastw:
                self.wait(e, self.lastw[k])
            for sk, val in self.readers.get(k, {}).items():
                self.wait(e, (sk, val))

    def _commit(self, tok, w, r):
        for k in r:
            k = _key(k)
            d = self.readers.setdefault(k, {})
            d[tok[0]] = max(d.get(tok[0], 0), tok[1])
        for k in w:
            k = _key(k)
            self.lastw[k] = tok
            self.readers[k] = {}

    def op(self, e, fn, w=(), r=()):
        self._deps(e, w, r)
        inst = fn(self.E[e])
        self.cnt[e] += 1
        inst.then_inc(self.sem[e], 1)
        self._commit((e, self.cnt[e]), w, r)
        self.ninst += 1
        return inst

    def dma(self, q, out, in_, w=None, r=None, fn=None, **kw):
        w = [out] if w is None else w
        r = [in_] if r is None else r
        i = self.dnext
        self.dnext = (i + 1) % NDS
        self.wait(q, (('d', i), self.dtarget[i]))
        self._deps(q, w, r)
        if fn is None:
            inst = self.E[q].dma_start(out=out, in_=in_, **kw)
        else:
            inst = fn(self.E[q])
        self.dtarget[i] += 16
        inst.then_inc(self.dsem[i], 16)
        self._commit((('d', i), self.dtarget[i]), w, r)
        self.ninst += 1

    def barrier(self):
        for e in self.E:
            for e2 in self.E:
                if e2 != e:
                    self.wait(e, (e2, self.cnt[e2]))
            for i in range(NDS):
                self.wait(e, (('d', i), self.dtarget[i]))

    def finish(self):
        for i in range(NDS):
            self.wait('sp', (('d', i), self.dtarget[i]))
        for e in self.E:
            if e != 'sp':
                self.wait('sp', (e, self.cnt[e]))

    def mm(self, out, lhsT, rhs, start=True, stop=True, **kw):
        return self.op('pe', lambda E: E.matmul(out, lhsT, rhs, start=start, stop=stop, **kw), w=[out], r=[lhsT, rhs])

    def tr(self, out, in_, ident):
        return self.op('pe', lambda E: E.transpose(out, in_, ident), w=[out], r=[in_, ident])

    def act(self, out, in_, func, bias=None, scale=None, accum=None, e='act'):
        kw = {}
        r = [in_]
        w = [out]
        if bias is not None:
            kw['bias'] = bias
            if not isinstance(bias, (int, float)):
                r.append(bias)
        if scale is not None:
            kw['scale'] = scale
            if not isinstance(scale, (int, float)):
                r.append(scale)
        if accum is not None:
            kw['accum_out'] = accum
            w.append(accum)
        return self.op('act', lambda E: E.activation(out, in_, func, **kw), w=w, r=r)

    def tt(self, out, a, b, op, e='dve'):
        return self.op(e, lambda E: E.tensor_tensor(out, a, b, op), w=[out], r=[a, b])

    def ts(self, out, a, s1, s2=None, op0=ALU.mult, op1=None, e='dve', accum=None):
        r = [a] + [s for s in (s1, s2) if s is not None and not isinstance(s, (int, float))]
        w = [out] + ([accum] if accum is not None else [])
        kw = {}
        if op1 is not None:
            kw['op1'] = op1
        if accum is not None:
            kw['accum_out'] = accum
        return self.op(e, lambda E: E.tensor_scalar(out, a, s1, s2, op0, **kw), w=w, r=r)

    def stt(self, out, a, sc, b, op0, op1):
        r = [a, b] + ([] if isinstance(sc, (int, float)) else [sc])
        return self.op('dve', lambda E: E.scalar_tensor_tensor(out, a, sc, b, op0, op1), w=[out], r=r)

    def copy(self, out, in_, e='dve'):
        if e == 'act':
            return self.op('act', lambda E: E.copy(out, in_), w=[out], r=[in_])
        return self.op(e, lambda E: E.tensor_copy(out, in_), w=[out], r=[in_])

    def memset(self, out, val, e='dve'):
        return self.op(e, lambda E: E.memset(out, val), w=[out], r=[])


WCOLS = 2048


class WTable:
    def __init__(self):
        self.ents = {}
        self.R = 0

    def add(self, name, K, N):
        n = K * N
        assert n % WCOLS == 0
        rows = n // WCOLS
        self.ents[name] = (self.R, rows, K, N)
        self.R += rows

    def host_shards(self, arrays):
        out = np.empty((self.R, WCOLS), np.float32)
        for name, (ro, rows, K, N) in self.ents.items():
            out[ro:ro + rows] = np.ascontiguousarray(arrays[name]).reshape(rows, WCOLS)
        return out

    def ap(self, gathered_flat, name, k0, kk, n0, nn):
        ro, rows, K, N = self.ents[name]
        base = ro * WCOLS + k0 * N
        return gathered_flat[base: base + kk * N].rearrange("(k n) -> k n", n=N)[:, n0:n0 + nn]


def build_wtable(layers):
    wt = WTable()
    if 0 in layers:
        wt.add('gla_w_in', D, 6176)
        wt.add('gla_w_out', D, D)
    if 1 in layers:
        wt.add('gdn_w_in', D, 12416)
        wt.add('gdn_w_out', 4096, D)
    if 2 in layers:
        wt.add('diff_w_in', D, 6144)
        wt.add('diff_w_out', D, D)
    if 3 in layers:
        wt.add('swa_w_in', D, 3072)
        wt.add('swa_w_out', D, D)
    return wt


class Ctx:
    pass


_UNIQ = [0]


_KB = [None]


def mk_sb(nc, st):
    st.callback(lambda: _KB[0].barrier())

    def sb(name, shape, dt):
        _UNIQ[0] += 1
        return st.enter_context(nc.sbuf_tensor('%s_u%d' % (name, _UNIQ[0]), shape, dt))
    return sb


def make_consts():
    ident = np.eye(128, dtype=np.float32)
    masku = np.triu(np.ones((128, 128), np.float32))
    maskl = np.tril(np.ones((128, 128), np.float32))
    d = {'c_ident': ident, 'c_masku': masku, 'c_maskl': maskl}
    d.update(attn_consts())
    return d


def rows_bcast(ap_row, n=128):
    return ap_row.broadcast_to([n, ap_row.shape[-1]])


def load_consts(kb, cx, st):
    nc = cx.nc
    cx.ident_f = st.enter_context(nc.sbuf_tensor('ident_f', [128, 128], F32))
    cx.ident_b = st.enter_context(nc.sbuf_tensor('ident_b', [128, 128], BF16))
    kb.dma('sp', cx.ident_f[:], cx.dram['c_ident'][:, :])
    kb.copy(cx.ident_b[:], cx.ident_f[:])
    cx.ps = [st.enter_context(nc.psum_tensor('ps%d' % i, [128, 512], F32)) for i in range(3)]
    cx.acc = [st.enter_context(nc.psum_tensor('acc%d' % i, [128, 512], F32)) for i in range(4)]
    cx.pb = [st.enter_context(nc.psum_tensor('pb%d' % i, [128, 1024], BF16)) for i in range(1)]
    cx.psi = 0
    cx.pbi = 0


def next_ps(cx):
    t = cx.ps[cx.psi % len(cx.ps)]
    cx.psi += 1
    return t


def next_pb(cx):
    t = cx.pb[cx.pbi % len(cx.pb)]
    cx.pbi += 1
    return t


def norm_tile(kb, cx, bufs, h_rows, gain_bc, i):
    ht = bufs['h'][i % 2]
    xn = bufs['xn'][i % 2]
    ss = bufs['ss'][i % 2]
    kb.dma('sp', ht[:], h_rows, r=['h'])
    kb.act(bufs['junk'][:], ht[:], AF.Square, accum=ss[:, 0:1])
    kb.act(ss[:, 1:2], ss[:, 0:1], AF.Sqrt, bias=cx.eps_t[:, 0:1], scale=1.0 / D)
    kb.op('dve', lambda E: E.reciprocal(ss[:, 2:3], ss[:, 1:2]), w=[ss], r=[ss])
    kb.stt(xn[:], ht[:], ss[:, 2:3], gain_bc[:], ALU.mult, ALU.mult)
    return xn


def transpose_to(kb, cx, src_bf, dstT, tok0, ntok=128, nchunks=16):
    for c0 in range(0, nchunks, 8):
        pb = next_pb(cx)
        n = min(8, nchunks - c0)
        for c in range(n):
            kb.tr(pb[:, c * 128:(c + 1) * 128], src_bf[:, (c0 + c) * 128:(c0 + c + 1) * 128], cx.ident_b[:])
        kb.copy(dstT[:, c0:c0 + n, tok0:tok0 + 128],
                pb[:, 0:n * 128].rearrange("p (c t) -> p c t", t=128), e='act' if (c0 // 8) % 2 else 'dve')


def moe_layer(kb, cx, li):
    nc = cx.nc
    S = cx.S
    NE, DFF = 16, 1024
    CAP = 2 * S // NE
    NJ = CAP // 128
    h = cx.h
    hn = cx.hn_bf
    with ExitStack() as st:
        sb = mk_sb(nc, st)
        affT = sb('m_affT', [NE, S], F32)
        stA = ExitStack()
        sbA = mk_sb(nc, stA)
        bufs = {'h': [sbA('m_h%d' % i, [128, D], F32) for i in range(2)],
                'xn': [sbA('m_xn%d' % i, [128, D], BF16) for i in range(2)],
                'ss': [sbA('m_ss%d' % i, [128, 4], F32) for i in range(2)],
                'junk': sbA('m_junk', [128, D], BF16)}
        gain = sbA('m_gain', [128, D], F32)
        rt_f = sbA('m_rtf', [128, 16, NE], F32)
        rt_b = sbA('m_rtb', [128, 16, NE], BF16)
        hnT = sbA('m_hnT', [128, 16, 128], BF16)
        lg = [sbA('m_lg%d' % i, [128, NE + 4], F32) for i in range(2)]
        kb.dma('sp', gain[:], rows_bcast(cx.dram['norm_ffn'][li:li + 1, :]))
        kb.dma('sp', rt_f[:], cx.dram['moe_router'][li].rearrange("(c p) e -> p c e", p=128))
        kb.copy(rt_b[:], rt_f[:])
        for t in range(S // 128):
            xn = norm_tile(kb, cx, bufs, h[t * 128:(t + 1) * 128, :], gain, t)
            kb.dma(SQ, hn[t * 128:(t + 1) * 128, :], xn[:], w=['hn'])
            transpose_to(kb, cx, xn, hnT, 0)
            ps = next_ps(cx)
            for c in range(16):
                kb.mm(ps[:, 0:NE], hnT[:, c, :], rt_b[:, c, :], start=(c == 0), stop=(c == 15))
            l = lg[t % 2]
            kb.op('dve', lambda E: E.reduce_max(l[:, NE:NE + 1], ps[:, 0:NE], AX.X), w=[l], r=[ps])
            kb.ts(l[:, NE + 1:NE + 2], l[:, NE:NE + 1], -1.0, None, op0=ALU.mult)
            kb.act(l[:, 0:NE], ps[:, 0:NE], AF.Exp, bias=l[:, NE + 1:NE + 2], scale=1.0, accum=l[:, NE + 2:NE + 3])
            kb.op('dve', lambda E: E.reciprocal(l[:, NE + 3:NE + 4], l[:, NE + 2:NE + 3]), w=[l], r=[l])
            kb.ts(l[:, 0:NE], l[:, 0:NE], l[:, NE + 3:NE + 4], None, op0=ALU.mult)
            ps2 = next_ps(cx)
            kb.tr(ps2[0:NE, 0:128], l[:, 0:NE], cx.ident_f[:])
            kb.copy(affT[:, t * 128:(t + 1) * 128], ps2[0:NE, 0:128])
        stA.close()
        gate = sb('m_gate', [NE, CAP], F32)
        idx = sb('m_idx', [NE, CAP], U32)
        for r8 in range(CAP // 8):
            g8 = gate[:, r8 * 8:(r8 + 1) * 8]
            kb.op('dve', lambda E: E.max(g8, affT[:]), w=[gate], r=[affT])
            kb.op('dve', lambda E: E.max_index(idx[:, r8 * 8:(r8 + 1) * 8], g8, affT[:]), w=[idx], r=[gate, affT])
            kb.op('dve', lambda E: E.match_replace(affT[:], g8, affT[:], -1.0), w=[affT], r=[gate, affT])
        kb.dma('sp', cx.sc_gate[:, :], gate[:], w=['sc_gate'])
        kb.dma('sp', cx.sc_idx[:, :], idx[:], w=['sc_idx'])
        gateT = sb('m_gateT', [128, NE * NJ], F32)
        idxT = sb('m_idxT', [128, NE * NJ], U32)
        for e in range(NE):
            kb.dma('sp', gateT[:, e * NJ:(e + 1) * NJ], cx.sc_gate[e].rearrange("(j p) -> p j", p=128),
                   r=['sc_gate'], allow_slow_non_contiguous=True)
            kb.dma('sp', idxT[:, e * NJ:(e + 1) * NJ], cx.sc_idx[e].rearrange("(j p) -> p j", p=128),
                   r=['sc_idx'], allow_slow_non_contiguous=True)
        xs = [[sb('m_xs%d_%d' % (i, j), [128, D], BF16) for j in range(NJ)] for i in range(2)]
        xsT = [sb('m_xsT%d' % i, [128, 16, CAP], BF16) for i in range(2)]
        wa = [sb('m_wa%d' % i, [128, 16, 256], BF16) for i in range(2)]
        wb = [sb('m_wb%d' % i, [128, 16, 256], BF16) for i in range(2)]
        w2t = [sb('m_w2%d' % i, [128, 8, 512], BF16) for i in range(2)]
        gT = sb('m_gT', [128, 8, CAP], BF16)
        tmp = [sb('m_tmp%d' % i, [128, 512], F32) for i in range(2)]
        ybuf = [sb('m_y%d' % i, [128, D], F32) for i in range(NJ)]
        nld = 0

        def gathers(e):
            for j in range(NJ):
                x_ = xs[e % 2][j]
                col = e * NJ + j
                kb.dma('pool', x_[:], hn[:, :], r=['hn', idxT], w=[x_],
                       fn=lambda E, x_=x_, col=col: E.indirect_dma_start(
                           out=x_[:], out_offset=None, in_=hn[:, :],
                           in_offset=bass.IndirectOffsetOnAxis(ap=idxT[:, col:col + 1], axis=0)))

        def transposes(e):
            for j in range(NJ):
                transpose_to(kb, cx, xs[e % 2][j], xsT[e % 2], j * 128)

        gathers(0)
        transposes(0)
        for e in range(NE):
            xT = xsT[e % 2]
            if e + 1 < NE:
                gathers(e + 1)
            for fg in range(4):
                a_ = wa[nld % 2]
                b_ = wb[nld % 2]
                nld += 1
                kb.dma('sp', a_[:], cx.Wtile('moe_w1_%d' % li, e * D, 16, fg * 256, 256), r=['wg'])
                kb.dma('sp', b_[:], cx.Wtile('moe_w3_%d' % li, e * D, 16, fg * 256, 256), r=['wg'])
                for f in range(2):
                    fc = fg * 2 + f
                    pa = next_ps(cx)
                    pbm = next_ps(cx)
                    for c in range(16):
                        kb.mm(pa[:, 0:CAP], a_[:, c, f * 128:(f + 1) * 128], xT[:, c, :], start=(c == 0), stop=(c == 15))
                    for c in range(16):
                        kb.mm(pbm[:, 0:CAP], b_[:, c, f * 128:(f + 1) * 128], xT[:, c, :], start=(c == 0), stop=(c == 15))
                    tm = tmp[fc % 2]
                    kb.act(tm[:, 0:CAP], pa[:, 0:CAP], AF.Silu)
                    kb.tt(gT[:, fc, :], tm[:, 0:CAP], pbm[:, 0:CAP], ALU.mult)
            for dc in range(4):
                w_ = w2t[dc % 2]
                kb.dma('sp', w_[:], cx.Wtile('moe_w2_%d' % li, e * 1024, 8, dc * 512, 512), r=['wg'])
                for j in range(NJ):
                    py = next_ps(cx)
                    for c in range(8):
                        kb.mm(py[:, :], gT[:, c, j * 128:(j + 1) * 128], w_[:, c, :], start=(c == 0), stop=(c == 7))
                    kb.ts(ybuf[j][:, dc * 512:(dc + 1) * 512], py[:, :], gateT[:, e * NJ + j:e * NJ + j + 1], None, op0=ALU.mult)
            if e + 1 < NE:
                transposes(e + 1)
            for j in range(NJ):
                col = e * NJ + j
                yb = ybuf[j]
                kb.dma('pool', h[:, :], yb[:], w=['h'], r=[yb, idxT],
                       fn=lambda E, yb=yb, col=col: E.indirect_dma_start(
                           out=h[:, :], out_offset=bass.IndirectOffsetOnAxis(ap=idxT[:, col:col + 1], axis=0),
                           in_=yb[:], in_offset=None, compute_op=ALU.add))


def prologue_weights(kb, cx):
    nc = cx.nc
    wsh = cx.dram['wsh']
    with ExitStack() as st:
        f = [st.enter_context(nc.sbuf_tensor('pw_f%d' % i, [128, 4096], F32)) for i in range(2)]
        b = [st.enter_context(nc.sbuf_tensor('pw_b%d' % i, [128, 4096], BF16)) for i in range(2)]
        i = 0
        for name, (ro, rows, K_, N_) in cx.wt.ents.items():
            per = rows * WCOLS // 128
            src = wsh[ro:ro + rows, :].rearrange("r c -> (r c)").rearrange("(p n) -> p n", p=128)
            dst = cx.wbf[name].rearrange("r c -> (r c)").rearrange("(p n) -> p n", p=128)
            for c0 in range(0, per, 4096):
                n = min(4096, per - c0)
                kb.dma('sp', f[i % 2][:, 0:n], src[:, c0:c0 + n])
                kb.copy(b[i % 2][:, 0:n], f[i % 2][:, 0:n], e=('dve' if i % 2 == 0 else 'pool'))
                kb.dma(SQ, dst[:, c0:c0 + n], b[i % 2][:, 0:n], w=['wg'])
                i += 1
        kb.barrier()


def build_program(cfg):
    nc = bass.Bass("TRN2", target_bir_lowering=False)
    cx = Ctx()
    cx.nc = nc
    cx.S = cfg['S']
    cx.ncores = cfg.get('ncores', NCORES)
    cx.debug_og = cfg.get('debug_og', False)
    cx.stop = cfg.get('stop', 0)
    Sx = cx.S
    layers = cfg['layers']
    wt = WTable()
    for li in layers:
        if cfg.get('mixers', True):
            if li == 0:
                wt.add('gla_w_in', D, 6176); wt.add('gla_w_out', D, D)
            if li == 1:
                wt.add('gdn_w_in', D, 12416); wt.add('gdn_w_out', 4096, D)
            if li == 2:
                wt.add('diff_w_in', D, 6144); wt.add('diff_w_out', D, D)
            if li == 3:
                wt.add('swa_w_in', D, 3072); wt.add('swa_w_out', D, D)
        if cfg.get('moe', True):
            wt.add('moe_w1_%d' % li, 16 * D, 1024)
            wt.add('moe_w3_%d' % li, 16 * D, 1024)
            wt.add('moe_w2_%d' % li, 16 * 1024, D)
    cx.wt = wt
    dram = {}

    def din(name, shape, dt=F32):
        dram[name] = nc.dram_tensor(name, list(shape), dt, kind="ExternalInput").ap()

    din('x', [Sx, D])
    din('wsh', [wt.R, WCOLS])
    din('c_ident', [128, 128]); din('c_masku', [128, 128]); din('c_maskl', [128, 128]); din('c_bk', [128, 384]); din('c_mneg', [128, 384])
    cx.scr = {}
    din('norm_mix', [4, D]); din('norm_ffn', [4, D]); din('moe_router', [4, D, 16]); din('rel_bias', [32, 16])
    for name, shape in cfg.get('small_inputs', {}).items():
        din(name, shape)
    out = nc.dram_tensor('out', [Sx, D], F32, kind="ExternalOutput").ap()
    cx.dram = dram
    cx.h = out
    cx.hn_bf = nc.dram_tensor('hn_bf', [Sx, D], BF16, kind="Internal").ap()
    cx.sc_gate = nc.dram_tensor('sc_gate', [16, 2 * Sx // 16], F32, kind="Internal").ap()
    cx.sc_idx = nc.dram_tensor('sc_idx', [16, 2 * Sx // 16], U32, kind="Internal").ap()
    cx.wbf = {}
    for name, (ro, rows, K_, N_) in wt.ents.items():
        cx.wbf[name] = nc.dram_tensor('wbf_' + name, [rows, WCOLS], BF16, kind="Internal").ap()

    def Wap(name, k0, kk, n0, nn):
        ro, rows, K_, N_ = wt.ents[name]
        flat = cx.wbf[name].rearrange("r c -> (r c)")
        return flat[k0 * N_: (k0 + kk) * N_].rearrange("(k n) -> k n", n=N_)[:, n0:n0 + nn]
    cx.W = Wap

    def Wtile(name, k0, KC, n0, nn):
        ro, rows, K_, N_ = wt.ents[name]
        flat = cx.wbf[name].rearrange("r c -> (r c)")
        return flat[k0 * N_: (k0 + KC * 128) * N_].rearrange("(c p n) -> p c n", p=128, n=N_)[:, :, n0:n0 + nn]
    cx.Wtile = Wtile
    cx.wmoe = {}
    for li in layers:
        cx.wmoe[('w1', li)] = (lambda e, k0, kk, n0, nn, li=li: cx.W('moe_w1_%d' % li, e * D + k0, kk, n0, nn))
        cx.wmoe[('w3', li)] = (lambda e, k0, kk, n0, nn, li=li: cx.W('moe_w3_%d' % li, e * D + k0, kk, n0, nn))
        cx.wmoe[('w2', li)] = (lambda e, k0, kk, n0, nn, li=li: cx.W('moe_w2_%d' % li, e * 1024 + k0, kk, n0, nn))

    with ExitStack() as st:
        kb = KB(nc, st)
        cx.kb = kb
        _KB[0] = kb
        load_consts(kb, cx, st)
        cx.eps_t = st.enter_context(nc.sbuf_tensor('eps_t', [128, 1], F32))
        kb.memset(cx.eps_t[:], 1e-6)
        prologue_weights(kb, cx)
        with ExitStack() as st2:
            tb = [st2.enter_context(nc.sbuf_tensor('cp%d' % i, [128, D], F32)) for i in range(2)]
            for t in range(Sx // 128):
                kb.dma('sp', tb[t % 2][:], dram['x'][t * 128:(t + 1) * 128, :])
                kb.dma(SQ, cx.h[t * 128:(t + 1) * 128, :], tb[t % 2][:], w=['h'])
            kb.barrier()
        for li in layers:
            if cfg.get('mixers', True):
                MIXERS[li](kb, cx, li)
            if cfg.get('moe', True):
                moe_layer(kb, cx, li)
        kb.finish()
    cx.ninst = kb.ninst
    return nc, cx


MIXERS = {}


def host_weights(inputs, cx):
    arrs = {}
    for name in cx.wt.ents:
        if name.startswith('moe_'):
            kind, li = name[4:6], int(name.split('_')[-1])
            a = inputs['moe_' + kind][li]
            arrs[name] = a.reshape(-1, a.shape[-1])
        else:
            a = inputs[name][0]
            arrs[name] = a
    return cx.wt.host_shards(arrs)


def run(cfg, inputs, xs):
    nc, cx = build_program(cfg)
    shards = host_weights(inputs, cx)
    consts = make_consts()
    in_maps = []
    for c in range(cx.ncores):
        m = {'x': np.ascontiguousarray(xs[c]), 'wsh': shards}
        m.update(consts)
        for k in ('norm_mix', 'norm_ffn', 'moe_router', 'rel_bias'):
            m[k] = np.ascontiguousarray(inputs[k])
        for name in cfg.get('small_inputs', {}):
            m[name] = np.ascontiguousarray(inputs[name]).reshape(cfg['small_inputs'][name])
        in_maps.append(m)
    res = run_bass_kernel_spmd(nc, in_maps, core_ids=list(range(cx.ncores)))
    return [r['out'] for r in res.results]


def proj_in(kb, cx, li, wname, groups):
    nc = cx.nc
    S = cx.S
    TT = 512
    with ExitStack() as st:
        sb = mk_sb(nc, st)
        bufs = {'h': [sb('p_h%d' % i, [128, D], F32) for i in range(2)],
                'xn': [sb('p_xn%d' % i, [128, D], BF16) for i in range(2)],
                'ss': [sb('p_ss%d' % i, [128, 4], F32) for i in range(2)],
                'junk': sb('p_junk', [128, D], BF16)}
        gain = sb('p_gain', [128, D], F32)
        hnT = sb('p_hnT', [128, 16, TT], BF16)
        wts = [sb('p_w%d' % i, [128, 16, 512], BF16) for i in range(2)]
        stg_b = [sb('p_sb%d' % i, [128, 512], BF16) for i in range(3)]
        stg_f = [sb('p_sf%d' % i, [128, 512], F32) for i in range(2)]
        kb.dma('sp', gain[:], rows_bcast(cx.dram['norm_mix'][li:li + 1, :]))
        nw = 0
        ns = 0
        for s0 in range(0, S, TT):
            for j in range(TT // 128):
                xn = norm_tile(kb, cx, bufs, cx.h[s0 + j * 128:s0 + (j + 1) * 128, :], gain, j)
                transpose_to(kb, cx, xn, hnT, j * 128)
            for (col0, ncols, mode, dst, scale) in groups:
                for c0 in range(0, ncols, 512):
                    cw = min(512, ncols - c0)
                    wt = wts[nw % 2]
                    nw += 1
                    kb.dma('sp', wt[:, :, 0:cw], cx.Wtile(wname, 0, 16, col0 + c0, cw), r=['wg'])
                    isb = (dst.dtype == BF16)
                    if mode == 'T':
                        for f0 in range(0, cw, 128):
                            fw = min(128, cw - f0)
                            ps = next_ps(cx)
                            for c in range(16):
                                kb.mm(ps[0:fw, :], wt[:, c, f0:f0 + fw], hnT[:, c, :], start=(c == 0), stop=(c == 15))
                            sg = (stg_b[ns % 3] if isb else stg_f[ns % 2])
                            ns += 1
                            kb.act(sg[0:fw, :], ps[0:fw, :], AF.Copy, scale=float(scale))
                            kb.dma(SQ, dst[c0 + f0:c0 + f0 + fw, s0:s0 + TT], sg[0:fw, :], w=[dst])
                    else:
                        for j in range(TT // 128):
                            ps = next_ps(cx)
                            for c in range(16):
                                kb.mm(ps[:, 0:cw], hnT[:, c, j * 128:(j + 1) * 128], wt[:, c, 0:cw], start=(c == 0), stop=(c == 15))
                            sg = (stg_b[ns % 3] if isb else stg_f[ns % 2])
                            ns += 1
                            if ns % 2:
                                kb.act(sg[:, 0:cw], ps[:, 0:cw], AF.Copy, scale=float(scale))
                            else:
                                kb.ts(sg[:, 0:cw], ps[:, 0:cw], float(scale), None, op0=ALU.mult)
                            kb.dma(SQ, dst[s0 + j * 128:s0 + (j + 1) * 128, c0:c0 + cw], sg[:, 0:cw], w=[dst])


def proj_out(kb, cx, wname, og, KD):
    nc = cx.nc
    S = cx.S
    KC = KD // 128
    TT = 512
    if getattr(cx, 'debug_og', False):
        with ExitStack() as st:
            sb = mk_sb(nc, st)
            a = [sb('dbg_a%d' % i, [128, 2048], BF16) for i in range(2)]
            b = [sb('dbg_b%d' % i, [128, 2048], F32) for i in range(2)]
            for t in range(S // 128):
                kb.dma('sp', a[t % 2][:], og[t * 128:(t + 1) * 128, 0:2048])
                kb.copy(b[t % 2][:], a[t % 2][:])
                kb.dma(SQ, cx.h[t * 128:(t + 1) * 128, :], b[t % 2][:], w=['h'])
        return
    with ExitStack() as st:
        sb = mk_sb(nc, st)
        ogt = [sb('o_og%d' % i, [128, KD], BF16) for i in range(2)]
        ogT = sb('o_ogT', [128, KC, TT], BF16)
        wts = [sb('o_w%d' % i, [128, KC, 512], BF16) for i in range(2)]
        hb = [sb('o_h%d' % i, [128, D], F32) for i in range(4)]
        nw = 0
        for s0 in range(0, S, TT):
            for j in range(4):
                o_ = ogt[j % 2]
                kb.dma('sp', o_[:], og[s0 + j * 128:s0 + (j + 1) * 128, :])
                transpose_to(kb, cx, o_, ogT, j * 128, nchunks=KC)
                kb.dma('sp', hb[j][:], cx.h[s0 + j * 128:s0 + (j + 1) * 128, :], r=['h'])
            for dc in range(4):
                wt = wts[nw % 2]
                nw += 1
                for c in range(0, KC, 16):
                    kb.dma('sp', wt[:, c:c + 16, :], cx.Wtile(wname, c * 128, 16, dc * 512, 512), r=['wg'])
                for j in range(4):
                    ps = next_ps(cx)
                    for c in range(KC):
                        kb.mm(ps[:, :], ogT[:, c, j * 128:(j + 1) * 128], wt[:, c, :], start=(c == 0), stop=(c == KC - 1))
                    kb.tt(hb[j][:, dc * 512:(dc + 1) * 512], hb[j][:, dc * 512:(dc + 1) * 512], ps[:, :], ALU.add)
            for j in range(4):
                kb.dma(SQ, cx.h[s0 + j * 128:s0 + (j + 1) * 128, :], hb[j][:], w=['h'])


def dscr(cx, name, shape, dt):
    if name not in cx.scr:
        cx.scr[name] = cx.nc.dram_tensor(name, list(shape), dt, kind="Internal").ap()
    return cx.scr[name]


def gla_mixer(kb, cx, li):
    nc = cx.nc
    S = cx.S
    NCH = S // 128
    qT = dscr(cx, 'gla_qT', [1024, S], BF16)
    kT = dscr(cx, 'gla_kT', [1024, S], BF16)
    vv = dscr(cx, 'gla_v', [S, 2048], BF16)
    rr = dscr(cx, 'gla_r', [S, 2048], BF16)
    gl = dscr(cx, 'gla_glo', [32, S], BF16)
    o1 = dscr(cx, 'gla_o1', [S, 2048], F32)
    og = dscr(cx, 'mix_og', [S, 4096], BF16)
    proj_in(kb, cx, li, 'gla_w_in', [(0, 1024, 'T', qT, 1.0 / 16.0), (1024, 1024, 'T', kT, 1.0),
                                     (2048, 2048, 'N', vv, 1.0), (4096, 2048, 'N', rr, 1.0),
                                     (6144, 32, 'T', gl, 1.0)])
    with ExitStack() as st:
        sb = mk_sb(nc, st)
        glo1 = sb('g_glo', [16, S], BF16)
        wup_f = sb('g_wupf', [16, 2, 1024], F32)
        wup = sb('g_wup', [16, 2, 1024], BF16)
        negb = sb('g_negb', [128, 2, 8], F32)
        hng = sb('g_hng', [128, 512], F32)
        mask = [sb('g_mask%d' % d, [128, 128], F32) for d in range(2)]
        qh = sb('g_q', [128, 2, S], BF16)
        kh = sb('g_k', [128, 2, S], BF16)
        qd = sb('g_qd', [128, 2, S], BF16)
        ki = sb('g_ki', [128, 2, S], BF16)
        kstT = sb('g_kstT', [128, S], BF16)
        kst = sb('g_kst', [128, NCH, 256], BF16)
        CSx = sb('g_cs', [128, S + 1], F32)
        T1 = sb('g_t1', [128, S], F32)
        T2 = sb('g_t2', [128, S], F32)
        T3 = sb('g_t3', [128, S], F32)
        dec = sb('g_dec', [128, 2, NCH], F32)
        Sf = sb('g_Sf', [128, 2, 512], F32)
        Sb = sb('g_Sb', [128, 2, 512], BF16)
        vt = [sb('g_v%d' % i, [128, 512], BF16) for i in range(2)]
        rt = [sb('g_r%d' % i, [128, 512], BF16) for i in range(2)]
        AT = [sb('g_AT%d' % i, [128, 128], BF16) for i in range(2)]
        of = [sb('g_of%d' % i, [128, 512], F32) for i in range(2)]
        op_ = [sb('g_op%d' % i, [128, 512], F32) for i in range(2)]
        sr = [sb('g_sr%d' % i, [128, 512], F32) for i in range(2)]
        ob = [sb('g_ob%d' % i, [128, 512], BF16) for i in range(2)]
        ss = [sb('g_ss%d' % i, [128, 4], F32) for i in range(2)]
        junk = sb('g_junk', [128, 512], BF16)
        kb.dma('sp', wup_f[:], cx.dram['gla_w_gate_up'].rearrange("d r e -> r d e"))
        kb.copy(wup[:], wup_f[:])
        for d in range(2):
            kb.dma('sp', negb[:, d, :], cx.dram['gla_b_gate'][d].rearrange("(c p) -> p c", p=128), allow_slow_non_contiguous=True)
        kb.ts(negb[:], negb[:], -1.0, None, op0=ALU.mult)
        kb.dma('sp', hng[:], rows_bcast(cx.dram['gla_head_norm'][0:1, :]))
        kb.dma('sp', mask[0][:], cx.dram['c_masku'][:, :])
        kb.dma('sp', mask[1][:], cx.dram['c_maskl'][:, :])
        kb.memset(CSx[:, 0:1], 0.0)
        ch = lambda t: t.rearrange("p (n c) -> p n c", c=128)
        for hh in range(4):
            kb.dma('sp', qh[:], qT[hh * 256:(hh + 1) * 256, :].rearrange("(c p) s -> p c s", p=128))
            kb.dma('sp', kh[:], kT[hh * 256:(hh + 1) * 256, :].rearrange("(c p) s -> p c s", p=128))
            for d in range(2):
                kb.dma('sp', glo1[:], gl[d * 16:(d + 1) * 16, :])
                for fc in range(2):
                    f0 = hh * 256 + fc * 128
                    for b0 in range(0, S, 512):
                        ps = next_ps(cx)
                        kb.mm(ps[:, :], wup[:, d, f0:f0 + 128], glo1[:, b0:b0 + 512])
                        kb.act(T1[:, b0:b0 + 512], ps[:, :], AF.Exp, bias=negb[:, d, hh * 2 + fc:hh * 2 + fc + 1], scale=-1.0)
                    kb.act(T1[:], T1[:], AF.Ln, bias=1.0, scale=1.0)
                    kb.op('dve', lambda E: E.tensor_tensor_scan(CSx[:, 1:S + 1], T1[:], T1[:], 0.0, ALU.add, ALU.max),
                          w=[CSx], r=[T1])
                    if d == 0:
                        kb.tt(ch(T2[:]), ch(CSx[:, 1:S + 1]), ch(CSx[:, 0:S])[:, :, 0:1].broadcast_to([128, NCH, 128]), ALU.subtract)
                        li_ = 127
                    else:
                        kb.tt(ch(T2[:]), ch(CSx[:, 1:S + 1])[:, :, 127:128].broadcast_to([128, NCH, 128]), ch(CSx[:, 0:S]), ALU.subtract)
                        li_ = 0
                    kb.act(T3[:], T2[:], AF.Exp, scale=-1.0 / 16.0)
                    kb.act(CSx[:, 1:S + 1], T2[:], AF.Exp, scale=1.0 / 16.0)
                    kb.tt(qd[:, fc, :], qh[:, fc, :], T3[:], ALU.mult)
                    kb.tt(T1[:], kh[:, fc, :], CSx[:, 1:S + 1], ALU.mult)
                    kb.copy(ki[:, fc, :], T1[:], e='pool')
                    kb.tt(ch(kstT[:]), ch(T1[:]), ch(T3[:])[:, :, li_:li_ + 1].broadcast_to([128, NCH, 128]), ALU.mult)
                    kb.copy(dec[:, fc, :], ch(T3[:])[:, :, li_])
                    for n0 in range(0, NCH, 8):
                        pb = next_pb(cx)
                        for n in range(8):
                            kb.tr(pb[:, n * 128:(n + 1) * 128], kstT[:, (n0 + n) * 128:(n0 + n + 1) * 128], cx.ident_b[:])
                        kb.copy(kst[:, n0:n0 + 8, fc * 128:(fc + 1) * 128], pb[:, :].rearrange("p (n t) -> p n t", t=128), e='act')
                kb.memset(Sf[:], 0.0)
                kb.memset(Sb[:], 0.0)
                order = list(range(NCH)) if d == 0 else list(range(NCH - 1, -1, -1))
                for it, n in enumerate(order):
                    tk = slice(n * 128, (n + 1) * 128)
                    v_ = vt[it % 2]
                    kb.dma('sp', v_[:], vv[tk, hh * 512:(hh + 1) * 512])
                    ps1 = next_ps(cx)
                    for fc in range(2):
                        kb.mm(ps1[:, 0:128], ki[:, fc, tk], qd[:, fc, tk], start=(fc == 0), stop=(fc == 1))
                    a_ = AT[it % 2]
                    kb.tt(a_[:], ps1[:, 0:128], mask[d][:], ALU.mult)
                    ps2 = next_ps(cx)
                    kb.mm(ps2[:, :], a_[:], v_[:], start=True, stop=False)
                    for fc in range(2):
                        kb.mm(ps2[:, :], qd[:, fc, tk], Sb[:, fc, :], start=False, stop=(fc == 1))
                    if d == 0:
                        o_ = of[it % 2]
                        kb.copy(o_[:], ps2[:, :], e='act')
                        kb.dma(SQ, o1[tk, hh * 512:(hh + 1) * 512], o_[:], w=[o1])
                    else:
                        p_ = op_[it % 2]
                        r_ = rt[it % 2]
                        kb.dma('sp', p_[:], o1[tk, hh * 512:(hh + 1) * 512])
                        kb.dma('sp', r_[:], rr[tk, hh * 512:(hh + 1) * 512])
                        o_ = of[it % 2]
                        kb.tt(o_[:], ps2[:, :], p_[:], ALU.add)
                        s_ = ss[it % 2]
                        kb.act(junk[:], o_[:], AF.Square, accum=s_[:, 0:1])
                        kb.act(s_[:, 1:2], s_[:, 0:1], AF.Sqrt, bias=cx.eps_t[:, 0:1], scale=1.0 / 512)
                        kb.op('dve', lambda E, s_=s_: E.reciprocal(s_[:, 2:3], s_[:, 1:2]), w=[s_], r=[s_])
                        kb.stt(o_[:], o_[:], s_[:, 2:3], hng[:], ALU.mult, ALU.mult)
                        sr_ = sr[it % 2]
                        kb.act(sr_[:], r_[:], AF.Silu)
                        b_ = ob[it % 2]
                        kb.tt(b_[:], o_[:], sr_[:], ALU.mult)
                        kb.dma(SQ, og[tk, hh * 512:(hh + 1) * 512], b_[:], w=[og])
                    for fc in range(2):
                        ps3 = next_ps(cx)
                        kb.mm(ps3[:, :], kst[:, n, fc * 128:(fc + 1) * 128], v_[:])
                        kb.stt(Sf[:, fc, :], Sf[:, fc, :], dec[:, fc, n:n + 1], ps3[:, :], ALU.mult, ALU.add)
                        kb.copy(Sb[:, fc, :], Sf[:, fc, :], e='act')
    proj_out(kb, cx, 'gla_w_out', og[:, 0:2048], 2048)


MIXERS[0] = gla_mixer


def t5_bucket_np(rel):
    import math
    n = np.abs(rel)
    lr = np.log(np.maximum(n, 1).astype(np.float32) / np.float32(8)) / np.float32(math.log(128 / 8))
    large = np.minimum(8 + (lr * np.float32(8)).astype(np.int32), 15)
    return np.where(rel > 0, 16, 0) + np.where(n < 8, n, large)


def attn_consts():
    kp = np.arange(128)[:, None, None]
    dl = np.arange(-1, 2)[None, :, None]
    qf = np.arange(128)[None, None, :]
    rel = dl * 128 + kp - qf
    bk = t5_bucket_np(rel).astype(np.float32)
    mneg = np.where(np.abs(rel) <= 128, 0.0, -30000.0).astype(np.float32)
    return {'c_bk': np.ascontiguousarray(bk.reshape(128, 384)), 'c_mneg': np.ascontiguousarray(mneg.reshape(128, 384))}


def build_bias_tiles(kb, cx, st, swa):
    nc = cx.nc
    sb0 = mk_sb(nc, st)
    T = sb0('a_T', [128, 16, 384], F32)
    cfar = sb0('a_cfar', [128, 32], F32)
    with ExitStack() as s2:
        sb = mk_sb(nc, s2)
        bk = sb('a_bk', [128, 384], F32)
        mk = sb('a_mk', [128, 32, 384], BF16)
        tb = sb('a_tb', [128, 512], F32)
        mn = sb('a_mn', [128, 384], F32)
        kb.dma('sp', bk[:], cx.dram['c_bk'][:, :])
        kb.dma('sp', mn[:], cx.dram['c_mneg'][:, :])
        kb.dma('sp', tb[:], cx.dram['rel_bias'].rearrange("b h -> (b h)").rearrange("(o n) -> o n", o=1).broadcast_to([128, 512]))
        for b in range(32):
            kb.ts(mk[:, b, :], bk[:], float(b), None, op0=ALU.is_equal)
        for h in range(16):
            if swa:
                kb.copy(T[:, h, :], mn[:])
            else:
                kb.memset(T[:, h, :], 0.0)
            for b in range(32):
                kb.stt(T[:, h, :], mk[:, b, :], tb[:, b * 16 + h:b * 16 + h + 1], T[:, h, :], ALU.mult, ALU.add)
            kb.copy(cfar[:, h * 2:h * 2 + 1], tb[:, 15 * 16 + h:15 * 16 + h + 1])
            kb.copy(cfar[:, h * 2 + 1:h * 2 + 2], tb[:, 31 * 16 + h:31 * 16 + h + 1])
    return T, cfar


def qknorm_pass(kb, cx, src, ncols, G, gain_ap, scale, dstT):
    nc = cx.nc
    S = cx.S
    NG = ncols // G
    NCk = ncols // 128
    with ExitStack() as st:
        sb = mk_sb(nc, st)
        xt = [sb('n_x%d' % i, [128, ncols], F32) for i in range(2)]
        sq = sb('n_sq', [128, ncols], F32)
        xb = [sb('n_xb%d' % i, [128, ncols], BF16) for i in range(2)]
        ss = [sb('n_ss%d' % i, [128, 3, NG], F32) for i in range(2)]
        gn = sb('n_g', [128, G], F32)
        stg = [sb('n_st%d' % i, [128, NCk, 128], BF16) for i in range(2)]
        kb.dma('sp', gn[:], gain_ap.broadcast_to([128, G]))
        kb.ts(gn[:], gn[:], float(scale), None, op0=ALU.mult)
        g3 = lambda t: t.rearrange("p (g d) -> p g d", d=G)
        for t in range(S // 128):
            x_ = xt[t % 2]
            s_ = ss[t % 2]
            b_ = xb[t % 2]
            kb.dma('sp', x_[:], src[t * 128:(t + 1) * 128, 0:ncols])
            kb.tt(sq[:], x_[:], x_[:], ALU.mult)
            kb.op('dve', lambda E, s_=s_: E.tensor_reduce(s_[:, 0, :], g3(sq[:]), AX.X, ALU.add), w=[s_], r=[sq])
            kb.act(s_[:, 1, :], s_[:, 0, :], AF.Sqrt, bias=cx.eps_t[:, 0:1], scale=1.0 / G)
            kb.op('dve', lambda E, s_=s_: E.reciprocal(s_[:, 2, :], s_[:, 1, :]), w=[s_], r=[s_])
            kb.tt(g3(sq[:]), g3(x_[:]), s_[:, 2, :].rearrange("p (g o) -> p g o", o=1).broadcast_to([128, NG, G]), ALU.mult)
            kb.tt(g3(b_[:]), g3(sq[:]), gn[:].rearrange("p (o d) -> p o d", o=1).broadcast_to([128, NG, G]), ALU.mult)
            sg = stg[t % 2]
            transpose_to(kb, cx, b_, sg, 0, nchunks=NCk)
            kb.dma(SQ, dstT[0:ncols, :].rearrange("(c p) s -> p c s", p=128)[:, :, t * 128:(t + 1) * 128], sg[:], w=[dstT])


def acc_slots(cx):
    return [(cx.acc[i], 0, True) for i in range(4)]


def diff_mixer(kb, cx, li):
    import math
    nc = cx.nc
    S = cx.S
    NCH = S // 128
    NQT = S // 512
    lam_init = 0.8 - 0.6 * math.exp(-0.3 * li)
    qf = dscr(cx, 'at_q', [S, 2048], F32)
    kf = dscr(cx, 'at_k', [S, 2048], F32)
    vv = dscr(cx, 'at_v', [S, 2048], BF16)
    qT = dscr(cx, 'at_qT', [2048, S], BF16)
    kT = dscr(cx, 'at_kT', [2048, S], BF16)
    og = dscr(cx, 'mix_og', [S, 4096], BF16)
    proj_in(kb, cx, li, 'diff_w_in', [(0, 2048, 'N', qf, 1.0), (2048, 2048, 'N', kf, 1.0), (4096, 2048, 'N', vv, 1.0)])
    qknorm_pass(kb, cx, qf, 2048, 64, cx.dram['diff_q_norm'][0:1, :], 0.125, qT)
    qknorm_pass(kb, cx, kf, 2048, 64, cx.dram['diff_k_norm'][0:1, :], 1.0, kT)
    with ExitStack() as st:
        sb = mk_sb(nc, st)
        T, cfar = build_bias_tiles(kb, cx, st, swa=False)
        lam = sb('d_lam', [128, 4, 64], F32)
        lw = sb('d_lw', [128, 8], F32)
        sub = sb('d_sub', [128, 128], F32)
        qh = sb('d_q', [128, S], BF16)
        kh = sb('d_k', [128, S], BF16)
        v1 = sb('d_v1', [128, NCH, 130], BF16)
        tmp = [sb('d_tmp%d' % i, [128, 512], F32) for i in range(2)]
        PT = [sb('d_PT%d' % i, [128, 512], BF16) for i in range(3)]
        res = [sb('d_res%d' % i, [128, 4, 130], F32) for i in range(2)]
        rc = [sb('d_rc%d' % i, [128, 8], F32) for i in range(2)]
        ot = [sb('d_ot%d' % i, [128, 128], F32) for i in range(2)]
        ob = [sb('d_ob%d' % i, [128, 128], BF16) for i in range(2)]
        junk = sb('d_junk', [128, 128], BF16)
        kb.dma('sp', lam[:], cx.dram['diff_lambda'].rearrange("a d -> (a d)").rearrange("(o n) -> o n", o=1).broadcast_to([128, 256]))
        kb.tt(lam[:, 0, :], lam[:, 0, :], lam[:, 1, :], ALU.mult)
        kb.tt(lam[:, 2, :], lam[:, 2, :], lam[:, 3, :], ALU.mult)
        kb.op('dve', lambda E: E.reduce_sum(lw[:, 0:1], lam[:, 0, :], AX.X), w=[lw], r=[lam])
        kb.op('dve', lambda E: E.reduce_sum(lw[:, 1:2], lam[:, 2, :], AX.X), w=[lw], r=[lam])
        kb.act(lw[:, 2:4], lw[:, 0:2], AF.Exp)
        kb.tt(lw[:, 4:5], lw[:, 3:4], lw[:, 2:3], ALU.subtract)
        kb.ts(lw[:, 5:6], lw[:, 4:5], -lam_init, None, op0=ALU.add)
        kb.dma('sp', sub[:], cx.dram['diff_subln'][0:1, :].broadcast_to([128, 128]))
        kb.ts(sub[:], sub[:], float(1.0 - lam_init), None, op0=ALU.mult)
        kb.memset(v1[:], 1.0)
        slots = acc_slots(cx)
        npt = 0
        for h in range(16):
            kb.dma('sp', qh[:], qT[h * 128:(h + 1) * 128, :])
            kb.dma('sp', kh[:], kT[h * 128:(h + 1) * 128, :])
            kb.dma('sp', v1[:, :, 0:128], vv[:, h * 128:(h + 1) * 128].rearrange("(n p) d -> p n d", p=128))
            for qt in range(NQT):
                for m in range(2):
                    pr = slice(m * 64, (m + 1) * 64)
                    def qk(kc):
                        ps = next_ps(cx)
                        kb.mm(ps[:, :], kh[pr, kc * 128:(kc + 1) * 128], qh[pr, qt * 512:(qt + 1) * 512])
                        return ps
                    ps_next = qk(0)
                    for kc in range(NCH):
                        ps = ps_next
                        if kc + 1 < NCH:
                            ps_next = qk(kc + 1)
                        p_ = PT[npt % 3]
                        npt += 1
                        dls = [kc - (qt * 4 + qb) for qb in range(4)]
                        if all(abs(dl) >= 2 for dl in dls):
                            sgn = 1 if dls[0] > 0 else 0
                            kb.act(p_[:], ps[:, :], AF.Exp, bias=cfar[:, h * 2 + sgn:h * 2 + sgn + 1], scale=1.0)
                        else:
                            t_ = tmp[npt % 2]
                            for qb, dl in enumerate(dls):
                                blk = slice(qb * 128, (qb + 1) * 128)
                                if abs(dl) <= 1:
                                    kb.tt(t_[:, blk], ps[:, blk], T[:, h, (dl + 1) * 128:(dl + 2) * 128], ALU.add)
                                else:
                                    sgn = 1 if dl > 0 else 0
                                    kb.ts(t_[:, blk], ps[:, blk], cfar[:, h * 2 + sgn:h * 2 + sgn + 1], None, op0=ALU.add)
                            kb.act(p_[:], t_[:], AF.Exp)
                        for qb in range(4):
                            bank, c0, first = slots[qb]
                            kb.mm(bank[:, c0:c0 + 130], p_[:, qb * 128:(qb + 1) * 128], v1[:, kc, :],
                                  start=(kc == 0 and first), stop=(kc == NCH - 1), skip_group_check=True)
                    r_ = res[m]
                    for qb in range(4):
                        bank, c0, first = slots[qb]
                        kb.copy(r_[:, qb, :], bank[:, c0:c0 + 130], e='act' if qb % 2 else 'dve')
                for qb in range(4):
                    c_ = rc[qb % 2]
                    o_ = ot[qb % 2]
                    kb.op('dve', lambda E, c_=c_, qb=qb: E.reciprocal(c_[:, 0:1], res[0][:, qb, 128:129]), w=[c_], r=[res[0]])
                    kb.op('dve', lambda E, c_=c_, qb=qb: E.reciprocal(c_[:, 1:2], res[1][:, qb, 128:129]), w=[c_], r=[res[1]])
                    kb.tt(c_[:, 2:3], c_[:, 1:2], lw[:, 5:6], ALU.mult)
                    kb.ts(o_[:], res[0][:, qb, 0:128], c_[:, 0:1], None, op0=ALU.mult)
                    kb.stt(o_[:], res[1][:, qb, 0:128], c_[:, 2:3], o_[:], ALU.mult, ALU.add)
                    kb.act(junk[:], o_[:], AF.Square, accum=c_[:, 3:4])
                    kb.act(c_[:, 4:5], c_[:, 3:4], AF.Sqrt, bias=cx.eps_t[:, 0:1], scale=1.0 / 128)
                    kb.op('dve', lambda E, c_=c_: E.reciprocal(c_[:, 5:6], c_[:, 4:5]), w=[c_], r=[c_])
                    b_ = ob[qb % 2]
                    kb.stt(b_[:], o_[:], c_[:, 5:6], sub[:], ALU.mult, ALU.mult)
                    t0 = (qt * 4 + qb) * 128
                    kb.dma(SQ, og[t0:t0 + 128, h * 128:(h + 1) * 128], b_[:], w=[og])
    proj_out(kb, cx, 'diff_w_out', og[:, 0:2048], 2048)


def swa_mixer(kb, cx, li):
    nc = cx.nc
    S = cx.S
    NCH = S // 128
    qf = dscr(cx, 'at_q', [S, 2048], F32)
    kf = dscr(cx, 'at_k', [S, 2048], F32)
    vv = dscr(cx, 'at_v', [S, 2048], BF16)
    qT = dscr(cx, 'at_qT', [2048, S], BF16)
    kT = dscr(cx, 'at_kT', [2048, S], BF16)
    og = dscr(cx, 'mix_og', [S, 4096], BF16)
    proj_in(kb, cx, li, 'swa_w_in', [(0, 2048, 'N', qf, 1.0), (2048, 512, 'N', kf, 1.0), (2560, 512, 'N', vv, 1.0)])
    qknorm_pass(kb, cx, qf, 2048, 128, cx.dram['swa_q_norm'][0:1, :], 128 ** -0.5, qT)
    qknorm_pass(kb, cx, kf, 512, 128, cx.dram['swa_k_norm'][0:1, :], 1.0, kT)
    with ExitStack() as st:
        sb = mk_sb(nc, st)
        T, cfar = build_bias_tiles(kb, cx, st, swa=True)
        snk = sb('w_snk', [128, 16], F32)
        q4 = sb('w_q4', [128, 4, S], BF16)
        kg = sb('w_kg', [128, S], BF16)
        v1 = sb('w_v1', [128, NCH, 130], BF16)
        tmp = [sb('w_tmp%d' % i, [128, 512], F32) for i in range(2)]
        PT = [sb('w_PT%d' % i, [128, 512], BF16) for i in range(3)]
        rc = [sb('w_rc%d' % i, [128, 4], F32) for i in range(2)]
        ob = [sb('w_ob%d' % i, [128, 128], BF16) for i in range(2)]
        kb.dma('sp', snk[:], cx.dram['swa_sink'][0:1, :].broadcast_to([128, 16]))
        kb.act(snk[:], snk[:], AF.Exp)
        kb.memset(v1[:], 1.0)
        slots = acc_slots(cx)
        npt = 0
        for g in range(4):
            kb.dma('sp', q4[:], qT[g * 512:(g + 1) * 512, :].rearrange("(c p) s -> p c s", p=128))
            kb.dma('sp', kg[:], kT[g * 128:(g + 1) * 128, :])
            kb.dma('sp', v1[:, :, 0:128], vv[:, g * 128:(g + 1) * 128].rearrange("(n p) d -> p n d", p=128))
            for qb in range(NCH):
                kcs = [kc for kc in (qb - 1, qb, qb + 1) if 0 <= kc < NCH]
                for ik, kc in enumerate(kcs):
                    dl = kc - qb
                    ps = next_ps(cx)
                    kb.mm(ps[:, :], kg[:, kc * 128:(kc + 1) * 128], q4[:, :, qb * 128:(qb + 1) * 128])
                    t_ = tmp[npt % 2]
                    p_ = PT[npt % 3]
                    npt += 1
                    for hh in range(4):
                        blk = slice(hh * 128, (hh + 1) * 128)
                        kb.tt(t_[:, blk], ps[:, blk], T[:, g * 4 + hh, (dl + 1) * 128:(dl + 2) * 128], ALU.add)
                    kb.act(p_[:], t_[:], AF.Exp)
                    for hh in range(4):
                        bank, c0, first = slots[hh]
                        kb.mm(bank[:, c0:c0 + 130], p_[:, hh * 128:(hh + 1) * 128], v1[:, kc, :],
                              start=(ik == 0 and first), stop=(ik == len(kcs) - 1), skip_group_check=True)
                for hh in range(4):
                    bank, c0, first = slots[hh]
                    h = g * 4 + hh
                    c_ = rc[hh % 2]
                    kb.tt(c_[:, 0:1], bank[:, c0 + 128:c0 + 129], snk[:, h:h + 1], ALU.add)
                    kb.op('dve', lambda E, c_=c_: E.reciprocal(c_[:, 1:2], c_[:, 0:1]), w=[c_], r=[c_])
                    b_ = ob[hh % 2]
                    kb.ts(b_[:], bank[:, c0:c0 + 128], c_[:, 1:2], None, op0=ALU.mult)
                    kb.dma(SQ, og[qb * 128:(qb + 1) * 128, h * 128:(h + 1) * 128], b_[:], w=[og])
    proj_out(kb, cx, 'swa_w_out', og[:, 0:2048], 2048)


MIXERS[2] = diff_mixer
MIXERS[3] = swa_mixer


def gdn_mixer(kb, cx, li):
    nc = cx.nc
    S = cx.S
    NCH = S // 128
    preT = dscr(cx, 'gd_preT', [8192, S], BF16)
    qkT = dscr(cx, 'gd_qkT', [4096, S], BF16)
    vT = dscr(cx, 'gd_vT', [4096, S], BF16)
    zz = dscr(cx, 'gd_z', [S, 4096], BF16)
    abt = dscr(cx, 'gd_ab', [S, 128], F32)
    o1 = dscr(cx, 'gd_o1', [S, 4096], F32)
    og = dscr(cx, 'mix_og', [S, 4096], BF16)
    proj_in(kb, cx, li, 'gdn_w_in', [(0, 8192, 'T', preT, 1.0), (8192, 4096, 'N', zz, 1.0), (12288, 128, 'N', abt, 1.0)])
    with ExitStack() as st:
        sb = mk_sb(nc, st)
        cwl = [sb('c_w%d' % i, [128, 5], F32) for i in range(2)]
        xp = [sb('c_xp%d' % i, [128, S + 4], BF16) for i in range(2)]
        y = sb('c_y', [128, S], F32)
        ys = sb('c_ys', [128, S], F32)
        sq = sb('c_sq', [128, S], BF16)
        rs = sb('c_rs', [128, 512], F32)
        ob = [sb('c_ob%d' % i, [128, S], BF16) for i in range(2)]
        onesb = sb('c_ones', [128, 128], BF16)
        kb.memset(onesb[:], 1.0)
        for i in range(2):
            kb.memset(xp[i][:, 0:2], 0.0)
            kb.memset(xp[i][:, S + 2:S + 4], 0.0)
        for c in range(64):
            x_ = xp[c % 2]
            kb.dma('sp', x_[:, 2:S + 2], preT[c * 128:(c + 1) * 128, :])
            cw = cwl[c % 2]
            kb.dma('sp', cw[:], cx.dram['gdn_conv'][:, c * 128:(c + 1) * 128].rearrange("j p -> p j"), allow_slow_non_contiguous=True)
            kb.ts(y[:], x_[:, 0:S], cw[:, 0:1], None, op0=ALU.mult)
            for j in range(1, 5):
                kb.stt(y[:], x_[:, j:j + S], cw[:, j:j + 1], y[:], ALU.mult, ALU.add)
            kb.act(ys[:], y[:], AF.Silu)
            o_ = ob[c % 2]
            if c < 32:
                kb.tt(sq[:], ys[:], ys[:], ALU.mult, e='pool')
                for b0 in range(0, S, 512):
                    ps = next_ps(cx)
                    kb.mm(ps[:, :], onesb[:], sq[:, b0:b0 + 512])
                    kb.act(rs[:], ps[:, :], AF.Sqrt, bias=cx.eps_t[:, 0:1], scale=1.0)
                    kb.op('dve', lambda E: E.reciprocal(rs[:], rs[:]), w=[rs], r=[rs])
                    if c < 16:
                        kb.stt(o_[:, b0:b0 + 512], ys[:, b0:b0 + 512], 128 ** -0.5, rs[:], ALU.mult, ALU.mult)
                    else:
                        kb.tt(o_[:, b0:b0 + 512], ys[:, b0:b0 + 512], rs[:], ALU.mult)
                kb.dma(SQ, qkT[c * 128:(c + 1) * 128, :], o_[:], w=[qkT])
            else:
                kb.copy(o_[:], ys[:], e='pool')
                kb.dma(SQ, vT[(c - 32) * 128:(c - 31) * 128, :], o_[:], w=[vT])
    with ExitStack() as st:
        sb = mk_sb(nc, st)
        psl = cx.ps + cx.acc
        pidx = [0]

        def nps():
            t = psl[pidx[0] % len(psl)]
            pidx[0] += 1
            return t
        g_all = sb('s_g', [128, NCH, 64], F32)
        nb_all = sb('s_nb', [128, NCH, 64], F32)
        gc_all = sb('s_gc', [128, NCH, 64], F32)
        ngc_all = sb('s_ngc', [128, NCH, 64], F32)
        alog = sb('s_alog', [128, 64], F32)
        dtb = sb('s_dtb', [128, 64], F32)
        hng = sb('s_hng', [128, 128], F32)
        UT = sb('s_ut', [128, 128], F32)
        LT = sb('s_lt', [128, 128], F32)
        MSU = sb('s_msu', [128, 128], F32)
        MSL = sb('s_msl', [128, 128], F32)
        abl = [sb('s_ab%d' % i, [128, 128], F32) for i in range(2)]
        tmpa = sb('s_tmpa', [128, 64], F32)
        kb.dma('sp', UT[:], cx.dram['c_masku'][:, :])
        kb.dma('sp', LT[:], cx.dram['c_maskl'][:, :])
        kb.tt(MSU[:], UT[:], cx.ident_f[:], ALU.subtract)
        kb.tt(MSL[:], LT[:], cx.ident_f[:], ALU.subtract)
        kb.dma('sp', alog[:], cx.dram['gdn_a_log'].rearrange("d h -> (d h)").rearrange("(o n) -> o n", o=1).broadcast_to([128, 64]))
        kb.dma('sp', dtb[:], cx.dram['gdn_dt_bias'].rearrange("d h -> (d h)").rearrange("(o n) -> o n", o=1).broadcast_to([128, 64]))
        kb.dma('sp', hng[:], cx.dram['gdn_head_norm'][0:1, :].broadcast_to([128, 128]))
        kb.act(alog[:], alog[:], AF.Exp)
        kb.ts(alog[:], alog[:], -1.0, None, op0=ALU.mult)
        for n in range(NCH):
            a_ = abl[n % 2]
            kb.dma('sp', a_[:], abt[n * 128:(n + 1) * 128, :])
            kb.tt(tmpa[:], a_[:, 0:64], dtb[:], ALU.add)
            kb.act(tmpa[:], tmpa[:], AF.Exp)
            kb.act(tmpa[:], tmpa[:], AF.Ln, bias=1.0, scale=1.0)
            kb.tt(g_all[:, n, :], tmpa[:], alog[:], ALU.mult)
            kb.act(tmpa[:], a_[:, 64:128], AF.Exp, scale=-1.0)
            kb.ts(tmpa[:], tmpa[:], 1.0, None, op0=ALU.add)
            kb.op('dve', lambda E: E.reciprocal(tmpa[:], tmpa[:]), w=[tmpa], r=[tmpa])
            kb.ts(nb_all[:, n, :], tmpa[:], -1.0, None, op0=ALU.mult)
            ps = nps()
            kb.mm(ps[:, 0:32], UT[:], g_all[:, n, 0:32])
            kb.mm(ps[:, 32:64], LT[:], g_all[:, n, 32:64])
            kb.copy(gc_all[:, n, :], ps[:, 0:64])
            kb.ts(ngc_all[:, n, :], gc_all[:, n, :], -1.0, None, op0=ALU.mult, e='dve')
        qh = sb('s_q', [128, S], BF16)
        kh = sb('s_k', [128, S], BF16)
        ktok = sb('s_ktok', [128, NCH, 128], BF16)
        vth = sb('s_vth', [128, S], BF16)
        vtok = [sb('s_vtok%d' % i, [128, NCH, 128], BF16) for i in range(2)]
        bv = [sb('s_bv%d' % i, [128, NCH, 128], BF16) for i in range(4)]
        Sf = [sb('s_Sf%d' % i, [128, 128], F32) for i in range(4)]
        Sb = [sb('s_Sb%d' % i, [128, 128], BF16) for i in range(4)]
        KKs = [sb('s_kk%d' % i, [128, 128], F32) for i in range(2)]
        QKr = [sb('s_qkr%d' % i, [128, 128], F32) for i in range(2)]
        R = 4
        mkf = lambda nm: [sb('s_%s%d' % (nm, i), [128, 128], F32) for i in range(R)]
        mkb = lambda nm: [sb('s_%s%d' % (nm, i), [128, 128], BF16) for i in range(R)]
        gB, gR, GT, GG, Er = mkf('gB'), mkf('gR'), mkf('GT'), mkf('GG'), mkf('Er')
        Xa, Xb_, Ya, Yb_, Pa = mkf('Xa'), mkf('Xb'), mkf('Ya'), mkf('Yb'), mkf('Pa')
        QKT, MT, kdT, qdT, kst, Xv, vnb = mkb('QKT'), mkb('MT'), mkb('kdT'), mkb('qdT'), mkb('kst'), mkb('Xv'), mkb('vnb')
        sc = [sb('s_sc%d' % i, [128, 8], F32) for i in range(R)]
        of = [sb('s_of%d' % i, [128, 128], F32) for i in range(2)]
        op_ = [sb('s_op%d' % i, [128, 128], F32) for i in range(2)]
        zt = [sb('s_z%d' % i, [128, 128], BF16) for i in range(2)]
        zs = [sb('s_zs%d' % i, [128, 128], F32) for i in range(2)]
        obf = [sb('s_obf%d' % i, [128, 128], BF16) for i in range(2)]
        junk = sb('s_junk', [128, 128], BF16)
        rot = [0]
        for hq in range(16):
            kb.dma('sp', qh[:], qkT[hq * 128:(hq + 1) * 128, :])
            kb.dma('sp', kh[:], qkT[2048 + hq * 128:2048 + (hq + 1) * 128, :])
            for n0 in range(0, NCH, 8):
                pb = next_pb(cx)
                for n in range(8):
                    kb.tr(pb[:, n * 128:(n + 1) * 128], kh[:, (n0 + n) * 128:(n0 + n + 1) * 128], cx.ident_b[:])
                kb.copy(ktok[:, n0:n0 + 8, :], pb[:, :].rearrange("p (n t) -> p n t", t=128), e='act')
            chains = []
            for v2 in range(2):
                hv = hq * 2 + v2
                kb.dma('sp', vth[:], vT[hv * 128:(hv + 1) * 128, :])
                for n0 in range(0, NCH, 8):
                    pb = next_pb(cx)
                    for n in range(8):
                        kb.tr(pb[:, n * 128:(n + 1) * 128], vth[:, (n0 + n) * 128:(n0 + n + 1) * 128], cx.ident_b[:])
                    kb.copy(vtok[v2][:, n0:n0 + 8, :], pb[:, :].rearrange("p (n t) -> p n t", t=128), e='act')
                for d in range(2):
                    ci = v2 * 2 + d
                    col = d * 32 + hv
                    kb.tt(bv[ci][:], vtok[v2][:], nb_all[:, :, col:col + 1].broadcast_to([128, NCH, 128]), ALU.mult)
                    kb.ts(bv[ci][:], bv[ci][:], -1.0, None, op0=ALU.mult, e='pool')
                    kb.memset(Sf[ci][:], 0.0)
                    kb.memset(Sb[ci][:], 0.0)
                    chains.append((ci, v2, hv, d, col))
            done = {}
            for s_ in range(NCH):
                raw = {}
                for d, n in ((0, s_), (1, NCH - 1 - s_)):
                    tk = slice(n * 128, (n + 1) * 128)
                    ps = nps()
                    kb.mm(ps[:, 0:128], kh[:, tk], kh[:, tk])
                    kb.mm(ps[:, 128:256], kh[:, tk], qh[:, tk])
                    kb.tt(KKs[d][:], ps[:, 0:128], (MSL if d == 0 else MSU)[:], ALU.mult)
                    kb.copy(QKr[d][:], ps[:, 128:256], e='dve')
                    raw[d] = n
                stt_ = {}
                for (ci, v2, hv, d, col) in chains:
                    n = raw[d]
                    r = ci
                    li_ = 127 if d == 0 else 0
                    gcol = gc_all[:, n, col:col + 1]
                    ngcol = ngc_all[:, n, col:col + 1]
                    nbcol = nb_all[:, n, col:col + 1]
                    kb.copy(gB[r][:], g_all[:, n, col:col + 1].broadcast_to([128, 128]), e='dve')
                    pg = nps()
                    kb.mm(pg[:, 0:128], gB[r][:], (UT if d == 0 else LT)[:])
                    kb.copy(gR[r][:], pg[:, 0:128], e='dve')
                    kb.ts(GT[r][:], gR[r][:], ngcol, 0.0, op0=ALU.add, op1=ALU.min)
                    kb.act(GT[r][:], GT[r][:], AF.Exp)
                    kb.ts(GG[r][:], gR[r][:], gcol, 0.0, op0=ALU.subtract, op1=ALU.max)
                    kb.act(GG[r][:], GG[r][:], AF.Exp, scale=-1.0)
                    kb.act(Er[r][:], gR[r][:], AF.Exp)
                    kb.copy(sc[r][:, 0:1], gR[r][:, li_:li_ + 1])
                    kb.act(sc[r][:, 1:2], sc[r][:, 0:1], AF.Exp)
                    kb.act(sc[r][:, 2:3], gcol, AF.Exp, bias=sc[r][:, 0:1], scale=-1.0)
                    kb.tt(GT[r][:], GT[r][:], (UT if d == 0 else LT)[:], ALU.mult)
                    kb.tt(QKT[r][:], QKr[d][:], GT[r][:], ALU.mult)
                    kb.stt(Ya[r][:], KKs[d][:], nbcol, GG[r][:], ALU.mult, ALU.mult)
                    stt_[ci] = [Xa[r], Ya[r], Xb_[r], Yb_[r]]
                for (ci, v2, hv, d, col) in chains:
                    r = ci
                    px = nps()
                    kb.tr(px[:, 0:128], Ya[r][:], cx.ident_f[:])
                    kb.copy(Xa[r][:], px[:, 0:128], e='dve')
                    kb.tt(Pa[r][:], Xa[r][:], cx.ident_f[:], ALU.add)
                for k_ in range(1, 7):
                    for (ci, v2, hv, d, col) in chains:
                        r = ci
                        X, Y, Xn, Yn = stt_[ci]
                        pyy = nps()
                        kb.mm(pyy[:, 0:128], X[:], Y[:])
                        if k_ < 6:
                            pxx = nps()
                            kb.mm(pxx[:, 0:128], Y[:], X[:])
                        kb.copy(Yn[:], pyy[:, 0:128], e='act')
                        if k_ < 6:
                            kb.copy(Xn[:], pxx[:, 0:128], e='dve')
                        stt_[ci] = [Xn, Yn, X, Y]
                    for (ci, v2, hv, d, col) in chains:
                        r = ci
                        Yc = stt_[ci][1]
                        pp = nps()
                        kb.mm(pp[:, 0:128], Yc[:], Pa[r][:])
                        kb.tt(Pa[r][:], Pa[r][:], pp[:, 0:128], ALU.add)
                for (ci, v2, hv, d, col) in chains:
                    n = raw[d]
                    tk = slice(n * 128, (n + 1) * 128)
                    r = ci
                    kb.copy(MT[r][:], Pa[r][:], e='act')
                    kb.tt(kdT[r][:], kh[:, tk], Er[r][:], ALU.mult)
                    kb.tt(qdT[r][:], qh[:, tk], Er[r][:], ALU.mult)
                    kb.ts(kst[r][:], ktok[:, n, :], sc[r][:, 2:3], None, op0=ALU.mult)
                for (ci, v2, hv, d, col) in chains:
                    r = ci
                    nbcol = nb_all[:, raw[d], col:col + 1]
                    p1 = nps()
                    kb.mm(p1[:, 0:128], kdT[r][:], Sb[ci][:])
                    kb.stt(Xv[r][:], p1[:, 0:128], nbcol, bv[ci][:, raw[d], :], ALU.mult, ALU.add)
                for (ci, v2, hv, d, col) in chains:
                    r = ci
                    p2 = nps()
                    kb.mm(p2[:, 0:128], MT[r][:], Xv[r][:])
                    kb.copy(vnb[r][:], p2[:, 0:128], e='act')
                for (ci, v2, hv, d, col) in chains:
                    n = raw[d]
                    tk = slice(n * 128, (n + 1) * 128)
                    r = ci
                    p3 = nps()
                    kb.mm(p3[:, 0:128], qdT[r][:], Sb[ci][:], start=True, stop=False)
                    kb.mm(p3[:, 0:128], QKT[r][:], vnb[r][:], start=False, stop=True)
                    key = (hv, n)
                    rot[0] += 1
                    i2 = rot[0] % 2
                    if key not in done:
                        done[key] = 1
                        kb.copy(of[i2][:], p3[:, 0:128], e='act')
                        kb.dma(SQ, o1[tk, hv * 128:(hv + 1) * 128], of[i2][:], w=[o1])
                    else:
                        kb.dma('sp', op_[i2][:], o1[tk, hv * 128:(hv + 1) * 128])
                        kb.dma('sp', zt[i2][:], zz[tk, hv * 128:(hv + 1) * 128])
                        kb.tt(of[i2][:], p3[:, 0:128], op_[i2][:], ALU.add)
                        kb.act(junk[:], of[i2][:], AF.Square, accum=sc[r][:, 3:4])
                        kb.act(sc[r][:, 4:5], sc[r][:, 3:4], AF.Sqrt, bias=cx.eps_t[:, 0:1], scale=1.0 / 128)
                        kb.op('dve', lambda E, r=r: E.reciprocal(sc[r][:, 5:6], sc[r][:, 4:5]), w=[sc[r]], r=[sc[r]])
                        kb.stt(of[i2][:], of[i2][:], sc[r][:, 5:6], hng[:], ALU.mult, ALU.mult)
                        kb.act(zs[i2][:], zt[i2][:], AF.Silu)
                        kb.tt(obf[i2][:], of[i2][:], zs[i2][:], ALU.mult)
                        kb.dma(SQ, og[tk, hv * 128:(hv + 1) * 128], obf[i2][:], w=[og])
                for (ci, v2, hv, d, col) in chains:
                    r = ci
                    p4 = nps()
                    kb.mm(p4[:, 0:128], kst[r][:], vnb[r][:])
                    kb.stt(Sf[ci][:], Sf[ci][:], sc[r][:, 1:2], p4[:, 0:128], ALU.mult, ALU.add)
                    kb.copy(Sb[ci][:], Sf[ci][:], e='act')
    proj_out(kb, cx, 'gdn_w_out', og[:, 0:4096], 4096)


MIXERS[1] = gdn_mixer


SMALL_INPUTS = {
    'gla_w_gate_up': [2, 16, 1024], 'gla_b_gate': [2, 1024], 'gla_head_norm': [1, 512],
    'gdn_conv': [5, 8192], 'gdn_a_log': [2, 32], 'gdn_dt_bias': [2, 32], 'gdn_head_norm': [1, 128],
    'diff_q_norm': [1, 64], 'diff_k_norm': [1, 64], 'diff_lambda': [4, 64], 'diff_subln': [1, 128],
    'swa_q_norm': [1, 128], 'swa_k_norm': [1, 128], 'swa_sink': [1, 16],
}


def kernel(**inputs):
    x = np.asarray(inputs['x'])
    ncores = NCORES
    cfg = dict(S=S, layers=[0, 1, 2, 3], ncores=ncores, small_inputs=SMALL_INPUTS)
    outs = run(cfg, inputs, [x[b] for b in range(ncores)])
    return np.stack(outs, axis=0).astype(np.float32)
```

```python
import numpy as np
from contextlib import ExitStack
import concourse.bass as bass
import concourse.mybir as mybir
from concourse.bass_utils import run_bass_kernel_spmd

F32 = mybir.dt.float32
BF16 = mybir.dt.bfloat16
I32 = mybir.dt.int32
U32 = mybir.dt.uint32
AF = mybir.ActivationFunctionType
ALU = mybir.AluOpType
AX = mybir.AxisListType

S = 4096
D = 2048
NCORES = 8
NDS = 40
SAME_SYNC = True
SQ = 'pool'


def _key(x):
    if isinstance(x, str):
        return x
    return getattr(x, 'tensor', x).name


class KB:
    def __init__(self, nc, st):
        self.nc = nc
        self.E = {'pe': nc.tensor, 'dve': nc.vector, 'act': nc.scalar, 'pool': nc.gpsimd, 'sp': nc.sync}
        self.sem = {e: st.enter_context(nc.semaphore('sm_' + e)) for e in self.E}
        self.cnt = {e: 0 for e in self.E}
        self.seen = {e: {} for e in self.E}
        self.lastw = {}
        self.readers = {}
        self.dsem = [st.enter_context(nc.semaphore('sd%d' % i)) for i in range(NDS)]
        self.dtarget = [0] * NDS
        self.dnext = 0
        self.ninst = 0

    def _semof(self, sk):
        return self.sem[sk] if isinstance(sk, str) else self.dsem[sk[1]]

    def wait(self, e, tok):
        sk, val = tok
        if val <= 0:
            return
        if sk == e and not (SAME_SYNC and e in ('dve', 'act', 'pool')):
            return
        if self.seen[e].get(sk, 0) >= val:
            return
        self.E[e].wait_ge(self._semof(sk), val)
        self.seen[e][sk] = val

    def _deps(self, e, w, r):
        for k in r:
            k = _key(k)
            if k in self.lastw:
                self.wait(e, self.lastw[k])
        for k in w:
            k = _key(k)
            if k in self.lastw:
                self.wait(e, self.lastw[k])
            for sk, val in self.readers.get(k, {}).items():
                self.wait(e, (sk, val))

    def _commit(self, tok, w, r):
        for k in r:
            k = _key(k)
            d = self.readers.setdefault(k, {})
            d[tok[0]] = max(d.get(tok[0], 0), tok[1])
        for k in w:
            k = _key(k)
            self.lastw[k] = tok
            self.readers[k] = {}

    def op(self, e, fn, w=(), r=()):
        self._deps(e, w, r)
        inst = fn(self.E[e])
        self.cnt[e] += 1
        inst.then_inc(self.sem[e], 1)
        self._commit((e, self.cnt[e]), w, r)
        self.ninst += 1
        return inst

    def dma(self, q, out, in_, w=None, r=None, fn=None, **kw):
        w = [out] if w is None else w
        r = [in_] if r is None else r
        i = self.dnext
        self.dnext = (i + 1) % NDS
        self.wait(q, (('d', i), self.dtarget[i]))
        self._deps(q, w, r)
        if fn is None:
            inst = self.E[q].dma_start(out=out, in_=in_, **kw)
        else:
            inst = fn(self.E[q])
        self.dtarget[i] += 16
        inst.then_inc(self.dsem[i], 16)
        self._commit((('d', i), self.dtarget[i]), w, r)
        self.ninst += 1

    def barrier(self):
        for e in self.E:
            for e2 in self.E:
                if e2 != e:
                    self.wait(e, (e2, self.cnt[e2]))
            for i in range(NDS):
                self.wait(e, (('d', i), self.dtarget[i]))

    def finish(self):
        for i in range(NDS):
            self.wait('sp', (('d', i), self.dtarget[i]))
        for e in self.E:
            if e != 'sp':
                self.wait('sp', (e, self.cnt[e]))

    def mm(self, out, lhsT, rhs, start=True, stop=True, **kw):
        return self.op('pe', lambda E: E.matmul(out, lhsT, rhs, start=start, stop=stop, **kw), w=[out], r=[lhsT, rhs])

    def tr(self, out, in_, ident):
        return self.op('pe', lambda E: E.transpose(out, in_, ident), w=[out], r=[in_, ident])

    def act(self, out, in_, func, bias=None, scale=None, accum=None, e='act'):
        kw = {}
        r = [in_]
        w = [out]
        if bias is not None:
            kw['bias'] = bias
            if not isinstance(bias, (int, float)):
                r.append(bias)
        if scale is not None:
            kw['scale'] = scale
            if not isinstance(scale, (int, float)):
                r.append(scale)
        if accum is not None:
            kw['accum_out'] = accum
            w.append(accum)
        return self.op('act', lambda E: E.activation(out, in_, func, **kw), w=w, r=r)

    def tt(self, out, a, b, op, e='dve'):
        return self.op(e, lambda E: E.tensor_tensor(out, a, b, op), w=[out], r=[a, b])

    def ts(self, out, a, s1, s2=None, op0=ALU.mult, op1=None, e='dve', accum=None):
        r = [a] + [s for s in (s1, s2) if s is not None and not isinstance(s, (int, float))]
        w = [out] + ([accum] if accum is not None else [])
        kw = {}
        if op1 is not None:
            kw['op1'] = op1
        if accum is not None:
            kw['accum_out'] = accum
        return self.op(e, lambda E: E.tensor_scalar(out, a, s1, s2, op0, **kw), w=w, r=r)

    def stt(self, out, a, sc, b, op0, op1):
        r = [a, b] + ([] if isinstance(sc, (int, float)) else [sc])
        return self.op('dve', lambda E: E.scalar_tensor_tensor(out, a, sc, b, op0, op1), w=[out], r=r)

    def copy(self, out, in_, e='dve'):
        if e == 'act':
            return self.op('act', lambda E: E.copy(out, in_), w=[out], r=[in_])
        return self.op(e, lambda E: E.tensor_copy(out, in_), w=[out], r=[in_])

    def memset(self, out, val, e='dve'):
        return self.op(e, lambda E: E.memset(out, val), w=[out], r=[])


WCOLS = 2048


class WTable:
    def __init__(self):
        self.ents = {}
        self.R = 0

    def add(self, name, K, N):
        n = K * N
        assert n % WCOLS == 0
        rows = n // WCOLS
        self.ents[name] = (self.R, rows, K, N)
        self.R += rows

    def host_shards(self, arrays):
        out = np.empty((self.R, WCOLS), np.float32)
        for name, (ro, rows, K, N) in self.ents.items():
            out[ro:ro + rows] = np.ascontiguousarray(arrays[name]).reshape(rows, WCOLS)
        return out

    def ap(self, gathered_flat, name, k0, kk, n0, nn):
        ro, rows, K, N = self.ents[name]
        base = ro * WCOLS + k0 * N
        return gathered_flat[base: base + kk * N].rearrange("(k n) -> k n", n=N)[:, n0:n0 + nn]


def build_wtable(layers):
    wt = WTable()
    if 0 in layers:
        wt.add('gla_w_in', D, 6176)
        wt.add('gla_w_out', D, D)
    if 1 in layers:
        wt.add('gdn_w_in', D, 12416)
        wt.add('gdn_w_out', 4096, D)
    if 2 in layers:
        wt.add('diff_w_in', D, 6144)
        wt.add('diff_w_out', D, D)
    if 3 in layers:
        wt.add('swa_w_in', D, 3072)
        wt.add('swa_w_out', D, D)
    return wt


class Ctx:
    pass


_UNIQ = [0]


_KB = [None]


def mk_sb(nc, st):
    st.callback(lambda: _KB[0].barrier())

    def sb(name, shape, dt):
        _UNIQ[0] += 1
        return st.enter_context(nc.sbuf_tensor('%s_u%d' % (name, _UNIQ[0]), shape, dt))
    return sb


def make_consts():
    ident = np.eye(128, dtype=np.float32)
    masku = np.triu(np.ones((128, 128), np.float32))
    maskl = np.tril(np.ones((128, 128), np.float32))
    d = {'c_ident': ident, 'c_masku': masku, 'c_maskl': maskl}
    d.update(attn_consts())
    return d


def rows_bcast(ap_row, n=128):
    return ap_row.broadcast_to([n, ap_row.shape[-1]])


def load_consts(kb, cx, st):
    nc = cx.nc
    cx.ident_f = st.enter_context(nc.sbuf_tensor('ident_f', [128, 128], F32))
    cx.ident_b = st.enter_context(nc.sbuf_tensor('ident_b', [128, 128], BF16))
    kb.dma('sp', cx.ident_f[:], cx.dram['c_ident'][:, :])
    kb.copy(cx.ident_b[:], cx.ident_f[:])
    cx.ps = [st.enter_context(nc.psum_tensor('ps%d' % i, [128, 512], F32)) for i in range(3)]
    cx.acc = [st.enter_context(nc.psum_tensor('acc%d' % i, [128, 512], F32)) for i in range(4)]
    cx.pb = [st.enter_context(nc.psum_tensor('pb%d' % i, [128, 1024], BF16)) for i in range(1)]
    cx.psi = 0
    cx.pbi = 0


def next_ps(cx):
    t = cx.ps[cx.psi % len(cx.ps)]
    cx.psi += 1
    return t


def next_pb(cx):
    t = cx.pb[cx.pbi % len(cx.pb)]
    cx.pbi += 1
    return t


def norm_tile(kb, cx, bufs, h_rows, gain_bc, i):
    ht = bufs['h'][i % 2]
    xn = bufs['xn'][i % 2]
    ss = bufs['ss'][i % 2]
    kb.dma('sp', ht[:], h_rows, r=['h'])
    kb.act(bufs['junk'][:], ht[:], AF.Square, accum=ss[:, 0:1])
    kb.act(ss[:, 1:2], ss[:, 0:1], AF.Sqrt, bias=cx.eps_t[:, 0:1], scale=1.0 / D)
    kb.op('dve', lambda E: E.reciprocal(ss[:, 2:3], ss[:, 1:2]), w=[ss], r=[ss])
    kb.stt(xn[:], ht[:], ss[:, 2:3], gain_bc[:], ALU.mult, ALU.mult)
    return xn


def transpose_to(kb, cx, src_bf, dstT, tok0, ntok=128, nchunks=16):
    for c0 in range(0, nchunks, 8):
        pb = next_pb(cx)
        n = min(8, nchunks - c0)
        for c in range(n):
            kb.tr(pb[:, c * 128:(c + 1) * 128], src_bf[:, (c0 + c) * 128:(c0 + c + 1) * 128], cx.ident_b[:])
        kb.copy(dstT[:, c0:c0 + n, tok0:tok0 + 128],
                pb[:, 0:n * 128].rearrange("p (c t) -> p c t", t=128), e='act' if (c0 // 8) % 2 else 'dve')


def moe_layer(kb, cx, li):
    nc = cx.nc
    S = cx.S
    NE, DFF = 16, 1024
    CAP = 2 * S // NE
    NJ = CAP // 128
    h = cx.h
    hn = cx.hn_bf
    with ExitStack() as st:
        sb = mk_sb(nc, st)
        affT = sb('m_affT', [NE, S], F32)
        stA = ExitStack()
        sbA = mk_sb(nc, stA)
        bufs = {'h': [sbA('m_h%d' % i, [128, D], F32) for i in range(2)],
                'xn': [sbA('m_xn%d' % i, [128, D], BF16) for i in range(2)],
                'ss': [sbA('m_ss%d' % i, [128, 4], F32) for i in range(2)],
                'junk': sbA('m_junk', [128, D], BF16)}
        gain = sbA('m_gain', [128, D], F32)
        rt_f = sbA('m_rtf', [128, 16, NE], F32)
        rt_b = sbA('m_rtb', [128, 16, NE], BF16)
        hnT = sbA('m_hnT', [128, 16, 128], BF16)
        lg = [sbA('m_lg%d' % i, [128, NE + 4], F32) for i in range(2)]
        kb.dma('sp', gain[:], rows_bcast(cx.dram['norm_ffn'][li:li + 1, :]))
        kb.dma('sp', rt_f[:], cx.dram['moe_router'][li].rearrange("(c p) e -> p c e", p=128))
        kb.copy(rt_b[:], rt_f[:])
        for t in range(S // 128):
            xn = norm_tile(kb, cx, bufs, h[t * 128:(t + 1) * 128, :], gain, t)
            kb.dma(SQ, hn[t * 128:(t + 1) * 128, :], xn[:], w=['hn'])
            transpose_to(kb, cx, xn, hnT, 0)
            ps = next_ps(cx)
            for c in range(16):
                kb.mm(ps[:, 0:NE], hnT[:, c, :], rt_b[:, c, :], start=(c == 0), stop=(c == 15))
            l = lg[t % 2]
            kb.op('dve', lambda E: E.reduce_max(l[:, NE:NE + 1], ps[:, 0:NE], AX.X), w=[l], r=[ps])
            kb.ts(l[:, NE + 1:NE + 2], l[:, NE:NE + 1], -1.0, None, op0=ALU.mult)
            kb.act(l[:, 0:NE], ps[:, 0:NE], AF.Exp, bias=l[:, NE + 1:NE + 2], scale=1.0, accum=l[:, NE + 2:NE + 3])
            kb.op('dve', lambda E: E.reciprocal(l[:, NE + 3:NE + 4], l[:, NE + 2:NE + 3]), w=[l], r=[l])
            kb.ts(l[:, 0:NE], l[:, 0:NE], l[:, NE + 3:NE + 4], None, op0=ALU.mult)
            ps2 = next_ps(cx)
            kb.tr(ps2[0:NE, 0:128], l[:, 0:NE], cx.ident_f[:])
            kb.copy(affT[:, t * 128:(t + 1) * 128], ps2[0:NE, 0:128])
        stA.close()
        gate = sb('m_gate', [NE, CAP], F32)
        idx = sb('m_idx', [NE, CAP], U32)
        for r8 in range(CAP // 8):
            g8 = gate[:, r8 * 8:(r8 + 1) * 8]
            kb.op('dve', lambda E: E.max(g8, affT[:]), w=[gate], r=[affT])
            kb.op('dve', lambda E: E.max_index(idx[:, r8 * 8:(r8 + 1) * 8], g8, affT[:]), w=[idx], r=[gate, affT])
            kb.op('dve', lambda E: E.match_replace(affT[:], g8, affT[:], -1.0), w=[affT], r=[gate, affT])
        kb.dma('sp', cx.sc_gate[:, :], gate[:], w=['sc_gate'])
        kb.dma('sp', cx.sc_idx[:, :], idx[:], w=['sc_idx'])
        gateT = sb('m_gateT', [128, NE * NJ], F32)
        idxT = sb('m_idxT', [128, NE * NJ], U32)
        for e in range(NE):
            kb.dma('sp', gateT[:, e * NJ:(e + 1) * NJ], cx.sc_gate[e].rearrange("(j p) -> p j", p=128),
                   r=['sc_gate'], allow_slow_non_contiguous=True)
            kb.dma('sp', idxT[:, e * NJ:(e + 1) * NJ], cx.sc_idx[e].rearrange("(j p) -> p j", p=128),
                   r=['sc_idx'], allow_slow_non_contiguous=True)
        xs = [[sb('m_xs%d_%d' % (i, j), [128, D], BF16) for j in range(NJ)] for i in range(2)]
        xsT = [sb('m_xsT%d' % i, [128, 16, CAP], BF16) for i in range(2)]
        wa = [sb('m_wa%d' % i, [128, 16, 256], BF16) for i in range(2)]
        wb = [sb('m_wb%d' % i, [128, 16, 256], BF16) for i in range(2)]
        w2t = [sb('m_w2%d' % i, [128, 8, 512], BF16) for i in range(2)]
        gT = sb('m_gT', [128, 8, CAP], BF16)
        tmp = [sb('m_tmp%d' % i, [128, 512], F32) for i in range(2)]
        ybuf = [sb('m_y%d' % i, [128, D], F32) for i in range(NJ)]
        nld = 0

        def gathers(e):
            for j in range(NJ):
                x_ = xs[e % 2][j]
                col = e * NJ + j
                kb.dma('pool', x_[:], hn[:, :], r=['hn', idxT], w=[x_],
                       fn=lambda E, x_=x_, col=col: E.indirect_dma_start(
                           out=x_[:], out_offset=None, in_=hn[:, :],
                           in_offset=bass.IndirectOffsetOnAxis(ap=idxT[:, col:col + 1], axis=0)))

        def transposes(e):
            for j in range(NJ):
                transpose_to(kb, cx, xs[e % 2][j], xsT[e % 2], j * 128)

        gathers(0)
        transposes(0)
        for e in range(NE):
            xT = xsT[e % 2]
            if e + 1 < NE:
                gathers(e + 1)
            for fg in range(4):
                a_ = wa[nld % 2]
                b_ = wb[nld % 2]
                nld += 1
                kb.dma('sp', a_[:], cx.Wtile('moe_w1_%d' % li, e * D, 16, fg * 256, 256), r=['wg'])
                kb.dma('sp', b_[:], cx.Wtile('moe_w3_%d' % li, e * D, 16, fg * 256, 256), r=['wg'])
                for f in range(2):
                    fc = fg * 2 + f
                    pa = next_ps(cx)
                    pbm = next_ps(cx)
                    for c in range(16):
                        kb.mm(pa[:, 0:CAP], a_[:, c, f * 128:(f + 1) * 128], xT[:, c, :], start=(c == 0), stop=(c == 15))
                    for c in range(16):
                        kb.mm(pbm[:, 0:CAP], b_[:, c, f * 128:(f + 1) * 128], xT[:, c, :], start=(c == 0), stop=(c == 15))
                    tm = tmp[fc % 2]
                    kb.act(tm[:, 0:CAP], pa[:, 0:CAP], AF.Silu)
                    kb.tt(gT[:, fc, :], tm[:, 0:CAP], pbm[:, 0:CAP], ALU.mult)
            for dc in range(4):
                w_ = w2t[dc % 2]
                kb.dma('sp', w_[:], cx.Wtile('moe_w2_%d' % li, e * 1024, 8, dc * 512, 512), r=['wg'])
                for j in range(NJ):
                    py = next_ps(cx)
                    for c in range(8):
                        kb.mm(py[:, :], gT[:, c, j * 128:(j + 1) * 128], w_[:, c, :], start=(c == 0), stop=(c == 7))
                    kb.ts(ybuf[j][:, dc * 512:(dc + 1) * 512], py[:, :], gateT[:, e * NJ + j:e * NJ + j + 1], None, op0=ALU.mult)
            if e + 1 < NE:
                transposes(e + 1)
            for j in range(NJ):
                col = e * NJ + j
                yb = ybuf[j]
                kb.dma('pool', h[:, :], yb[:], w=['h'], r=[yb, idxT],
                       fn=lambda E, yb=yb, col=col: E.indirect_dma_start(
                           out=h[:, :], out_offset=bass.IndirectOffsetOnAxis(ap=idxT[:, col:col + 1], axis=0),
                           in_=yb[:], in_offset=None, compute_op=ALU.add))


def prologue_weights(kb, cx):
    nc = cx.nc
    wsh = cx.dram['wsh']
    with ExitStack() as st:
        f = [st.enter_context(nc.sbuf_tensor('pw_f%d' % i, [128, 4096], F32)) for i in range(2)]
        b = [st.enter_context(nc.sbuf_tensor('pw_b%d' % i, [128, 4096], BF16)) for i in range(2)]
        i = 0
        for name, (ro, rows, K_, N_) in cx.wt.ents.items():
            per = rows * WCOLS // 128
            src = wsh[ro:ro + rows, :].rearrange("r c -> (r c)").rearrange("(p n) -> p n", p=128)
            dst = cx.wbf[name].rearrange("r c -> (r c)").rearrange("(p n) -> p n", p=128)
            for c0 in range(0, per, 4096):
                n = min(4096, per - c0)
                kb.dma('sp', f[i % 2][:, 0:n], src[:, c0:c0 + n])
                kb.copy(b[i % 2][:, 0:n], f[i % 2][:, 0:n], e=('dve' if i % 2 == 0 else 'act'))
                kb.dma(SQ, dst[:, c0:c0 + n], b[i % 2][:, 0:n], w=['wg'])
                i += 1
        kb.barrier()


def build_program(cfg):
    nc = bass.Bass("TRN2", target_bir_lowering=False)
    cx = Ctx()
    cx.nc = nc
    cx.S = cfg['S']
    cx.ncores = cfg.get('ncores', NCORES)
    cx.debug_og = cfg.get('debug_og', False)
    cx.stop = cfg.get('stop', 0)
    Sx = cx.S
    layers = cfg['layers']
    wt = WTable()
    for li in layers:
        if cfg.get('mixers', True):
            if li == 0:
                wt.add('gla_w_in', D, 6176); wt.add('gla_w_out', D, D)
            if li == 1:
                wt.add('gdn_w_in', D, 12416); wt.add('gdn_w_out', 4096, D)
            if li == 2:
                wt.add('diff_w_in', D, 6144); wt.add('diff_w_out', D, D)
            if li == 3:
                wt.add('swa_w_in', D, 3072); wt.add('swa_w_out', D, D)
        if cfg.get('moe', True):
            wt.add('moe_w1_%d' % li, 16 * D, 1024)
            wt.add('moe_w3_%d' % li, 16 * D, 1024)
            wt.add('moe_w2_%d' % li, 16 * 1024, D)
    cx.wt = wt
    dram = {}

    def din(name, shape, dt=F32):
        dram[name] = nc.dram_tensor(name, list(shape), dt, kind="ExternalInput").ap()

    din('x', [Sx, D])
    din('wsh', [wt.R, WCOLS])
    din('c_ident', [128, 128]); din('c_masku', [128, 128]); din('c_maskl', [128, 128]); din('c_bk', [128, 384]); din('c_mneg', [128, 384])
    cx.scr = {}
    din('norm_mix', [4, D]); din('norm_ffn', [4, D]); din('moe_router', [4, D, 16]); din('rel_bias', [32, 16])
    for name, shape in cfg.get('small_inputs', {}).items():
        din(name, shape)
    out = nc.dram_tensor('out', [Sx, D], F32, kind="ExternalOutput").ap()
    cx.dram = dram
    cx.h = out
    cx.hn_bf = nc.dram_tensor('hn_bf', [Sx, D], BF16, kind="Internal").ap()
    cx.sc_gate = nc.dram_tensor('sc_gate', [16, 2 * Sx // 16], F32, kind="Internal").ap()
    cx.sc_idx = nc.dram_tensor('sc_idx', [16, 2 * Sx // 16], U32, kind="Internal").ap()
    cx.wbf = {}
    for name, (ro, rows, K_, N_) in wt.ents.items():
        cx.wbf[name] = nc.dram_tensor('wbf_' + name, [rows, WCOLS], BF16, kind="Internal").ap()

    def Wap(name, k0, kk, n0, nn):
        ro, rows, K_, N_ = wt.ents[name]
        flat = cx.wbf[name].rearrange("r c -> (r c)")
        return flat[k0 * N_: (k0 + kk) * N_].rearrange("(k n) -> k n", n=N_)[:, n0:n0 + nn]
    cx.W = Wap

    def Wtile(name, k0, KC, n0, nn):
        ro, rows, K_, N_ = wt.ents[name]
        flat = cx.wbf[name].rearrange("r c -> (r c)")
        return flat[k0 * N_: (k0 + KC * 128) * N_].rearrange("(c p n) -> p c n", p=128, n=N_)[:, :, n0:n0 + nn]
    cx.Wtile = Wtile
    cx.wmoe = {}
    for li in layers:
        cx.wmoe[('w1', li)] = (lambda e, k0, kk, n0, nn, li=li: cx.W('moe_w1_%d' % li, e * D + k0, kk, n0, nn))
        cx.wmoe[('w3', li)] = (lambda e, k0, kk, n0, nn, li=li: cx.W('moe_w3_%d' % li, e * D + k0, kk, n0, nn))
        cx.wmoe[('w2', li)] = (lambda e, k0, kk, n0, nn, li=li: cx.W('moe_w2_%d' % li, e * 1024 + k0, kk, n0, nn))

    with ExitStack() as st:
        kb = KB(nc, st)
        cx.kb = kb
        _KB[0] = kb
        load_consts(kb, cx, st)
        cx.eps_t = st.enter_context(nc.sbuf_tensor('eps_t', [128, 1], F32))
        kb.memset(cx.eps_t[:], 1e-6)
        prologue_weights(kb, cx)
        with ExitStack() as st2:
            tb = [st2.enter_context(nc.sbuf_tensor('cp%d' % i, [128, D], F32)) for i in range(2)]
            for t in range(Sx // 128):
                kb.dma('sp', tb[t % 2][:], dram['x'][t * 128:(t + 1) * 128, :])
                kb.dma(SQ, cx.h[t * 128:(t + 1) * 128, :], tb[t % 2][:], w=['h'])
            kb.barrier()
        for li in layers:
            if cfg.get('mixers', True):
                MIXERS[li](kb, cx, li)
            if cfg.get('moe', True):
                moe_layer(kb, cx, li)
        kb.finish()
    cx.ninst = kb.ninst
    return nc, cx


MIXERS = {}


def host_weights(inputs, cx):
    arrs = {}
    for name in cx.wt.ents:
        if name.startswith('moe_'):
            kind, li = name[4:6], int(name.split('_')[-1])
            a = inputs['moe_' + kind][li]
            arrs[name] = a.reshape(-1, a.shape[-1])
        else:
            a = inputs[name][0]
            arrs[name] = a
    return cx.wt.host_shards(arrs)


def run(cfg, inputs, xs):
    nc, cx = build_program(cfg)
    shards = host_weights(inputs, cx)
    consts = make_consts()
    in_maps = []
    for c in range(cx.ncores):
        m = {'x': np.ascontiguousarray(xs[c]), 'wsh': shards}
        m.update(consts)
        for k in ('norm_mix', 'norm_ffn', 'moe_router', 'rel_bias'):
            m[k] = np.ascontiguousarray(inputs[k])
        for name in cfg.get('small_inputs', {}):
            m[name] = np.ascontiguousarray(inputs[name]).reshape(cfg['small_inputs'][name])
        in_maps.append(m)
    res = run_bass_kernel_spmd(nc, in_maps, core_ids=list(range(cx.ncores)))
    return [r['out'] for r in res.results]


def proj_in(kb, cx, li, wname, groups):
    nc = cx.nc
    S = cx.S
    TT = 512
    with ExitStack() as st:
        sb = mk_sb(nc, st)
        bufs = {'h': [sb('p_h%d' % i, [128, D], F32) for i in range(2)],
                'xn': [sb('p_xn%d' % i, [128, D], BF16) for i in range(2)],
                'ss': [sb('p_ss%d' % i, [128, 4], F32) for i in range(2)],
                'junk': sb('p_junk', [128, D], BF16)}
        gain = sb('p_gain', [128, D], F32)
        hnT = sb('p_hnT', [128, 16, TT], BF16)
        wts = [sb('p_w%d' % i, [128, 16, 512], BF16) for i in range(2)]
        stg_b = [sb('p_sb%d' % i, [128, 512], BF16) for i in range(3)]
        stg_f = [sb('p_sf%d' % i, [128, 512], F32) for i in range(2)]
        kb.dma('sp', gain[:], rows_bcast(cx.dram['norm_mix'][li:li + 1, :]))
        nw = 0
        ns = 0
        for s0 in range(0, S, TT):
            for j in range(TT // 128):
                xn = norm_tile(kb, cx, bufs, cx.h[s0 + j * 128:s0 + (j + 1) * 128, :], gain, j)
                transpose_to(kb, cx, xn, hnT, j * 128)
            for (col0, ncols, mode, dst, scale) in groups:
                for c0 in range(0, ncols, 512):
                    cw = min(512, ncols - c0)
                    wt = wts[nw % 2]
                    nw += 1
                    kb.dma('sp', wt[:, :, 0:cw], cx.Wtile(wname, 0, 16, col0 + c0, cw), r=['wg'])
                    isb = (dst.dtype == BF16)
                    if mode == 'T':
                        for f0 in range(0, cw, 128):
                            fw = min(128, cw - f0)
                            ps = next_ps(cx)
                            for c in range(16):
                                kb.mm(ps[0:fw, :], wt[:, c, f0:f0 + fw], hnT[:, c, :], start=(c == 0), stop=(c == 15))
                            sg = (stg_b[ns % 3] if isb else stg_f[ns % 2])
                            ns += 1
                            kb.act(sg[0:fw, :], ps[0:fw, :], AF.Copy, scale=float(scale))
                            kb.dma(SQ, dst[c0 + f0:c0 + f0 + fw, s0:s0 + TT], sg[0:fw, :], w=[dst])
                    else:
                        for j in range(TT // 128):
                            ps = next_ps(cx)
                            for c in range(16):
                                kb.mm(ps[:, 0:cw], hnT[:, c, j * 128:(j + 1) * 128], wt[:, c, 0:cw], start=(c == 0), stop=(c == 15))
                            sg = (stg_b[ns % 3] if isb else stg_f[ns % 2])
                            ns += 1
                            if ns % 2:
                                kb.act(sg[:, 0:cw], ps[:, 0:cw], AF.Copy, scale=float(scale))
                            else:
                                kb.ts(sg[:, 0:cw], ps[:, 0:cw], float(scale), None, op0=ALU.mult)
                            kb.dma(SQ, dst[s0 + j * 128:s0 + (j + 1) * 128, c0:c0 + cw], sg[:, 0:cw], w=[dst])


def proj_out(kb, cx, wname, og, KD):
    nc = cx.nc
    S = cx.S
    KC = KD // 128
    TT = 512
    if getattr(cx, 'debug_og', False):
        with ExitStack() as st:
            sb = mk_sb(nc, st)
            a = [sb('dbg_a%d' % i, [128, 2048], BF16) for i in range(2)]
            b = [sb('dbg_b%d' % i, [128, 2048], F32) for i in range(2)]
            for t in range(S // 128):
                kb.dma('sp', a[t % 2][:], og[t * 128:(t + 1) * 128, 0:2048])
                kb.copy(b[t % 2][:], a[t % 2][:])
                kb.dma(SQ, cx.h[t * 128:(t + 1) * 128, :], b[t % 2][:], w=['h'])
        return
    with ExitStack() as st:
        sb = mk_sb(nc, st)
        ogt = [sb('o_og%d' % i, [128, KD], BF16) for i in range(2)]
        ogT = sb('o_ogT', [128, KC, TT], BF16)
        wts = [sb('o_w%d' % i, [128, KC, 512], BF16) for i in range(2)]
        hb = [sb('o_h%d' % i, [128, D], F32) for i in range(4)]
        nw = 0
        for s0 in range(0, S, TT):
            for j in range(4):
                o_ = ogt[j % 2]
                kb.dma('sp', o_[:], og[s0 + j * 128:s0 + (j + 1) * 128, :])
                transpose_to(kb, cx, o_, ogT, j * 128, nchunks=KC)
                kb.dma('sp', hb[j][:], cx.h[s0 + j * 128:s0 + (j + 1) * 128, :], r=['h'])
            for dc in range(4):
                wt = wts[nw % 2]
                nw += 1
                for c in range(0, KC, 16):
                    kb.dma('sp', wt[:, c:c + 16, :], cx.Wtile(wname, c * 128, 16, dc * 512, 512), r=['wg'])
                for j in range(4):
                    ps = next_ps(cx)
                    for c in range(KC):
                        kb.mm(ps[:, :], ogT[:, c, j * 128:(j + 1) * 128], wt[:, c, :], start=(c == 0), stop=(c == KC - 1))
                    kb.tt(hb[j][:, dc * 512:(dc + 1) * 512], hb[j][:, dc * 512:(dc + 1) * 512], ps[:, :], ALU.add)
            for j in range(4):
                kb.dma(SQ, cx.h[s0 + j * 128:s0 + (j + 1) * 128, :], hb[j][:], w=['h'])


def dscr(cx, name, shape, dt):
    if name not in cx.scr:
        cx.scr[name] = cx.nc.dram_tensor(name, list(shape), dt, kind="Internal").ap()
    return cx.scr[name]


def gla_mixer(kb, cx, li):
    nc = cx.nc
    S = cx.S
    NCH = S // 128
    qT = dscr(cx, 'gla_qT', [1024, S], BF16)
    kT = dscr(cx, 'gla_kT', [1024, S], BF16)
    vv = dscr(cx, 'gla_v', [S, 2048], BF16)
    rr = dscr(cx, 'gla_r', [S, 2048], BF16)
    gl = dscr(cx, 'gla_glo', [32, S], BF16)
    o1 = dscr(cx, 'gla_o1', [S, 2048], F32)
    og = dscr(cx, 'mix_og', [S, 4096], BF16)
    proj_in(kb, cx, li, 'gla_w_in', [(0, 1024, 'T', qT, 1.0 / 16.0), (1024, 1024, 'T', kT, 1.0),
                                     (2048, 2048, 'N', vv, 1.0), (4096, 2048, 'N', rr, 1.0),
                                     (6144, 32, 'T', gl, 1.0)])
    with ExitStack() as st:
        sb = mk_sb(nc, st)
        glo1 = sb('g_glo', [16, S], BF16)
        wup_f = sb('g_wupf', [16, 2, 1024], F32)
        wup = sb('g_wup', [16, 2, 1024], BF16)
        negb = sb('g_negb', [128, 2, 8], F32)
        hng = sb('g_hng', [128, 512], F32)
        mask = [sb('g_mask%d' % d, [128, 128], F32) for d in range(2)]
        qh = sb('g_q', [128, 2, S], BF16)
        kh = sb('g_k', [128, 2, S], BF16)
        qd = sb('g_qd', [128, 2, S], BF16)
        ki = sb('g_ki', [128, 2, S], BF16)
        kstT = sb('g_kstT', [128, S], BF16)
        kst = sb('g_kst', [128, NCH, 256], BF16)
        CSx = sb('g_cs', [128, S + 1], F32)
        T1 = sb('g_t1', [128, S], F32)
        T2 = sb('g_t2', [128, S], F32)
        T3 = sb('g_t3', [128, S], F32)
        dec = sb('g_dec', [128, 2, NCH], F32)
        Sf = sb('g_Sf', [128, 2, 512], F32)
        Sb = sb('g_Sb', [128, 2, 512], BF16)
        vt = [sb('g_v%d' % i, [128, 512], BF16) for i in range(2)]
        rt = [sb('g_r%d' % i, [128, 512], BF16) for i in range(2)]
        AT = [sb('g_AT%d' % i, [128, 128], BF16) for i in range(2)]
        of = [sb('g_of%d' % i, [128, 512], F32) for i in range(2)]
        op_ = [sb('g_op%d' % i, [128, 512], F32) for i in range(2)]
        sr = [sb('g_sr%d' % i, [128, 512], F32) for i in range(2)]
        ob = [sb('g_ob%d' % i, [128, 512], BF16) for i in range(2)]
        ss = [sb('g_ss%d' % i, [128, 4], F32) for i in range(2)]
        junk = sb('g_junk', [128, 512], BF16)
        kb.dma('sp', wup_f[:], cx.dram['gla_w_gate_up'].rearrange("d r e -> r d e"))
        kb.copy(wup[:], wup_f[:])
        for d in range(2):
            kb.dma('sp', negb[:, d, :], cx.dram['gla_b_gate'][d].rearrange("(c p) -> p c", p=128), allow_slow_non_contiguous=True)
        kb.ts(negb[:], negb[:], -1.0, None, op0=ALU.mult)
        kb.dma('sp', hng[:], rows_bcast(cx.dram['gla_head_norm'][0:1, :]))
        kb.dma('sp', mask[0][:], cx.dram['c_masku'][:, :])
        kb.dma('sp', mask[1][:], cx.dram['c_maskl'][:, :])
        kb.memset(CSx[:, 0:1], 0.0)
        ch = lambda t: t.rearrange("p (n c) -> p n c", c=128)
        for hh in range(4):
            kb.dma('sp', qh[:], qT[hh * 256:(hh + 1) * 256, :].rearrange("(c p) s -> p c s", p=128))
            kb.dma('sp', kh[:], kT[hh * 256:(hh + 1) * 256, :].rearrange("(c p) s -> p c s", p=128))
            for d in range(2):
                kb.dma('sp', glo1[:], gl[d * 16:(d + 1) * 16, :])
                for fc in range(2):
                    f0 = hh * 256 + fc * 128
                    for b0 in range(0, S, 512):
                        ps = next_ps(cx)
                        kb.mm(ps[:, :], wup[:, d, f0:f0 + 128], glo1[:, b0:b0 + 512])
                        kb.act(T1[:, b0:b0 + 512], ps[:, :], AF.Exp, bias=negb[:, d, hh * 2 + fc:hh * 2 + fc + 1], scale=-1.0)
                    kb.act(T1[:], T1[:], AF.Ln, bias=1.0, scale=1.0)
                    kb.op('dve', lambda E: E.tensor_tensor_scan(CSx[:, 1:S + 1], T1[:], T1[:], 0.0, ALU.add, ALU.max),
                          w=[CSx], r=[T1])
                    if d == 0:
                        kb.tt(ch(T2[:]), ch(CSx[:, 1:S + 1]), ch(CSx[:, 0:S])[:, :, 0:1].broadcast_to([128, NCH, 128]), ALU.subtract)
                        li_ = 127
                    else:
                        kb.tt(ch(T2[:]), ch(CSx[:, 1:S + 1])[:, :, 127:128].broadcast_to([128, NCH, 128]), ch(CSx[:, 0:S]), ALU.subtract)
                        li_ = 0
                    kb.act(T3[:], T2[:], AF.Exp, scale=-1.0 / 16.0)
                    kb.act(CSx[:, 1:S + 1], T2[:], AF.Exp, scale=1.0 / 16.0)
                    kb.tt(qd[:, fc, :], qh[:, fc, :], T3[:], ALU.mult)
                    kb.tt(T1[:], kh[:, fc, :], CSx[:, 1:S + 1], ALU.mult)
                    kb.copy(ki[:, fc, :], T1[:], e='pool')
                    kb.tt(ch(kstT[:]), ch(T1[:]), ch(T3[:])[:, :, li_:li_ + 1].broadcast_to([128, NCH, 128]), ALU.mult)
                    kb.copy(dec[:, fc, :], ch(T3[:])[:, :, li_])
                    for n0 in range(0, NCH, 8):
                        pb = next_pb(cx)
                        for n in range(8):
                            kb.tr(pb[:, n * 128:(n + 1) * 128], kstT[:, (n0 + n) * 128:(n0 + n + 1) * 128], cx.ident_b[:])
                        kb.copy(kst[:, n0:n0 + 8, fc * 128:(fc + 1) * 128], pb[:, :].rearrange("p (n t) -> p n t", t=128), e='act')
                kb.memset(Sf[:], 0.0)
                kb.memset(Sb[:], 0.0)
                order = list(range(NCH)) if d == 0 else list(range(NCH - 1, -1, -1))
                for it, n in enumerate(order):
                    tk = slice(n * 128, (n + 1) * 128)
                    v_ = vt[it % 2]
                    kb.dma('sp', v_[:], vv[tk, hh * 512:(hh + 1) * 512])
                    ps1 = next_ps(cx)
                    for fc in range(2):
                        kb.mm(ps1[:, 0:128], ki[:, fc, tk], qd[:, fc, tk], start=(fc == 0), stop=(fc == 1))
                    a_ = AT[it % 2]
                    kb.tt(a_[:], ps1[:, 0:128], mask[d][:], ALU.mult)
                    ps2 = next_ps(cx)
                    kb.mm(ps2[:, :], a_[:], v_[:], start=True, stop=False)
                    for fc in range(2):
                        kb.mm(ps2[:, :], qd[:, fc, tk], Sb[:, fc, :], start=False, stop=(fc == 1))
                    if d == 0:
                        o_ = of[it % 2]
                        kb.copy(o_[:], ps2[:, :], e='act')
                        kb.dma(SQ, o1[tk, hh * 512:(hh + 1) * 512], o_[:], w=[o1])
                    else:
                        p_ = op_[it % 2]
                        r_ = rt[it % 2]
                        kb.dma('sp', p_[:], o1[tk, hh * 512:(hh + 1) * 512])
                        kb.dma('sp', r_[:], rr[tk, hh * 512:(hh + 1) * 512])
                        o_ = of[it % 2]
                        kb.tt(o_[:], ps2[:, :], p_[:], ALU.add)
                        s_ = ss[it % 2]
                        kb.act(junk[:], o_[:], AF.Square, accum=s_[:, 0:1])
                        kb.act(s_[:, 1:2], s_[:, 0:1], AF.Sqrt, bias=cx.eps_t[:, 0:1], scale=1.0 / 512)
                        kb.op('dve', lambda E, s_=s_: E.reciprocal(s_[:, 2:3], s_[:, 1:2]), w=[s_], r=[s_])
                        kb.stt(o_[:], o_[:], s_[:, 2:3], hng[:], ALU.mult, ALU.mult)
                        sr_ = sr[it % 2]
                        kb.act(sr_[:], r_[:], AF.Silu)
                        b_ = ob[it % 2]
                        kb.tt(b_[:], o_[:], sr_[:], ALU.mult)
                        kb.dma(SQ, og[tk, hh * 512:(hh + 1) * 512], b_[:], w=[og])
                    for fc in range(2):
                        ps3 = next_ps(cx)
                        kb.mm(ps3[:, :], kst[:, n, fc * 128:(fc + 1) * 128], v_[:])
                        kb.stt(Sf[:, fc, :], Sf[:, fc, :], dec[:, fc, n:n + 1], ps3[:, :], ALU.mult, ALU.add)
                        kb.copy(Sb[:, fc, :], Sf[:, fc, :], e='act')
    proj_out(kb, cx, 'gla_w_out', og[:, 0:2048], 2048)


MIXERS[0] = gla_mixer


def t5_bucket_np(rel):
    import math
    n = np.abs(rel)
    lr = np.log(np.maximum(n, 1).astype(np.float32) / np.float32(8)) / np.float32(math.log(128 / 8))
    large = np.minimum(8 + (lr * np.float32(8)).astype(np.int32), 15)
    return np.where(rel > 0, 16, 0) + np.where(n < 8, n, large)


def attn_consts():
    kp = np.arange(128)[:, None, None]
    dl = np.arange(-1, 2)[None, :, None]
    qf = np.arange(128)[None, None, :]
    rel = dl * 128 + kp - qf
    bk = t5_bucket_np(rel).astype(np.float32)
    mneg = np.where(np.abs(rel) <= 128, 0.0, -30000.0).astype(np.float32)
    return {'c_bk': np.ascontiguousarray(bk.reshape(128, 384)), 'c_mneg': np.ascontiguousarray(mneg.reshape(128, 384))}


def build_bias_tiles(kb, cx, st, swa):
    nc = cx.nc
    sb0 = mk_sb(nc, st)
    T = sb0('a_T', [128, 16, 384], F32)
    cfar = sb0('a_cfar', [128, 32], F32)
    with ExitStack() as s2:
        sb = mk_sb(nc, s2)
        bk = sb('a_bk', [128, 384], F32)
        mk = sb('a_mk', [128, 32, 384], BF16)
        tb = sb('a_tb', [128, 512], F32)
        mn = sb('a_mn', [128, 384], F32)
        kb.dma('sp', bk[:], cx.dram['c_bk'][:, :])
        kb.dma('sp', mn[:], cx.dram['c_mneg'][:, :])
        kb.dma('sp', tb[:], cx.dram['rel_bias'].rearrange("b h -> (b h)").rearrange("(o n) -> o n", o=1).broadcast_to([128, 512]))
        for b in range(32):
            kb.ts(mk[:, b, :], bk[:], float(b), None, op0=ALU.is_equal)
        for h in range(16):
            if swa:
                kb.copy(T[:, h, :], mn[:])
            else:
                kb.memset(T[:, h, :], 0.0)
            for b in range(32):
                kb.stt(T[:, h, :], mk[:, b, :], tb[:, b * 16 + h:b * 16 + h + 1], T[:, h, :], ALU.mult, ALU.add)
            kb.copy(cfar[:, h * 2:h * 2 + 1], tb[:, 15 * 16 + h:15 * 16 + h + 1])
            kb.copy(cfar[:, h * 2 + 1:h * 2 + 2], tb[:, 31 * 16 + h:31 * 16 + h + 1])
    return T, cfar


def qknorm_pass(kb, cx, src, ncols, G, gain_ap, scale, dstT):
    nc = cx.nc
    S = cx.S
    NG = ncols // G
    NCk = ncols // 128
    with ExitStack() as st:
        sb = mk_sb(nc, st)
        xt = [sb('n_x%d' % i, [128, ncols], F32) for i in range(2)]
        sq = sb('n_sq', [128, ncols], F32)
        xb = [sb('n_xb%d' % i, [128, ncols], BF16) for i in range(2)]
        ss = [sb('n_ss%d' % i, [128, 3, NG], F32) for i in range(2)]
        gn = sb('n_g', [128, G], F32)
        stg = [sb('n_st%d' % i, [128, NCk, 128], BF16) for i in range(2)]
        kb.dma('sp', gn[:], gain_ap.broadcast_to([128, G]))
        kb.ts(gn[:], gn[:], float(scale), None, op0=ALU.mult)
        g3 = lambda t: t.rearrange("p (g d) -> p g d", d=G)
        for t in range(S // 128):
            x_ = xt[t % 2]
            s_ = ss[t % 2]
            b_ = xb[t % 2]
            kb.dma('sp', x_[:], src[t * 128:(t + 1) * 128, 0:ncols])
            kb.tt(sq[:], x_[:], x_[:], ALU.mult)
            kb.op('dve', lambda E, s_=s_: E.tensor_reduce(s_[:, 0, :], g3(sq[:]), AX.X, ALU.add), w=[s_], r=[sq])
            kb.act(s_[:, 1, :], s_[:, 0, :], AF.Sqrt, bias=cx.eps_t[:, 0:1], scale=1.0 / G)
            kb.op('dve', lambda E, s_=s_: E.reciprocal(s_[:, 2, :], s_[:, 1, :]), w=[s_], r=[s_])
            kb.tt(g3(sq[:]), g3(x_[:]), s_[:, 2, :].rearrange("p (g o) -> p g o", o=1).broadcast_to([128, NG, G]), ALU.mult)
            kb.tt(g3(b_[:]), g3(sq[:]), gn[:].rearrange("p (o d) -> p o d", o=1).broadcast_to([128, NG, G]), ALU.mult)
            sg = stg[t % 2]
            transpose_to(kb, cx, b_, sg, 0, nchunks=NCk)
            kb.dma(SQ, dstT[0:ncols, :].rearrange("(c p) s -> p c s", p=128)[:, :, t * 128:(t + 1) * 128], sg[:], w=[dstT])


def acc_slots(cx):
    return [(cx.acc[i], 0, True) for i in range(4)]


def diff_mixer(kb, cx, li):
    import math
    nc = cx.nc
    S = cx.S
    NCH = S // 128
    NQT = S // 512
    lam_init = 0.8 - 0.6 * math.exp(-0.3 * li)
    qf = dscr(cx, 'at_q', [S, 2048], F32)
    kf = dscr(cx, 'at_k', [S, 2048], F32)
    vv = dscr(cx, 'at_v', [S, 2048], BF16)
    qT = dscr(cx, 'at_qT', [2048, S], BF16)
    kT = dscr(cx, 'at_kT', [2048, S], BF16)
    og = dscr(cx, 'mix_og', [S, 4096], BF16)
    proj_in(kb, cx, li, 'diff_w_in', [(0, 2048, 'N', qf, 1.0), (2048, 2048, 'N', kf, 1.0), (4096, 2048, 'N', vv, 1.0)])
    qknorm_pass(kb, cx, qf, 2048, 64, cx.dram['diff_q_norm'][0:1, :], 0.125, qT)
    qknorm_pass(kb, cx, kf, 2048, 64, cx.dram['diff_k_norm'][0:1, :], 1.0, kT)
    with ExitStack() as st:
        sb = mk_sb(nc, st)
        T, cfar = build_bias_tiles(kb, cx, st, swa=False)
        lam = sb('d_lam', [128, 4, 64], F32)
        lw = sb('d_lw', [128, 8], F32)
        sub = sb('d_sub', [128, 128], F32)
        qh = sb('d_q', [128, S], BF16)
        kh = sb('d_k', [128, S], BF16)
        v1 = sb('d_v1', [128, NCH, 130], BF16)
        tmp = [sb('d_tmp%d' % i, [128, 512], F32) for i in range(2)]
        PT = [sb('d_PT%d' % i, [128, 512], BF16) for i in range(3)]
        res = [sb('d_res%d' % i, [128, 4, 130], F32) for i in range(2)]
        rc = [sb('d_rc%d' % i, [128, 8], F32) for i in range(2)]
        ot = [sb('d_ot%d' % i, [128, 128], F32) for i in range(2)]
        ob = [sb('d_ob%d' % i, [128, 128], BF16) for i in range(2)]
        junk = sb('d_junk', [128, 128], BF16)
        kb.dma('sp', lam[:], cx.dram['diff_lambda'].rearrange("a d -> (a d)").rearrange("(o n) -> o n", o=1).broadcast_to([128, 256]))
        kb.tt(lam[:, 0, :], lam[:, 0, :], lam[:, 1, :], ALU.mult)
        kb.tt(lam[:, 2, :], lam[:, 2, :], lam[:, 3, :], ALU.mult)
        kb.op('dve', lambda E: E.reduce_sum(lw[:, 0:1], lam[:, 0, :], AX.X), w=[lw], r=[lam])
        kb.op('dve', lambda E: E.reduce_sum(lw[:, 1:2], lam[:, 2, :], AX.X), w=[lw], r=[lam])
        kb.act(lw[:, 2:4], lw[:, 0:2], AF.Exp)
        kb.tt(lw[:, 4:5], lw[:, 3:4], lw[:, 2:3], ALU.subtract)
        kb.ts(lw[:, 5:6], lw[:, 4:5], -lam_init, None, op0=ALU.add)
        kb.dma('sp', sub[:], cx.dram['diff_subln'][0:1, :].broadcast_to([128, 128]))
        kb.ts(sub[:], sub[:], float(1.0 - lam_init), None, op0=ALU.mult)
        kb.memset(v1[:], 1.0)
        slots = acc_slots(cx)
        npt = 0
        for h in range(16):
            kb.dma('sp', qh[:], qT[h * 128:(h + 1) * 128, :])
            kb.dma('sp', kh[:], kT[h * 128:(h + 1) * 128, :])
            kb.dma('sp', v1[:, :, 0:128], vv[:, h * 128:(h + 1) * 128].rearrange("(n p) d -> p n d", p=128))
            for qt in range(NQT):
                for m in range(2):
                    pr = slice(m * 64, (m + 1) * 64)
                    def qk(kc):
                        ps = next_ps(cx)
                        kb.mm(ps[:, :], kh[pr, kc * 128:(kc + 1) * 128], qh[pr, qt * 512:(qt + 1) * 512])
                        return ps
                    ps_next = qk(0)
                    for kc in range(NCH):
                        ps = ps_next
                        if kc + 1 < NCH:
                            ps_next = qk(kc + 1)
                        p_ = PT[npt % 3]
                        npt += 1
                        dls = [kc - (qt * 4 + qb) for qb in range(4)]
                        if all(abs(dl) >= 2 for dl in dls):
                            sgn = 1 if dls[0] > 0 else 0
                            kb.act(p_[:], ps[:, :], AF.Exp, bias=cfar[:, h * 2 + sgn:h * 2 + sgn + 1], scale=1.0)
                        else:
                            t_ = tmp[npt % 2]
                            for qb, dl in enumerate(dls):
                                blk = slice(qb * 128, (qb + 1) * 128)
                                if abs(dl) <= 1:
                                    kb.tt(t_[:, blk], ps[:, blk], T[:, h, (dl + 1) * 128:(dl + 2) * 128], ALU.add)
                                else:
                                    sgn = 1 if dl > 0 else 0
                                    kb.ts(t_[:, blk], ps[:, blk], cfar[:, h * 2 + sgn:h * 2 + sgn + 1], None, op0=ALU.add)
                            kb.act(p_[:], t_[:], AF.Exp)
                        for qb in range(4):
                            bank, c0, first = slots[qb]
                            kb.mm(bank[:, c0:c0 + 130], p_[:, qb * 128:(qb + 1) * 128], v1[:, kc, :],
                                  start=(kc == 0 and first), stop=(kc == NCH - 1), skip_group_check=True)
                    r_ = res[m]
                    for qb in range(4):
                        bank, c0, first = slots[qb]
                        kb.copy(r_[:, qb, :], bank[:, c0:c0 + 130], e='act' if qb % 2 else 'dve')
                for qb in range(4):
                    c_ = rc[qb % 2]
                    o_ = ot[qb % 2]
                    kb.op('dve', lambda E, c_=c_, qb=qb: E.reciprocal(c_[:, 0:1], res[0][:, qb, 128:129]), w=[c_], r=[res[0]])
                    kb.op('dve', lambda E, c_=c_, qb=qb: E.reciprocal(c_[:, 1:2], res[1][:, qb, 128:129]), w=[c_], r=[res[1]])
                    kb.tt(c_[:, 2:3], c_[:, 1:2], lw[:, 5:6], ALU.mult)
                    kb.ts(o_[:], res[0][:, qb, 0:128], c_[:, 0:1], None, op0=ALU.mult)
                    kb.stt(o_[:], res[1][:, qb, 0:128], c_[:, 2:3], o_[:], ALU.mult, ALU.add)
                    kb.act(junk[:], o_[:], AF.Square, accum=c_[:, 3:4])
                    kb.act(c_[:, 4:5], c_[:, 3:4], AF.Sqrt, bias=cx.eps_t[:, 0:1], scale=1.0 / 128)
                    kb.op('dve', lambda E, c_=c_: E.reciprocal(c_[:, 5:6], c_[:, 4:5]), w=[c_], r=[c_])
                    b_ = ob[qb % 2]
                    kb.stt(b_[:], o_[:], c_[:, 5:6], sub[:], ALU.mult, ALU.mult)
                    t0 = (qt * 4 + qb) * 128
                    kb.dma(SQ, og[t0:t0 + 128, h * 128:(h + 1) * 128], b_[:], w=[og])
    proj_out(kb, cx, 'diff_w_out', og[:, 0:2048], 2048)


def swa_mixer(kb, cx, li):
    nc = cx.nc
    S = cx.S
    NCH = S // 128
    qf = dscr(cx, 'at_q', [S, 2048], F32)
    kf = dscr(cx, 'at_k', [S, 2048], F32)
    vv = dscr(cx, 'at_v', [S, 2048], BF16)
    qT = dscr(cx, 'at_qT', [2048, S], BF16)
    kT = dscr(cx, 'at_kT', [2048, S], BF16)
    og = dscr(cx, 'mix_og', [S, 4096], BF16)
    proj_in(kb, cx, li, 'swa_w_in', [(0, 2048, 'N', qf, 1.0), (2048, 512, 'N', kf, 1.0), (2560, 512, 'N', vv, 1.0)])
    qknorm_pass(kb, cx, qf, 2048, 128, cx.dram['swa_q_norm'][0:1, :], 128 ** -0.5, qT)
    qknorm_pass(kb, cx, kf, 512, 128, cx.dram['swa_k_norm'][0:1, :], 1.0, kT)
    with ExitStack() as st:
        sb = mk_sb(nc, st)
        T, cfar = build_bias_tiles(kb, cx, st, swa=True)
        snk = sb('w_snk', [128, 16], F32)
        q4 = sb('w_q4', [128, 4, S], BF16)
        kg = sb('w_kg', [128, S], BF16)
        v1 = sb('w_v1', [128, NCH, 130], BF16)
        tmp = [sb('w_tmp%d' % i, [128, 512], F32) for i in range(2)]
        PT = [sb('w_PT%d' % i, [128, 512], BF16) for i in range(3)]
        rc = [sb('w_rc%d' % i, [128, 4], F32) for i in range(2)]
        ob = [sb('w_ob%d' % i, [128, 128], BF16) for i in range(2)]
        kb.dma('sp', snk[:], cx.dram['swa_sink'][0:1, :].broadcast_to([128, 16]))
        kb.act(snk[:], snk[:], AF.Exp)
        kb.memset(v1[:], 1.0)
        slots = acc_slots(cx)
        npt = 0
        for g in range(4):
            kb.dma('sp', q4[:], qT[g * 512:(g + 1) * 512, :].rearrange("(c p) s -> p c s", p=128))
            kb.dma('sp', kg[:], kT[g * 128:(g + 1) * 128, :])
            kb.dma('sp', v1[:, :, 0:128], vv[:, g * 128:(g + 1) * 128].rearrange("(n p) d -> p n d", p=128))
            for qb in range(NCH):
                kcs = [kc for kc in (qb - 1, qb, qb + 1) if 0 <= kc < NCH]
                for ik, kc in enumerate(kcs):
                    dl = kc - qb
                    ps = next_ps(cx)
                    kb.mm(ps[:, :], kg[:, kc * 128:(kc + 1) * 128], q4[:, :, qb * 128:(qb + 1) * 128])
                    t_ = tmp[npt % 2]
                    p_ = PT[npt % 3]
                    npt += 1
                    for hh in range(4):
                        blk = slice(hh * 128, (hh + 1) * 128)
                        kb.tt(t_[:, blk], ps[:, blk], T[:, g * 4 + hh, (dl + 1) * 128:(dl + 2) * 128], ALU.add)
                    kb.act(p_[:], t_[:], AF.Exp)
                    for hh in range(4):
                        bank, c0, first = slots[hh]
                        kb.mm(bank[:, c0:c0 + 130], p_[:, hh * 128:(hh + 1) * 128], v1[:, kc, :],
                              start=(ik == 0 and first), stop=(ik == len(kcs) - 1), skip_group_check=True)
                for hh in range(4):
                    bank, c0, first = slots[hh]
                    h = g * 4 + hh
                    c_ = rc[hh % 2]
                    kb.tt(c_[:, 0:1], bank[:, c0 + 128:c0 + 129], snk[:, h:h + 1], ALU.add)
                    kb.op('dve', lambda E, c_=c_: E.reciprocal(c_[:, 1:2], c_[:, 0:1]), w=[c_], r=[c_])
                    b_ = ob[hh % 2]
                    kb.ts(b_[:], bank[:, c0:c0 + 128], c_[:, 1:2], None, op0=ALU.mult)
                    kb.dma(SQ, og[qb * 128:(qb + 1) * 128, h * 128:(h + 1) * 128], b_[:], w=[og])
    proj_out(kb, cx, 'swa_w_out', og[:, 0:2048], 2048)


MIXERS[2] = diff_mixer
MIXERS[3] = swa_mixer


def gdn_mixer(kb, cx, li):
    nc = cx.nc
    S = cx.S
    NCH = S // 128
    preT = dscr(cx, 'gd_preT', [8192, S], BF16)
    qkT = dscr(cx, 'gd_qkT', [4096, S], BF16)
    vT = dscr(cx, 'gd_vT', [4096, S], BF16)
    zz = dscr(cx, 'gd_z', [S, 4096], BF16)
    abt = dscr(cx, 'gd_ab', [S, 128], F32)
    o1 = dscr(cx, 'gd_o1', [S, 4096], F32)
    og = dscr(cx, 'mix_og', [S, 4096], BF16)
    proj_in(kb, cx, li, 'gdn_w_in', [(0, 8192, 'T', preT, 1.0), (8192, 4096, 'N', zz, 1.0), (12288, 128, 'N', abt, 1.0)])
    with ExitStack() as st:
        sb = mk_sb(nc, st)
        cwl = [sb('c_w%d' % i, [128, 5], F32) for i in range(2)]
        xp = [sb('c_xp%d' % i, [128, S + 4], BF16) for i in range(2)]
        y = sb('c_y', [128, S], F32)
        ys = sb('c_ys', [128, S], F32)
        sq = sb('c_sq', [128, S], BF16)
        rs = sb('c_rs', [128, 512], F32)
        ob = [sb('c_ob%d' % i, [128, S], BF16) for i in range(2)]
        onesb = sb('c_ones', [128, 128], BF16)
        kb.memset(onesb[:], 1.0)
        for i in range(2):
            kb.memset(xp[i][:, 0:2], 0.0)
            kb.memset(xp[i][:, S + 2:S + 4], 0.0)
        for c in range(64):
            x_ = xp[c % 2]
            kb.dma('sp', x_[:, 2:S + 2], preT[c * 128:(c + 1) * 128, :])
            cw = cwl[c % 2]
            kb.dma('sp', cw[:], cx.dram['gdn_conv'][:, c * 128:(c + 1) * 128].rearrange("j p -> p j"), allow_slow_non_contiguous=True)
            kb.ts(y[:], x_[:, 0:S], cw[:, 0:1], None, op0=ALU.mult)
            for j in range(1, 5):
                kb.stt(y[:], x_[:, j:j + S], cw[:, j:j + 1], y[:], ALU.mult, ALU.add)
            kb.act(ys[:], y[:], AF.Silu)
            o_ = ob[c % 2]
            if c < 32:
                kb.tt(sq[:], ys[:], ys[:], ALU.mult, e='pool')
                for b0 in range(0, S, 512):
                    ps = next_ps(cx)
                    kb.mm(ps[:, :], onesb[:], sq[:, b0:b0 + 512])
                    kb.act(rs[:], ps[:, :], AF.Sqrt, bias=cx.eps_t[:, 0:1], scale=1.0)
                    kb.op('dve', lambda E: E.reciprocal(rs[:], rs[:]), w=[rs], r=[rs])
                    if c < 16:
                        kb.stt(o_[:, b0:b0 + 512], ys[:, b0:b0 + 512], 128 ** -0.5, rs[:], ALU.mult, ALU.mult)
                    else:
                        kb.tt(o_[:, b0:b0 + 512], ys[:, b0:b0 + 512], rs[:], ALU.mult)
                kb.dma(SQ, qkT[c * 128:(c + 1) * 128, :], o_[:], w=[qkT])
            else:
                kb.copy(o_[:], ys[:], e='pool')
                kb.dma(SQ, vT[(c - 32) * 128:(c - 31) * 128, :], o_[:], w=[vT])
    with ExitStack() as st:
        sb = mk_sb(nc, st)
        psl = cx.ps + cx.acc
        pidx = [0]

        def nps():
            t = psl[pidx[0] % len(psl)]
            pidx[0] += 1
            return t
        g_all = sb('s_g', [128, NCH, 64], F32)
        nb_all = sb('s_nb', [128, NCH, 64], F32)
        gc_all = sb('s_gc', [128, NCH, 64], F32)
        ngc_all = sb('s_ngc', [128, NCH, 64], F32)
        alog = sb('s_alog', [128, 64], F32)
        dtb = sb('s_dtb', [128, 64], F32)
        hng = sb('s_hng', [128, 128], F32)
        UT = sb('s_ut', [128, 128], F32)
        LT = sb('s_lt', [128, 128], F32)
        MSU = sb('s_msu', [128, 128], F32)
        MSL = sb('s_msl', [128, 128], F32)
        abl = [sb('s_ab%d' % i, [128, 128], F32) for i in range(2)]
        tmpa = sb('s_tmpa', [128, 64], F32)
        kb.dma('sp', UT[:], cx.dram['c_masku'][:, :])
        kb.dma('sp', LT[:], cx.dram['c_maskl'][:, :])
        kb.tt(MSU[:], UT[:], cx.ident_f[:], ALU.subtract)
        kb.tt(MSL[:], LT[:], cx.ident_f[:], ALU.subtract)
        kb.dma('sp', alog[:], cx.dram['gdn_a_log'].rearrange("d h -> (d h)").rearrange("(o n) -> o n", o=1).broadcast_to([128, 64]))
        kb.dma('sp', dtb[:], cx.dram['gdn_dt_bias'].rearrange("d h -> (d h)").rearrange("(o n) -> o n", o=1).broadcast_to([128, 64]))
        kb.dma('sp', hng[:], cx.dram['gdn_head_norm'][0:1, :].broadcast_to([128, 128]))
        kb.act(alog[:], alog[:], AF.Exp)
        kb.ts(alog[:], alog[:], -1.0, None, op0=ALU.mult)
        for n in range(NCH):
            a_ = abl[n % 2]
            kb.dma('sp', a_[:], abt[n * 128:(n + 1) * 128, :])
            kb.tt(tmpa[:], a_[:, 0:64], dtb[:], ALU.add)
            kb.act(tmpa[:], tmpa[:], AF.Exp)
            kb.act(tmpa[:], tmpa[:], AF.Ln, bias=1.0, scale=1.0)
            kb.tt(g_all[:, n, :], tmpa[:], alog[:], ALU.mult)
            kb.act(tmpa[:], a_[:, 64:128], AF.Exp, scale=-1.0)
            kb.ts(tmpa[:], tmpa[:], 1.0, None, op0=ALU.add)
            kb.op('dve', lambda E: E.reciprocal(tmpa[:], tmpa[:]), w=[tmpa], r=[tmpa])
            kb.ts(nb_all[:, n, :], tmpa[:], -1.0, None, op0=ALU.mult)
            ps = nps()
            kb.mm(ps[:, 0:32], UT[:], g_all[:, n, 0:32])
            kb.mm(ps[:, 32:64], LT[:], g_all[:, n, 32:64])
            kb.copy(gc_all[:, n, :], ps[:, 0:64])
            kb.ts(ngc_all[:, n, :], gc_all[:, n, :], -1.0, None, op0=ALU.mult, e='dve')
        qh = sb('s_q', [128, S], BF16)
        kh = sb('s_k', [128, S], BF16)
        ktok = sb('s_ktok', [128, NCH, 128], BF16)
        vth = sb('s_vth', [128, S], BF16)
        vtok = [sb('s_vtok%d' % i, [128, NCH, 128], BF16) for i in range(2)]
        bv = [sb('s_bv%d' % i, [128, NCH, 128], BF16) for i in range(4)]
        Sf = [sb('s_Sf%d' % i, [128, 128], F32) for i in range(4)]
        Sb = [sb('s_Sb%d' % i, [128, 128], BF16) for i in range(4)]
        KKs = [sb('s_kk%d' % i, [128, 128], F32) for i in range(2)]
        QKr = [sb('s_qkr%d' % i, [128, 128], F32) for i in range(2)]
        R = 4
        mkf = lambda nm: [sb('s_%s%d' % (nm, i), [128, 128], F32) for i in range(R)]
        mkb = lambda nm: [sb('s_%s%d' % (nm, i), [128, 128], BF16) for i in range(R)]
        gB, gR, GT, GG, Er = mkf('gB'), mkf('gR'), mkf('GT'), mkf('GG'), mkf('Er')
        Xa, Xb_, Ya, Yb_, Pa = mkf('Xa'), mkf('Xb'), mkf('Ya'), mkf('Yb'), mkf('Pa')
        QKT, MT, kdT, qdT, kst, Xv, vnb = mkb('QKT'), mkb('MT'), mkb('kdT'), mkb('qdT'), mkb('kst'), mkb('Xv'), mkb('vnb')
        sc = [sb('s_sc%d' % i, [128, 8], F32) for i in range(R)]
        of = [sb('s_of%d' % i, [128, 128], F32) for i in range(2)]
        op_ = [sb('s_op%d' % i, [128, 128], F32) for i in range(2)]
        zt = [sb('s_z%d' % i, [128, 128], BF16) for i in range(2)]
        zs = [sb('s_zs%d' % i, [128, 128], F32) for i in range(2)]
        obf = [sb('s_obf%d' % i, [128, 128], BF16) for i in range(2)]
        junk = sb('s_junk', [128, 128], BF16)
        rot = [0]
        for hq in range(16):
            kb.dma('sp', qh[:], qkT[hq * 128:(hq + 1) * 128, :])
            kb.dma('sp', kh[:], qkT[2048 + hq * 128:2048 + (hq + 1) * 128, :])
            for n0 in range(0, NCH, 8):
                pb = next_pb(cx)
                for n in range(8):
                    kb.tr(pb[:, n * 128:(n + 1) * 128], kh[:, (n0 + n) * 128:(n0 + n + 1) * 128], cx.ident_b[:])
                kb.copy(ktok[:, n0:n0 + 8, :], pb[:, :].rearrange("p (n t) -> p n t", t=128), e='act')
            chains = []
            for v2 in range(2):
                hv = hq * 2 + v2
                kb.dma('sp', vth[:], vT[hv * 128:(hv + 1) * 128, :])
                for n0 in range(0, NCH, 8):
                    pb = next_pb(cx)
                    for n in range(8):
                        kb.tr(pb[:, n * 128:(n + 1) * 128], vth[:, (n0 + n) * 128:(n0 + n + 1) * 128], cx.ident_b[:])
                    kb.copy(vtok[v2][:, n0:n0 + 8, :], pb[:, :].rearrange("p (n t) -> p n t", t=128), e='act')
                for d in range(2):
                    ci = v2 * 2 + d
                    col = d * 32 + hv
                    kb.tt(bv[ci][:], vtok[v2][:], nb_all[:, :, col:col + 1].broadcast_to([128, NCH, 128]), ALU.mult)
                    kb.ts(bv[ci][:], bv[ci][:], -1.0, None, op0=ALU.mult, e='pool')
                    kb.memset(Sf[ci][:], 0.0)
                    kb.memset(Sb[ci][:], 0.0)
                    chains.append((ci, v2, hv, d, col))
            done = {}
            for s_ in range(NCH):
                raw = {}
                for d, n in ((0, s_), (1, NCH - 1 - s_)):
                    tk = slice(n * 128, (n + 1) * 128)
                    ps = nps()
                    kb.mm(ps[:, 0:128], kh[:, tk], kh[:, tk])
                    kb.mm(ps[:, 128:256], kh[:, tk], qh[:, tk])
                    kb.tt(KKs[d][:], ps[:, 0:128], (MSL if d == 0 else MSU)[:], ALU.mult)
                    kb.copy(QKr[d][:], ps[:, 128:256], e='dve')
                    raw[d] = n
                stt_ = {}
                for (ci, v2, hv, d, col) in chains:
                    n = raw[d]
                    r = ci
                    li_ = 127 if d == 0 else 0
                    gcol = gc_all[:, n, col:col + 1]
                    ngcol = ngc_all[:, n, col:col + 1]
                    nbcol = nb_all[:, n, col:col + 1]
                    kb.copy(gB[r][:], g_all[:, n, col:col + 1].broadcast_to([128, 128]), e='pool')
                    pg = nps()
                    kb.mm(pg[:, 0:128], gB[r][:], (UT if d == 0 else LT)[:])
                    kb.copy(gR[r][:], pg[:, 0:128], e='dve')
                    kb.ts(GT[r][:], gR[r][:], ngcol, 0.0, op0=ALU.add, op1=ALU.min)
                    kb.act(GT[r][:], GT[r][:], AF.Exp)
                    kb.ts(GG[r][:], gR[r][:], gcol, 0.0, op0=ALU.subtract, op1=ALU.max)
                    kb.act(GG[r][:], GG[r][:], AF.Exp, scale=-1.0)
                    kb.act(Er[r][:], gR[r][:], AF.Exp)
                    kb.copy(sc[r][:, 0:1], gR[r][:, li_:li_ + 1])
                    kb.act(sc[r][:, 1:2], sc[r][:, 0:1], AF.Exp)
                    kb.act(sc[r][:, 2:3], gcol, AF.Exp, bias=sc[r][:, 0:1], scale=-1.0)
                    kb.tt(GT[r][:], GT[r][:], (UT if d == 0 else LT)[:], ALU.mult)
                    kb.tt(QKT[r][:], QKr[d][:], GT[r][:], ALU.mult)
                    kb.stt(Ya[r][:], KKs[d][:], nbcol, GG[r][:], ALU.mult, ALU.mult)
                    stt_[ci] = [Xa[r], Ya[r], Xb_[r], Yb_[r]]
                for (ci, v2, hv, d, col) in chains:
                    r = ci
                    px = nps()
                    kb.tr(px[:, 0:128], Ya[r][:], cx.ident_f[:])
                    kb.copy(Xa[r][:], px[:, 0:128], e='dve')
                    kb.tt(Pa[r][:], Xa[r][:], cx.ident_f[:], ALU.add)
                for k_ in range(1, 7):
                    for (ci, v2, hv, d, col) in chains:
                        r = ci
                        X, Y, Xn, Yn = stt_[ci]
                        pyy = nps()
                        kb.mm(pyy[:, 0:128], X[:], Y[:])
                        if k_ < 6:
                            pxx = nps()
                            kb.mm(pxx[:, 0:128], Y[:], X[:])
                        kb.copy(Yn[:], pyy[:, 0:128], e='act')
                        if k_ < 6:
                            kb.copy(Xn[:], pxx[:, 0:128], e='dve')
                        stt_[ci] = [Xn, Yn, X, Y]
                    for (ci, v2, hv, d, col) in chains:
                        r = ci
                        Yc = stt_[ci][1]
                        pp = nps()
                        kb.mm(pp[:, 0:128], Yc[:], Pa[r][:])
                        kb.tt(Pa[r][:], Pa[r][:], pp[:, 0:128], ALU.add)
                for (ci, v2, hv, d, col) in chains:
                    n = raw[d]
                    tk = slice(n * 128, (n + 1) * 128)
                    r = ci
                    kb.copy(MT[r][:], Pa[r][:], e='act')
                    kb.tt(kdT[r][:], kh[:, tk], Er[r][:], ALU.mult, e='pool')
                    kb.tt(qdT[r][:], qh[:, tk], Er[r][:], ALU.mult, e='pool')
                    kb.ts(kst[r][:], ktok[:, n, :], sc[r][:, 2:3], None, op0=ALU.mult)
                for (ci, v2, hv, d, col) in chains:
                    r = ci
                    nbcol = nb_all[:, raw[d], col:col + 1]
                    p1 = nps()
                    kb.mm(p1[:, 0:128], kdT[r][:], Sb[ci][:])
                    kb.stt(Xv[r][:], p1[:, 0:128], nbcol, bv[ci][:, raw[d], :], ALU.mult, ALU.add)
                for (ci, v2, hv, d, col) in chains:
                    r = ci
                    p2 = nps()
                    kb.mm(p2[:, 0:128], MT[r][:], Xv[r][:])
                    kb.copy(vnb[r][:], p2[:, 0:128], e='act')
                for (ci, v2, hv, d, col) in chains:
                    n = raw[d]
                    tk = slice(n * 128, (n + 1) * 128)
                    r = ci
                    p3 = nps()
                    kb.mm(p3[:, 0:128], qdT[r][:], Sb[ci][:], start=True, stop=False)
                    kb.mm(p3[:, 0:128], QKT[r][:], vnb[r][:], start=False, stop=True)
                    key = (hv, n)
                    rot[0] += 1
                    i2 = rot[0] % 2
                    if key not in done:
                        done[key] = 1
                        kb.copy(of[i2][:], p3[:, 0:128], e='act')
                        kb.dma(SQ, o1[tk, hv * 128:(hv + 1) * 128], of[i2][:], w=[o1])
                    else:
                        kb.dma('sp', op_[i2][:], o1[tk, hv * 128:(hv + 1) * 128])
                        kb.dma('sp', zt[i2][:], zz[tk, hv * 128:(hv + 1) * 128])
                        kb.tt(of[i2][:], p3[:, 0:128], op_[i2][:], ALU.add)
                        kb.act(junk[:], of[i2][:], AF.Square, accum=sc[r][:, 3:4])
                        kb.act(sc[r][:, 4:5], sc[r][:, 3:4], AF.Sqrt, bias=cx.eps_t[:, 0:1], scale=1.0 / 128)
                        kb.op('dve', lambda E, r=r: E.reciprocal(sc[r][:, 5:6], sc[r][:, 4:5]), w=[sc[r]], r=[sc[r]])
                        kb.stt(of[i2][:], of[i2][:], sc[r][:, 5:6], hng[:], ALU.mult, ALU.mult)
                        kb.act(zs[i2][:], zt[i2][:], AF.Silu)
                        kb.tt(obf[i2][:], of[i2][:], zs[i2][:], ALU.mult)
                        kb.dma(SQ, og[tk, hv * 128:(hv + 1) * 128], obf[i2][:], w=[og])
                for (ci, v2, hv, d, col) in chains:
                    r = ci
                    p4 = nps()
                    kb.mm(p4[:, 0:128], kst[r][:], vnb[r][:])
                    kb.stt(Sf[ci][:], Sf[ci][:], sc[r][:, 1:2], p4[:, 0:128], ALU.mult, ALU.add)
                    kb.copy(Sb[ci][:], Sf[ci][:], e='act')
    proj_out(kb, cx, 'gdn_w_out', og[:, 0:4096], 4096)


MIXERS[1] = gdn_mixer


SMALL_INPUTS = {
    'gla_w_gate_up': [2, 16, 1024], 'gla_b_gate': [2, 1024], 'gla_head_norm': [1, 512],
    'gdn_conv': [5, 8192], 'gdn_a_log': [2, 32], 'gdn_dt_bias': [2, 32], 'gdn_head_norm': [1, 128],
    'diff_q_norm': [1, 64], 'diff_k_norm': [1, 64], 'diff_lambda': [4, 64], 'diff_subln': [1, 128],
    'swa_q_norm': [1, 128], 'swa_k_norm': [1, 128], 'swa_sink': [1, 16],
}


def kernel(**inputs):
    x = np.asarray(inputs['x'])
    ncores = NCORES
    cfg = dict(S=S, layers=[0, 1, 2, 3], ncores=ncores, small_inputs=SMALL_INPUTS)
    outs = run(cfg, inputs, [x[b] for b in range(ncores)])
    return np.stack(outs, axis=0).astype(np.float32)
```
